# Optimizing a Trainium2 kernel written in Bass

```python
import math
import jax, jax.numpy as jnp
from jax import lax
import numpy as np

D_MODEL = 1024
BATCH = 8
SEQ = 4096
DEPTH = 2

D_MIX = D_MODEL
HEAD_DIM = 64
ATT_WIDTH = 3 * D_MIX // 8
ATT_HEADS = ATT_WIDTH // HEAD_DIM
LRU_WIDTH = 3 * D_MIX // 8
LRU_BLOCKS = 6
LRU_BLOCK_DIM = LRU_WIDTH // LRU_BLOCKS
S5_WIDTH = D_MIX - ATT_WIDTH - LRU_WIDTH
S5_GROUP_CH = 16
S5_GROUPS = S5_WIDTH // S5_GROUP_CH
S5_STATE = 64
CONV_WIDTH = 4
LRU_C = 8.0
MOBA_BLOCK = 256
MOBA_TOPK = 3
Q_CHUNK = 64
ROPE_THETA = 10000.0
D_FF = 2816
N_EXPERTS = 8
TOP_K = 2
D_FF_EXPERT = 3584
N_DENSE = (DEPTH + 1) // 2
N_MOE = DEPTH // 2
IN_WIDTH = 3 * ATT_WIDTH + 2 * LRU_WIDTH + S5_WIDTH
IN_SPLITS = (ATT_WIDTH, 2 * ATT_WIDTH, 3 * ATT_WIDTH,
             3 * ATT_WIDTH + LRU_WIDTH, 3 * ATT_WIDTH + 2 * LRU_WIDTH)
EPS = 1e-6
NEG = -1e30

kernel_name = 'hybrid_moba_rglru_s5_moe_block'


def rms_norm(x, g):
    x32 = x.astype(jnp.float32)
    y = x32 * lax.rsqrt(jnp.mean(x32 * x32, axis=-1, keepdims=True) + EPS)
    return (y * g.astype(jnp.float32)).astype(x.dtype)


def rope_tables(positions):
    inv = ROPE_THETA ** (-jnp.arange(0, HEAD_DIM, 2, dtype=jnp.float32) / HEAD_DIM)
    ang = positions.astype(jnp.float32)[..., None] * inv
    return jnp.cos(ang)[:, :, None, :], jnp.sin(ang)[:, :, None, :]


def apply_rope(x, cos, sin):
    half = x.shape[-1] // 2
    x32 = x.astype(jnp.float32)
    x1, x2 = x32[..., :half], x32[..., half:]
    return jnp.concatenate([x1 * cos - x2 * sin, x2 * cos + x1 * sin], axis=-1).astype(x.dtype)


def moba_attention(q, k, v):
    b, s, h, d = q.shape
    n_blk = -(-s // MOBA_BLOCK)
    s_pad = n_blk * MOBA_BLOCK
    top_k = min(MOBA_TOPK, n_blk)
    q = jnp.transpose(q, (0, 2, 1, 3)) * (d ** -0.5)
    pad = ((0, 0), (0, 0), (0, s_pad - s), (0, 0))
    k = jnp.pad(jnp.transpose(k, (0, 2, 1, 3)), pad)
    v = jnp.pad(jnp.transpose(v, (0, 2, 1, 3)), pad)
    k_blocks = k.reshape(b, h, n_blk, MOBA_BLOCK, d)
    v_blocks = v.reshape(b, h, n_blk, MOBA_BLOCK, d)
    k_mean = jnp.mean(k_blocks.astype(jnp.float32), axis=3)
    gather = jax.vmap(jax.vmap(lambda blocks, idx: blocks[idx]))
    blk_ids = jnp.arange(n_blk)
    q_off = jnp.arange(Q_CHUNK)
    k_off = jnp.arange(MOBA_BLOCK)

    def chunk(ci):
        s0 = ci * Q_CHUNK
        j = s0 // MOBA_BLOCK
        qc = lax.dynamic_slice_in_dim(q, s0, Q_CHUNK, axis=2)
        gate = jnp.einsum('bhqd,bhnd->bhqn', qc.astype(jnp.float32), k_mean)
        gate = jnp.where(blk_ids < j, gate, NEG)
        _, sel = lax.top_k(gate, top_k)
        sel_valid = jnp.arange(top_k) < j
        k_sel = gather(k_blocks, sel)
        v_sel = gather(v_blocks, sel)
        k_own = lax.dynamic_slice_in_dim(k, j * MOBA_BLOCK, MOBA_BLOCK, axis=2)
        v_own = lax.dynamic_slice_in_dim(v, j * MOBA_BLOCK, MOBA_BLOCK, axis=2)
        s_sel = jnp.einsum('bhqd,bhqnkd->bhqnk', qc, k_sel).astype(jnp.float32)
        s_sel = jnp.where(sel_valid[:, None], s_sel, NEG).reshape(b, h, Q_CHUNK, top_k * MOBA_BLOCK)
        s_own = jnp.einsum('bhqd,bhkd->bhqk', qc, k_own).astype(jnp.float32)
        causal = (j * MOBA_BLOCK + k_off)[None, :] <= (s0 + q_off)[:, None]
        s_own = jnp.where(causal, s_own, NEG)
        p = jax.nn.softmax(jnp.concatenate([s_own, s_sel], axis=-1), axis=-1).astype(v.dtype)
        p_own = p[..., :MOBA_BLOCK]
        p_sel = p[..., MOBA_BLOCK:].reshape(b, h, Q_CHUNK, top_k, MOBA_BLOCK)
        return (jnp.einsum('bhqk,bhkd->bhqd', p_own, v_own)
                + jnp.einsum('bhqnk,bhqnkd->bhqd', p_sel, v_sel))

    out = lax.map(chunk, jnp.arange(s // Q_CHUNK))
    return jnp.transpose(out, (1, 0, 3, 2, 4)).reshape(b, s, h * d)


def linear_scan(a, u):
    def combine(e1, e2):
        return (e1[0] * e2[0], e2[0] * e1[1] + e2[1])
    return lax.associative_scan(combine, (a, u), axis=1)[1]


def rglru_branch(xr, gate, conv_w, conv_b, w_a, b_a, w_x, b_x, lam):
    b, s, w = xr.shape
    xc = lax.conv_general_dilated(xr, conv_w[:, None, :], window_strides=(1,),
                                  padding=[(CONV_WIDTH - 1, 0)],
                                  dimension_numbers=('NWC', 'WIO', 'NWC'),
                                  feature_group_count=w) + conv_b
    xb = xc.reshape(b, s, LRU_BLOCKS, LRU_BLOCK_DIM)
    r = jax.nn.sigmoid(jnp.einsum('bshi,hij->bshj', xb, w_a).reshape(b, s, w) + b_a)
    i = jax.nn.sigmoid(jnp.einsum('bshi,hij->bshj', xb, w_x).reshape(b, s, w) + b_x)
    log_a = -LRU_C * r.astype(jnp.float32) * jax.nn.softplus(-lam.astype(jnp.float32))
    a = jnp.exp(log_a)
    u = jnp.sqrt(jnp.maximum(-jnp.expm1(2.0 * log_a), 0.0)) * (i * xc).astype(jnp.float32)
    hseq = linear_scan(a, u)
    return hseq.astype(xr.dtype) * jax.nn.gelu(gate)


def s5_branch(u, lam_re, lam_im, log_dt, b_re, b_im, c_re, c_im, d_skip, glu_w, glu_b):
    b, s, w = u.shape
    f32 = jnp.float32
    ug = u.reshape(b, s, S5_GROUPS, S5_GROUP_CH).astype(f32)
    dt = jnp.exp(log_dt.astype(f32))[:, None]
    lr, li = lam_re.astype(f32), lam_im.astype(f32)
    mag = jnp.exp(lr * dt)
    ab_re, ab_im = mag * jnp.cos(li * dt), mag * jnp.sin(li * dt)
    den = lr * lr + li * li
    nr, ni = ab_re - 1.0, ab_im
    co_re = (nr * lr + ni * li) / den
    co_im = (ni * lr - nr * li) / den
    br, bi = b_re.astype(f32), b_im.astype(f32)
    bb_re = co_re[..., None] * br - co_im[..., None] * bi
    bb_im = co_re[..., None] * bi + co_im[..., None] * br
    bu_re = jnp.einsum('bsgh,gph->bsgp', ug, bb_re)
    bu_im = jnp.einsum('bsgh,gph->bsgp', ug, bb_im)
    a_re = jnp.broadcast_to(ab_re, bu_re.shape)
    a_im = jnp.broadcast_to(ab_im, bu_re.shape)

    def combine(e1, e2):
        ar1, ai1, xr1, xi1 = e1
        ar2, ai2, xr2, xi2 = e2
        return (ar2 * ar1 - ai2 * ai1, ar2 * ai1 + ai2 * ar1,
                ar2 * xr1 - ai2 * xi1 + xr2, ar2 * xi1 + ai2 * xr1 + xi2)

    _, _, st_re, st_im = lax.associative_scan(combine, (a_re, a_im, bu_re, bu_im), axis=1)
    y = (jnp.einsum('bsgp,ghp->bsgh', st_re, c_re.astype(f32))
         - jnp.einsum('bsgp,ghp->bsgh', st_im, c_im.astype(f32))
         + d_skip.astype(f32).reshape(S5_GROUPS, S5_GROUP_CH) * ug)
    y = jax.nn.gelu(y.reshape(b, s, w)).astype(u.dtype)
    return y * jax.nn.sigmoid(y @ glu_w + glu_b)


def mixer(h, cos, sin, w_in, conv_w, conv_b, w_a, b_a, w_x, b_x, lam,
          lam_re, lam_im, log_dt, s5b_re, s5b_im, s5c_re, s5c_im, s5_d, glu_w, glu_b,
          mix_gain, w_out):
    b, s, _ = h.shape
    proj = h @ w_in
    q, k, v, xr, gate, u = jnp.split(proj, IN_SPLITS, axis=-1)
    q = apply_rope(q.reshape(b, s, ATT_HEADS, HEAD_DIM), cos, sin)
    k = apply_rope(k.reshape(b, s, ATT_HEADS, HEAD_DIM), cos, sin)
    v = v.reshape(b, s, ATT_HEADS, HEAD_DIM)
    o_att = moba_attention(q, k, v)
    o_lru = rglru_branch(xr, gate, conv_w, conv_b, w_a, b_a, w_x, b_x, lam)
    o_s5 = s5_branch(u, lam_re, lam_im, log_dt, s5b_re, s5b_im, s5c_re, s5c_im, s5_d, glu_w, glu_b)
    o = jnp.concatenate([
        rms_norm(o_att, mix_gain[:ATT_WIDTH]),
        rms_norm(o_lru, mix_gain[ATT_WIDTH:ATT_WIDTH + LRU_WIDTH]),
        rms_norm(o_s5, mix_gain[ATT_WIDTH + LRU_WIDTH:])], axis=-1)
    return o @ w_out


def swiglu(h, wg, wu, wd):
    return (jax.nn.silu(h @ wg) * (h @ wu)) @ wd


def moe_swiglu(h, router_w, wg, wu, wd):
    logits = (h @ router_w).astype(jnp.float32)
    top_v, top_i = lax.top_k(logits, TOP_K)
    probs = jax.nn.softmax(top_v, axis=-1)
    weights = jnp.sum(jax.nn.one_hot(top_i, N_EXPERTS, dtype=jnp.float32) * probs[..., None], axis=-2)
    out = jnp.zeros_like(h)
    for e in range(N_EXPERTS):
        out = out + weights[..., e:e + 1].astype(h.dtype) * swiglu(h, wg[e], wu[e], wd[e])
    return out


def setup_inputs(seed: int = 0) -> dict:
    key = jax.random.key(seed)
    keys = jax.random.split(key, 48)
    counter = [0]
    f32 = jnp.float32
    L = DEPTH

    def nxt():
        kk = keys[counter[0]]
        counter[0] += 1
        return kk

    def nrm(shape, scale):
        return jax.random.normal(nxt(), shape, f32) * scale

    x = nrm((BATCH, SEQ, D_MODEL), 1.0)
    c = nrm((BATCH, D_MODEL), 1.0)
    offs = jax.random.randint(nxt(), (BATCH, 1), 0, 2048, dtype=jnp.int32)
    positions = offs + jnp.arange(SEQ, dtype=jnp.int32)[None, :]
    w_in = nrm((L, D_MODEL, IN_WIDTH), D_MODEL ** -0.5)
    lru_conv_w = nrm((L, CONV_WIDTH, LRU_WIDTH), CONV_WIDTH ** -0.5)
    lru_conv_b = nrm((L, LRU_WIDTH), 0.01)
    lru_w_a = nrm((L, LRU_BLOCKS, LRU_BLOCK_DIM, LRU_BLOCK_DIM), LRU_BLOCK_DIM ** -0.5)
    lru_b_a = nrm((L, LRU_WIDTH), 0.01)
    lru_w_x = nrm((L, LRU_BLOCKS, LRU_BLOCK_DIM, LRU_BLOCK_DIM), LRU_BLOCK_DIM ** -0.5)
    lru_b_x = nrm((L, LRU_WIDTH), 0.01)
    a_c = jax.random.uniform(nxt(), (L, LRU_WIDTH), f32, 0.9, 0.999)
    a0 = a_c ** (1.0 / LRU_C)
    lru_lambda = jnp.log(a0) - jnp.log1p(-a0)
    s5_lambda_re = -0.5 + nrm((L, S5_GROUPS, S5_STATE), 0.01)
    s5_lambda_im = math.pi * jnp.arange(S5_STATE, dtype=f32) + nrm((L, S5_GROUPS, S5_STATE), 0.01)
    s5_log_dt = jax.random.uniform(nxt(), (L, S5_GROUPS), f32, math.log(1e-3), math.log(1e-1))
    s5_b_re = nrm((L, S5_GROUPS, S5_STATE, S5_GROUP_CH), (2.0 * S5_GROUP_CH) ** -0.5)
    s5_b_im = nrm((L, S5_GROUPS, S5_STATE, S5_GROUP_CH), (2.0 * S5_GROUP_CH) ** -0.5)
    s5_c_re = nrm((L, S5_GROUPS, S5_GROUP_CH, S5_STATE), (2.0 * S5_STATE) ** -0.5)
    s5_c_im = nrm((L, S5_GROUPS, S5_GROUP_CH, S5_STATE), (2.0 * S5_STATE) ** -0.5)
    s5_d = nrm((L, S5_WIDTH), 1.0)
    s5_glu_w = nrm((L, S5_WIDTH, S5_WIDTH), S5_WIDTH ** -0.5)
    s5_glu_b = nrm((L, S5_WIDTH), 0.01)
    mix_gain = 1.0 + nrm((L, D_MIX), 0.01)
    w_out = nrm((L, D_MIX, D_MODEL), D_MIX ** -0.5)
    norm1_g = 1.0 + nrm((L, D_MODEL), 0.01)
    norm2_g = 1.0 + nrm((L, D_MODEL), 0.01)
    ada_w = nrm((L, D_MODEL, 6 * D_MODEL), 0.5 * D_MODEL ** -0.5)
    ada_b = nrm((L, 6 * D_MODEL), 0.01)
    ffn_w_gate = nrm((N_DENSE, D_MODEL, D_FF), D_MODEL ** -0.5)
    ffn_w_up = nrm((N_DENSE, D_MODEL, D_FF), D_MODEL ** -0.5)
    ffn_w_down = nrm((N_DENSE, D_FF, D_MODEL), D_FF ** -0.5)
    router_w = nrm((N_MOE, D_MODEL, N_EXPERTS), D_MODEL ** -0.5)
    moe_w_gate = nrm((N_MOE, N_EXPERTS, D_MODEL, D_FF_EXPERT), D_MODEL ** -0.5)
    moe_w_up = nrm((N_MOE, N_EXPERTS, D_MODEL, D_FF_EXPERT), D_MODEL ** -0.5)
    moe_w_down = nrm((N_MOE, N_EXPERTS, D_FF_EXPERT, D_MODEL), D_FF_EXPERT ** -0.5)
    final_g = 1.0 + nrm((D_MODEL,), 0.01)
    return {'x': x, 'c': c, 'positions': positions, 'w_in': w_in,
            'lru_conv_w': lru_conv_w, 'lru_conv_b': lru_conv_b, 'lru_w_a': lru_w_a,
            'lru_b_a': lru_b_a, 'lru_w_x': lru_w_x, 'lru_b_x': lru_b_x, 'lru_lambda': lru_lambda,
            's5_lambda_re': s5_lambda_re, 's5_lambda_im': s5_lambda_im, 's5_log_dt': s5_log_dt,
            's5_b_re': s5_b_re, 's5_b_im': s5_b_im, 's5_c_re': s5_c_re, 's5_c_im': s5_c_im,
            's5_d': s5_d, 's5_glu_w': s5_glu_w, 's5_glu_b': s5_glu_b, 'mix_gain': mix_gain,
            'w_out': w_out, 'norm1_g': norm1_g, 'norm2_g': norm2_g, 'ada_w': ada_w, 'ada_b': ada_b,
            'ffn_w_gate': ffn_w_gate, 'ffn_w_up': ffn_w_up, 'ffn_w_down': ffn_w_down,
            'router_w': router_w, 'moe_w_gate': moe_w_gate, 'moe_w_up': moe_w_up,
            'moe_w_down': moe_w_down, 'final_g': final_g}


def reference(x, c, positions, w_in, lru_conv_w, lru_conv_b, lru_w_a, lru_b_a, lru_w_x, lru_b_x,
              lru_lambda, s5_lambda_re, s5_lambda_im, s5_log_dt, s5_b_re, s5_b_im, s5_c_re, s5_c_im,
              s5_d, s5_glu_w, s5_glu_b, mix_gain, w_out, norm1_g, norm2_g, ada_w, ada_b,
              ffn_w_gate, ffn_w_up, ffn_w_down, router_w, moe_w_gate, moe_w_up, moe_w_down, final_g):
    cos, sin = rope_tables(positions)
    cond = jax.nn.silu(c)
    for l in range(DEPTH):
        mod = (cond @ ada_w[l] + ada_b[l])[:, None, :]
        shift1, scale1, gate1, shift2, scale2, gate2 = jnp.split(mod, 6, axis=-1)
        h = rms_norm(x, norm1_g[l]) * (1.0 + scale1) + shift1
        x = x + gate1 * mixer(h, cos, sin, w_in[l], lru_conv_w[l], lru_conv_b[l], lru_w_a[l],
                              lru_b_a[l], lru_w_x[l], lru_b_x[l], lru_lambda[l],
                              s5_lambda_re[l], s5_lambda_im[l], s5_log_dt[l], s5_b_re[l],
                              s5_b_im[l], s5_c_re[l], s5_c_im[l], s5_d[l], s5_glu_w[l],
                              s5_glu_b[l], mix_gain[l], w_out[l])
        h = rms_norm(x, norm2_g[l]) * (1.0 + scale2) + shift2
        if l % 2 == 0:
            f = swiglu(h, ffn_w_gate[l // 2], ffn_w_up[l // 2], ffn_w_down[l // 2])
        else:
            f = moe_swiglu(h, router_w[l // 2], moe_w_gate[l // 2], moe_w_up[l // 2], moe_w_down[l // 2])
        x = x + gate2 * f
    return rms_norm(x, final_g)
```

```python
import math
from contextlib import ExitStack
import numpy as np
import ml_dtypes
import concourse.bass as bass
import concourse.mybir as mybir
from concourse.bass_utils import run_bass_kernel_spmd

F32 = mybir.dt.float32
BF16 = mybir.dt.bfloat16
I32 = mybir.dt.int32
AF = mybir.ActivationFunctionType
ALU = mybir.AluOpType
AX = mybir.AxisListType

S = 4096
D = 1024
NT = S // 128
NG = S // 512
DEPTH = 2
ATT_W = 384
LRU_W = 384
S5_W = 256
IN_EXT = 2176 + 768
D_FF = 2816
N_EXP = 8
D_FFE = 3584
EPS = 1e-6
MAGIC = 12582912.0
NEGB = -30000.0
DEBUG = False


_CACHE = {}


class Buf:
    __slots__ = ("name", "w", "r")

    def __init__(self, name=""):
        self.name = name
        self.w = None
        self.r = []


class Eng:
    def __init__(self, name):
        self.name = name
        self.ops = []
        self.count = 0
        self.sem = None
        self.waited = {}
        self.dma_sems = []
        self.dma_uses = []
        self.dma_next = 0


class K:
    def __init__(self, nc, n_dma_sems=20):
        self.nc = nc
        self.engs = {n: Eng(n) for n in ("pe", "act", "dve", "pool", "sp")}
        self.sem_objs = {}
        self.n_dma_sems = n_dma_sems
        self.es = ExitStack()

    def setup_sems(self):
        sid = 0
        for n, e in self.engs.items():
            e.sem = sid
            self.sem_objs[sid] = self.es.enter_context(self.nc.semaphore("s_" + n))
            sid += 1
        for n in ("sp", "pool", "act"):
            e = self.engs[n]
            for i in range(self.n_dma_sems):
                self.sem_objs[sid] = self.es.enter_context(self.nc.semaphore(f"d_{n}{i}"))
                e.dma_sems.append(sid)
                e.dma_uses.append(0)
                sid += 1

    def op(self, engname, fn, reads=(), writes=(), dma=False):
        E = self.engs[engname]
        deps = {}

        def add(t):
            cur = deps.get(t[0])
            if cur is None or cur[0] < t[1]:
                deps[t[0]] = (t[1], t[2], t[3])

        for b in reads:
            if b.w is not None:
                add(b.w)
        for b in writes:
            if b.w is not None:
                add(b.w)
            for t in b.r:
                add(t)
        waits = []
        for sem, (val, src, src_dma) in deps.items():
            if engname == "pe" and src == "pe" and not src_dma:
                continue
            if E.waited.get(sem, 0) >= val:
                continue
            E.waited[sem] = val
            waits.append((sem, val))
        if dma:
            i = E.dma_next
            E.dma_next = (i + 1) % len(E.dma_sems)
            sem = E.dma_sems[i]
            prev = 16 * E.dma_uses[i]
            E.dma_uses[i] += 1
            val = prev + 16
            if prev > 0 and E.waited.get(sem, 0) < prev:
                E.waited[sem] = prev
                waits.append((sem, prev))
            inc = 16
        else:
            sem = E.sem
            E.count += 1
            val = E.count
            inc = 1
        tok = (sem, val, engname, dma)
        for b in reads:
            b.r.append(tok)
        for b in writes:
            b.w = tok
            b.r = []
        E.ops.append((waits, fn, sem, inc))
        return tok

    def dma(self, q, out, in_, reads=(), writes=(), **kw):
        return self.op(q, lambda e: e.dma_start(out=out, in_=in_, **kw), reads, writes, dma=True)

    def barrier(self):
        targets = []
        for n, e in self.engs.items():
            if e.count > 0:
                targets.append((e.sem, e.count))
            for s, u in zip(e.dma_sems, e.dma_uses):
                if u > 0:
                    targets.append((s, 16 * u))
        for n, e in self.engs.items():
            waits = []
            for s, v in targets:
                if e.waited.get(s, 0) < v:
                    e.waited[s] = v
                    waits.append((s, v))
            if waits:
                e.ops.append((waits, None, None, 0))

    def emit(self):
        nc = self.nc
        so = self.sem_objs
        with nc.Block() as block:
            def mk(E):
                ops = E.ops
                E.ops = []

                def body(e):
                    for waits, fn, sem, inc in ops:
                        for s, v in waits:
                            e.wait_ge(so[s], v)
                        if fn is not None:
                            fn(e).then_inc(so[sem], inc)
                return body
            block.tensor(mk(self.engs["pe"]))
            block.scalar(mk(self.engs["act"]))
            block.vector(mk(self.engs["dve"]))
            block.gpsimd(mk(self.engs["pool"]))
            block.sync(mk(self.engs["sp"]))


class Ring:
    def __init__(self, tiles):
        self.tiles = tiles
        self.bufs = [Buf() for _ in tiles]
        self.i = 0

    def next(self):
        t, b = self.tiles[self.i], self.bufs[self.i]
        self.i = (self.i + 1) % len(self.tiles)
        return t, b


class Prog:
    def __init__(self, dbg=()):
        self.nc = nc = bass.Bass("TRN2", target_bir_lowering=False)
        self.k = K(nc)
        self.k.setup_sems()
        self.dbg = set(dbg)
        self.dr = {}
        self.db = {}

    def nm(self, name):
        self.cnt = getattr(self, "cnt", 0) + 1
        return f"{name}_{self.cnt}"

    def din(self, name, shape, dt=F32):
        self.dr[name] = self.nc.dram_tensor(name, list(shape), dt, kind="ExternalInput").ap()
        self.db[name] = Buf(name)
        return self.dr[name]

    def dscratch(self, name, shape, dt=F32):
        kind = "ExternalOutput" if name in self.dbg else "Internal"
        self.dr[name] = self.nc.dram_tensor(name, list(shape), dt, kind=kind).ap()
        self.db[name] = Buf(name)
        return self.dr[name]

    def dout(self, name, shape, dt=F32):
        self.dr[name] = self.nc.dram_tensor(name, list(shape), dt, kind="ExternalOutput").ap()
        self.db[name] = Buf(name)
        return self.dr[name]


def build(dbg=(), upto="all"):
    P = Prog(dbg)
    nc, k = P.nc, P.k
    dr, db = P.dr, P.db

    P.din("x", [S, D])
    P.din("cT", [128, 8])
    P.din("pos", [1, S], I32)
    P.din("invt", [128, 1])
    P.din("ident", [128, 128])
    P.din("w_in", [DEPTH, D, IN_EXT])
    P.din("ada_w", [DEPTH, D, 6 * D])
    P.din("ada_bT", [128, DEPTH * 48])
    P.din("ada_b", [DEPTH, 6 * D])
    P.din("g1T", [128, DEPTH * 8])
    P.din("g2T", [128, DEPTH * 8])
    P.dout("y", [S, D])
    for l in range(DEPTH):
        P.dscratch(f"QT{l}", [ATT_W, S], BF16)
        P.dscratch(f"KT{l}", [ATT_W, S], BF16)
        P.dscratch(f"KM{l}", [128, 3 * 16], F32)
        P.dscratch(f"V{l}", [S, 6 * 65], BF16)
        P.dscratch(f"XR{l}", [LRU_W, S], F32)
        P.dscratch(f"GG{l}", [LRU_W, S], F32)
        P.dscratch(f"U{l}", [S5_W, S], F32)
    P.dscratch("MODT", [128, DEPTH * 48], F32)
    P.dscratch("GBC", [128, DEPTH * 2 * D], F32)
    P.din("ohk", [16, S], BF16)
    P.din("identb", [128, 128], BF16)
    P.din("cbias", [128, 4 * 512], BF16)
    P.din("padm", [128, 256])
    P.din("pada", [128, 256])
    P.din("padm32", [128, 512])
    P.din("pada32", [128, 512])
    for l in range(DEPTH):
        P.dscratch(f"OATT{l}", [S, ATT_W], F32)
    P.din("s5p", [128, DEPTH * 48])
    P.din("s5_b1", [128, DEPTH * 16 * 128])
    P.din("s5_b2", [128, DEPTH * 16 * 128])
    P.din("s5_c", [128, DEPTH * 2 * 256])
    P.din("s5_dg", [128, DEPTH * 4])
    P.din("s5_gw", [DEPTH, 256, 256])
    P.din("tvals", [128, S])
    P.din("tbm", [128, 4])
    for l in range(DEPTH):
        P.dscratch(f"OS5{l}", [S5_W, S], F32)
        P.dscratch(f"SS5{l}", [128, NT], F32)
    P.din("w_out", [DEPTH, D, D])
    P.din("gainT", [128, DEPTH * 8])
    P.din("router_w", [128, 64])
    P.din("ffn_wg", [1, D, D_FF]); P.din("ffn_wu", [1, D, D_FF]); P.din("ffn_wd", [1, D_FF, D])
    P.din("moe_wg", [N_EXP * D * 4, 896]); P.din("moe_wu", [N_EXP * D * 4, 896]); P.din("moe_wd", [N_EXP * D_FFE, D])
    P.din("tri", [128, 128]); P.din("sTv", [128, 16]); P.din("base60", [128, 60]); P.din("mult60", [128, 60])
    P.din("final_g", [1, D])
    for l in range(DEPTH):
        P.dscratch(f"XA{l}", [S, D], F32)
        P.dscratch(f"XB{l}", [S, D], F32)
        P.dscratch(f"H2T{l}", [D, S], BF16)
        P.dscratch(f"RW{l}", [128, NT * 18], F32)
        if l == 1:
            P.dscratch("SLOTS", [128, 2 * NT], mybir.dt.uint32)
        P.dscratch(f"HS{l}", [16 * 1024, D], BF16)
        P.dscratch(f"OS{l}", [16 * 1024, D], F32)
    P.din("lru_cw", [128, DEPTH * 12])
    P.din("lru_vec", [128, DEPTH * 12])
    P.din("lru_wa", [128, DEPTH * 3 * 128])
    P.din("lru_wx", [128, DEPTH * 3 * 128])
    for l in range(DEPTH):
        P.dscratch(f"OLRU{l}", [LRU_W, S], F32)
        P.dscratch(f"SSL{l}", [128, NT], F32)

    top = ExitStack()
    sbt = lambda name, shape, dt=F32: top.enter_context(nc.sbuf_tensor(P.nm(name), list(shape), dt))

    ident = sbt("ident_sb", [128, 128]); b_ident = Buf()
    modT = sbt("modT", [128, DEPTH * 48]); b_modT = Buf()
    gs1T = sbt("gs1T", [128, DEPTH * 8]); b_gs1T = Buf()
    gs2T = sbt("gs2T", [128, DEPTH * 8]); b_gs2T = Buf()

    def ACT(out, in_, func, reads, writes, **kw):
        return k.op("act", lambda e: e.activation(out=out, in_=in_, func=func, **kw), reads, writes)

    def TS(out, in0, s1, s2, op0, op1, reads, writes, eng="dve", **kw):
        return k.op(eng, lambda e: e.tensor_scalar(out=out, in0=in0, scalar1=s1, scalar2=s2, op0=op0, op1=op1, **kw), reads, writes)

    def TT(out, in0, in1, op, reads, writes, eng="dve"):
        return k.op(eng, lambda e: e.tensor_tensor(out=out, in0=in0, in1=in1, op=op), reads, writes)

    def STT(out, in0, scalar, in1, op0, op1, reads, writes):
        return k.op("dve", lambda e: e.scalar_tensor_tensor(out=out, in0=in0, scalar=scalar, in1=in1, op0=op0, op1=op1), reads, writes)

    def CP(out, in_, reads, writes, eng="dve"):
        return k.op(eng, lambda e: e.tensor_copy(out=out, in_=in_), reads, writes)

    def MM(out, lhsT, rhs, start, stop, reads, writes):
        return k.op("pe", lambda e: e.matmul(out, lhsT, rhs, start=start, stop=stop), reads, writes)

    def TR(out, in_, idn, reads, writes):
        return k.op("pe", lambda e: e.transpose(out, in_, idn), reads, writes)

    cond = sbt("cond", [128, 8]); b_cond = Buf()
    condB = sbt("condB", [128, 8, 128]); b_condB = Buf()
    abT = sbt("abT", [128, DEPTH * 48]); b_abT = Buf()
    g1T = sbt("g1T_sb", [128, DEPTH * 8]); g2T = sbt("g2T_sb", [128, DEPTH * 8]); b_gT = Buf()

    def ada_layer(l, sb, ps):
        gate_bc = sb("gate_bc", [128, 2, D]); b_gate_bc = Buf()
        wring = Ring([sb(f"adaw{i}", [128, 8, 512]) for i in range(2)])
        psm = ps("psm", [128, 48]); b_psm = Buf()
        psb = Ring([ps(f"psb{i}", [128, 512]) for i in range(2)])
        abb = Ring([sb(f"abb{i}", [128, 512]) for i in range(2)])
        for cb in range(12):
            wt, wb = wring.next()
            k.dma("sp", wt[:], dr["ada_w"][l, :, cb * 512:(cb + 1) * 512].rearrange("(k p) n -> p k n", p=128), writes=[wb])
            for s in range(4):
                j = cb * 4 + s
                for kk in range(8):
                    MM(psm[:, j:j + 1], wt[:, kk, s * 128:(s + 1) * 128], cond[:, kk:kk + 1], kk == 0, kk == 7,
                       [wb, b_cond], [b_psm])
            if cb in (4, 5, 10, 11):
                pt, pb = psb.next()
                for kk in range(8):
                    MM(pt[:], condB[:, kk, :], wt[:, kk, :], kk == 0, kk == 7, [wb, b_condB], [pb])
                at, ab = abb.next()
                k.dma("sp", at[:], dr["ada_b"][l:l + 1, cb * 512:(cb + 1) * 512].partition_broadcast(128), writes=[ab])
                gi = 0 if cb < 6 else 1
                half = cb % 2
                TT(gate_bc[:, gi, half * 512:(half + 1) * 512], pt[:], at[:], ALU.add, [pb, ab], [b_gate_bc])
        TT(modT[:, l * 48:(l + 1) * 48], psm[:], abT[:, l * 48:(l + 1) * 48], ALU.add, [b_psm, b_abT], [b_modT])
        STT(gs1T[:, l * 8:(l + 1) * 8], modT[:, l * 48 + 8:l * 48 + 16], 1.0, g1T[:, l * 8:(l + 1) * 8], ALU.add, ALU.mult,
            [b_modT, b_gT], [b_gs1T])
        STT(gs2T[:, l * 8:(l + 1) * 8], modT[:, l * 48 + 32:l * 48 + 40], 1.0, g2T[:, l * 8:(l + 1) * 8], ALU.add, ALU.mult,
            [b_modT, b_gT], [b_gs2T])
        k.dma("sp", dr["GBC"][:, l * 2 * D:(l + 1) * 2 * D], gate_bc[:].rearrange("p a d -> p (a d)"), reads=[b_gate_bc], writes=[db["GBC"]])

    with ExitStack() as es:
        sb = lambda name, shape, dt=F32: es.enter_context(nc.sbuf_tensor(P.nm(name), list(shape), dt))
        ps = lambda name, shape, dt=F32: es.enter_context(nc.psum_tensor(P.nm(name), list(shape), dt))
        k.dma("sp", ident[:], dr["ident"], writes=[b_ident])
        cT = sb("cT_sb", [128, 8]); b_cT = Buf()
        k.dma("sp", cT[:], dr["cT"], writes=[b_cT])
        ACT(cond[:], cT[:], AF.Silu, [b_cT], [b_cond])
        ones = sb("ones", [128, 128]); b_ones = Buf()
        k.op("dve", lambda e: e.memset(ones[:], 1.0), [], [b_ones])
        for kk in range(8):
            TS(condB[:, kk, :], ones[:], cond[:, kk:kk + 1], None, ALU.mult, ALU.bypass, [b_ones, b_cond], [b_condB])
        k.dma("sp", abT[:], dr["ada_bT"], writes=[b_abT])
        k.dma("sp", g1T[:], dr["g1T"], writes=[b_gT])
        k.dma("sp", g2T[:], dr["g2T"], writes=[b_gT])
        ada_layer(0, sb, ps)
        if upto == "setup":
            ada_layer(1, sb, ps)
            if "MODT" in P.dbg:
                k.dma("sp", dr["MODT"], modT[:], reads=[b_modT], writes=[db["MODT"]])
        k.barrier()
        k.emit()

    if upto == "setup":
        return P

    P.ada_layer = ada_layer
    for l in range(DEPTH):
        phase_in(P, l, dict(ident=(ident, b_ident), modT=(modT, b_modT), gs1T=(gs1T, b_gs1T)),
                 ACT, TS, TT, STT, CP, MM, TR)
        if upto == f"in{l}":
            return P
        phase_att(P, l, ACT, TS, TT, STT, CP, MM, TR)
        if upto == f"att{l}":
            return P
        phase_lru(P, l, ACT, TS, TT, STT, CP, MM, TR)
        if upto == f"lru{l}":
            return P
        phase_s5(P, l, ACT, TS, TT, STT, CP, MM, TR)
        if upto == f"s5{l}":
            return P
        G2 = dict(ident=(ident, b_ident), modT=(modT, b_modT), gs2T=(gs2T, b_gs2T))
        phase_out(P, l, G2, ACT, TS, TT, STT, CP, MM, TR)
        if upto == f"out{l}":
            return P
        if l % 2 == 1:
            phase_moe(P, l, G2, ACT, TS, TT, STT, CP, MM, TR)
        else:
            phase_ffn(P, l, G2, ACT, TS, TT, STT, CP, MM, TR)
        if upto == f"ffn{l}":
            return P
    return P


def phase_in(P, l, G, ACT, TS, TT, STT, CP, MM, TR):
    nc, k, dr, db = P.nc, P.k, P.dr, P.db
    ident, b_ident = G["ident"]
    modT, b_modT = G["modT"]
    gs1T, b_gs1T = G["gs1T"]
    xin = "x" if l == 0 else f"XB{l - 1}"
    with ExitStack() as es:
        sb = lambda name, shape, dt=F32: es.enter_context(nc.sbuf_tensor(P.nm(name), list(shape), dt))
        ps = lambda name, shape, dt=F32: es.enter_context(nc.psum_tensor(P.nm(name), list(shape), dt))
        COS = sb("COS", [128, S]); SINS = sb("SINS", [128, S]); b_rope = Buf()
        if True:
            sb2 = sb
            posi = sb2("posi", [128, 1024], I32); b_posi = Buf()
            posf = sb2("posf", [128, 1024]); b_posf = Buf()
            invt = sb2("invt_sb", [128, 1]); b_invt = Buf()
            k.dma("sp", invt[:], dr["invt"], writes=[b_invt])
            t1 = sb2("rt1", [128, 1024]); b_t1 = Buf()
            t2 = sb2("rt2", [128, 1024]); b_t2 = Buf()
            sgn = sb2("sgn", [128, 1]); b_sgn = Buf()
            k.op("dve", lambda e: e.memset(sgn[:], 1.0), [], [b_sgn])
            k.op("dve", lambda e: e.memset(sgn[0:32, :], -1.0), [b_sgn], [b_sgn])
            k.op("dve", lambda e: e.memset(sgn[64:96, :], -1.0), [b_sgn], [b_sgn])
            for q4 in range(4):
                qs = slice(q4 * 1024, (q4 + 1) * 1024)
                k.dma("sp", posi[:], dr["pos"][:, qs].partition_broadcast(128), writes=[b_posi])
                CP(posf[:], posi[:], [b_posi], [b_posf])
                for which, dst, off in (("sin", SINS, 0.0), ("cos", COS, 0.25)):
                    TS(t1[:], posf[:], invt[:, 0:1], off, ALU.mult, ALU.add, [b_posf, b_invt], [b_t1])
                    TS(t2[:], t1[:], MAGIC, None, ALU.add, ALU.bypass, [b_t1], [b_t2])
                    TS(t2[:], t2[:], MAGIC, None, ALU.subtract, ALU.bypass, [b_t2], [b_t2])
                    TT(t1[:], t1[:], t2[:], ALU.subtract, [b_t1, b_t2], [b_t1])
                    ACT(dst[:, qs], t1[:], AF.Sin, [b_t1], [b_rope], scale=2.0 * math.pi)
            TS(SINS[:], SINS[:], sgn[:, 0:1], None, ALU.mult, ALU.bypass, [b_rope, b_sgn], [b_rope])
        W = sb("Win", [128, 8, IN_EXT], BF16); b_W = Buf()
        for kk in range(8):
            k.dma("pool", W[:, kk, :], dr["w_in"][l, kk * 128:(kk + 1) * 128, :], writes=[b_W], max_dma_last_dim=2048)
        xr_ = Ring([sb(f"xt{i}", [128, D]) for i in range(4)])
        xn_ = Ring([sb(f"xn{i}", [128, D]) for i in range(3)])
        sq = sb("sq", [128, D]); b_sq = Buf()
        st_ = Ring([sb(f"st{i}", [128, 2]) for i in range(4)])
        hT_ = Ring([sb(f"hT{i}", [128, 8, 512], BF16) for i in range(3)])
        psT_ = Ring([ps(f"psT{i}", [128, D]) for i in range(2)])
        pm_ = Ring([ps(f"pm{i}", [128, 512]) for i in range(4)])
        ra_ = Ring([sb(f"ra{i}", [128, 512]) for i in range(2)])
        rb_ = Ring([sb(f"rb{i}", [128, 512]) for i in range(2)])
        qo_ = Ring([sb(f"qo{i}", [128, 512], BF16) for i in range(3)])
        fo_ = Ring([sb(f"fo{i}", [128, 512]) for i in range(3)])
        vo_ = Ring([sb(f"vo{i}", [128, 6, 65], BF16) for i in range(2)])
        for vt in vo_.tiles:
            k.op("pool", lambda e, vt=vt: e.memset(vt[:], 1.0), [], [vo_.bufs[vo_.tiles.index(vt)]])
        km = sb("km", [128, 48]); b_km = Buf()
        shT = modT[:, l * 48 + 0:l * 48 + 8]
        def normT(tg):
            hT, b_hT = hT_.next()
            hts[tg] = (hT, b_hT)
            for i in range(4):
                tt = tg * 4 + i
                xt, b_x = xr_.next()
                k.dma("sp", xt[:], dr[xin][tt * 128:(tt + 1) * 128, :], reads=[db[xin]], writes=[b_x])
                st, b_st = st_.next()
                ACT(sq[:], xt[:], AF.Square, [b_x], [b_sq, b_st], accum_out=st[:, 0:1])
                TS(st[:, 1:2], st[:, 0:1], 1.0 / D, EPS, ALU.mult, ALU.add, [b_st], [b_st])
                ACT(st[:, 1:2], st[:, 1:2], AF.Sqrt, [b_st], [b_st])
                k.op("dve", lambda e, st=st: e.reciprocal(out=st[:, 1:2], in_=st[:, 1:2]), [b_st], [b_st])
                xn, b_xn = xn_.next()
                TS(xn[:], xt[:], st[:, 1:2], None, ALU.mult, ALU.bypass, [b_x, b_st], [b_xn])
                pT, b_pT = psT_.next()
                for kk in range(8):
                    TR(pT[:, kk * 128:(kk + 1) * 128], xn[:, kk * 128:(kk + 1) * 128], ident[:], [b_xn, b_ident], [b_pT])
                for kk in range(8):
                    ACT(hT[:, kk, i * 128:(i + 1) * 128], pT[:, kk * 128:(kk + 1) * 128], AF.Identity, [b_pT, b_gs1T, b_modT], [b_hT],
                        scale=gs1T[:, l * 8 + kk:l * 8 + kk + 1], bias=shT[:, kk:kk + 1])
        def proj(tg):
            hT, b_hT = hts.pop(tg)
            tsl = slice(tg * 512, (tg + 1) * 512)
            for which in range(2):
                for c in range(3):
                    base = which * 768 + c * 128
                    pa, b_pa = pm_.next()
                    pb, b_pb = pm_.next()
                    for kk in range(8):
                        MM(pa[:], W[:, kk, base:base + 128], hT[:, kk, :], kk == 0, kk == 7, [b_W, b_hT], [b_pa])
                    for kk in range(8):
                        MM(pb[:], W[:, kk, base + 384:base + 512], hT[:, kk, :], kk == 0, kk == 7, [b_W, b_hT], [b_pb])
                    ra, b_ra = ra_.next()
                    rb, b_rb = rb_.next()
                    TT(ra[:], pa[:], COS[:, tsl], ALU.mult, [b_pa, b_rope], [b_ra])
                    TT(rb[:], pb[:], SINS[:, tsl], ALU.mult, [b_pb, b_rope], [b_rb])
                    qo, b_qo = qo_.next()
                    TT(qo[:], ra[:], rb[:], ALU.add, [b_ra, b_rb], [b_qo])
                    name = ("QT", "KT")[which] + str(l)
                    k.dma("pool", dr[name][c * 128:(c + 1) * 128, tsl], qo[:], reads=[b_qo], writes=[db[name]])
                    if which == 1:
                        k.op("dve", lambda e, qo=qo, c=c, tg=tg: e.tensor_reduce(
                            out=km[:, c * 16 + 2 * tg:c * 16 + 2 * tg + 2], in_=qo[:].rearrange("p (b n) -> p b n", b=2),
                            axis=AX.X, op=ALU.add), [b_qo], [b_km])
            for i in range(4):
                tt = tg * 4 + i
                pa, b_pa = pm_.next()
                for kk in range(8):
                    MM(pa[:, 0:384], hT[:, kk, i * 128:(i + 1) * 128], W[:, kk, 1536:1920], kk == 0, kk == 7, [b_W, b_hT], [b_pa])
                vo, b_vo = vo_.next()
                ACT(vo[:, :, 0:64], pa[:, 0:384].rearrange("p (h d) -> p h d", h=6), AF.Copy, [b_pa], [b_vo])
                k.dma("pool", dr[f"V{l}"][tt * 128:(tt + 1) * 128, :], vo[:].rearrange("p h d -> p (h d)"), reads=[b_vo], writes=[db[f"V{l}"]])
            for j, (name, base, nch) in enumerate((("XR", 1920, 3), ("GG", 2304, 3), ("U", 2688, 2))):
                for c in range(nch):
                    pa, b_pa = pm_.next()
                    for kk in range(8):
                        MM(pa[:], W[:, kk, base + c * 128:base + (c + 1) * 128], hT[:, kk, :], kk == 0, kk == 7, [b_W, b_hT], [b_pa])
                    fo, b_fo = fo_.next()
                    if name == "GG":
                        gelu_tanh(P, fo, b_fo, pa, b_pa, ra_, rb_, ACT, TS, TT)
                    else:
                        ACT(fo[:], pa[:], AF.Copy, [b_pa], [b_fo])
                    k.dma("pool", dr[f"{name}{l}"][c * 128:(c + 1) * 128, tsl], fo[:], reads=[b_fo], writes=[db[f"{name}{l}"]])
        hts = {}
        normT(0)
        normT(1)
        for tg in range(NG):
            if tg + 2 < NG:
                normT(tg + 2)
            proj(tg)
        k.dma("sp", dr[f"KM{l}"], km[:], reads=[b_km], writes=[db[f"KM{l}"]])
        k.barrier()
        k.emit()


def phase_att(P, l, ACT, TS, TT, STT, CP, MM, TR):
    nc, k, dr, db = P.nc, P.k, P.dr, P.db
    with ExitStack() as es:
        sb = lambda name, shape, dt=F32: es.enter_context(nc.sbuf_tensor(P.nm(name), list(shape), dt))
        ps = lambda name, shape, dt=F32: es.enter_context(nc.psum_tensor(P.nm(name), list(shape), dt))
        identf = sb("identf", [128, 128]); identb = sb("identb", [128, 128], BF16); b_c = Buf()
        cb = sb("cb", [128, 4, 512], BF16)
        padm = sb("padm", [128, 16, 16]); pada = sb("pada", [128, 16, 16])
        k.dma("sp", identf[:], dr["ident"], writes=[b_c])
        k.dma("sp", identb[:], dr["identb"], writes=[b_c])
        k.dma("sp", cb[:].rearrange("p a b -> p (a b)"), dr["cbias"], writes=[b_c])
        k.dma("sp", padm[:].rearrange("p a b -> p (a b)"), dr["padm"], writes=[b_c])
        k.dma("sp", pada[:].rearrange("p a b -> p (a b)"), dr["pada"], writes=[b_c])
        vall = sb("vall", [128, NT, 454], BF16); b_v = Buf()
        k.op("pool", lambda e: e.memset(vall[:, :, 390:454], 0.0), [], [b_v])

        def load_v():
            for q4 in range(4):
                k.dma("sp", vall[:, q4 * 8:(q4 + 1) * 8, 0:390], dr[f"V{l}"][q4 * 1024:(q4 + 1) * 1024, :].rearrange("(t p) d -> p t d", p=128),
                      reads=[db[f"V{l}"]], writes=[b_v])
        kta_ = Ring([sb(f"kta{i}", [128, S], BF16) for i in range(2)])
        qta_ = Ring([sb(f"qta{i}", [128, S], BF16) for i in range(2)])
        for t_, b_ in zip(kta_.tiles, kta_.bufs):
            k.op("pool", lambda e, t_=t_: e.memset(t_[64:128, :], 0.0), [], [b_])
            k.dma("sp", t_[64:80, :], dr["ohk"], writes=[b_])
        for t_, b_ in zip(qta_.tiles, qta_.bufs):
            k.op("pool", lambda e, t_=t_: e.memset(t_[64:128, :], 0.0), [], [b_])
        kmf_ = Ring([sb(f"kmf{i}", [64, 16]) for i in range(2)])
        kmb_ = Ring([sb(f"kmb{i}", [64, 16], BF16) for i in range(2)])
        padm32 = sb("padm32", [128, NT, 16]); pada32 = sb("pada32", [128, NT, 16])
        k.dma("sp", padm32[:].rearrange("p a b -> p (a b)"), dr["padm32"], writes=[b_c])
        k.dma("sp", pada32[:].rearrange("p a b -> p (a b)"), dr["pada32"], writes=[b_c])
        g80A = sb("g80A", [128, NT, 80]); b_g80 = Buf()
        k.op("pool", lambda e: e.memset(g80A[:].rearrange("p a b -> p (a b)"), 0.0), [], [b_g80])
        gsA = sb("gsA", [128, NT, 16]); b_gs = Buf()
        t8A = sb("t8A", [128, NT, 8]); b_t8 = Buf()
        pt_ = Ring([sb(f"pt{i}", [128, 512], BF16) for i in range(4)])
        rc_ = Ring([sb(f"rc{i}", [128, 4]) for i in range(4)])
        ot_ = Ring([sb(f"ot{i}", [128, 512]) for i in range(2)])
        oall = sb("oall", [128, NT, ATT_W]); b_oall = Buf()
        pg_ = Ring([ps(f"pg{i}", [128, 512]) for i in range(2)])
        sp_ = Ring([ps(f"sp{i}", [128, 512]) for i in range(3)])
        po_ = Ring([ps(f"po{i}", [128, 512]) for i in range(2)])
        ptr_ = Ring([ps(f"ptr{i}", [128, 512]) for i in range(1)])
        heads = {}

        def load_and_gate(h):
            kta, b_kta = kta_.next()
            qta, b_qta = qta_.next()
            heads[h] = (kta, b_kta, qta, b_qta)
            k.dma("sp", kta[0:64, :], dr[f"KT{l}"][h * 64:(h + 1) * 64, :], reads=[db[f"KT{l}"]], writes=[b_kta])
            k.dma("sp", qta[0:64, :], dr[f"QT{l}"][h * 64:(h + 1) * 64, :], reads=[db[f"QT{l}"]], writes=[b_qta])
            kmf, b_kmf = kmf_.next()
            kmb, b_kmb = kmb_.next()
            c, hb = h // 2, (h % 2) * 64
            k.dma("sp", kmf[:], dr[f"KM{l}"][hb:hb + 64, c * 16:(c + 1) * 16], reads=[db[f"KM{l}"]], writes=[b_kmf])
            CP(kmb[:], kmf[:], [b_kmf], [b_kmb])
            pg, b_pg = pg_.next()
            for qt in range(NT):
                MM(pg[:, qt * 16:(qt + 1) * 16], qta[0:64, qt * 128:(qt + 1) * 128], kmb[:, :], True, True, [b_qta, b_kmb], [b_pg])
            gflat = gsA[:].rearrange("p a b -> p (a b)")
            TT(gflat, pg[:], padm32[:].rearrange("p a b -> p (a b)"), ALU.mult, [b_pg, b_c], [b_gs])
            TT(gflat, gflat, pada32[:].rearrange("p a b -> p (a b)"), ALU.add, [b_gs, b_c], [b_gs])
            for qt in range(NT):
                k.op("dve", lambda e, qt=qt: e.max(out=t8A[:, qt, :], in_=gsA[:, qt, :]), [b_gs], [b_t8])
            for qt in range(NT):
                TS(g80A[:, qt, 64:80], gsA[:, qt, :], t8A[:, qt, 4:5], -1.0, ALU.is_gt, ALU.add, [b_gs, b_t8], [b_g80])
            for q4 in range(NG):
                pg2, b_pg2 = pg_.next()
                for i in range(4):
                    TR(pg2[0:80, i * 128:(i + 1) * 128], g80A[:, q4 * 4 + i, :], identf[:], [b_g80, b_c], [b_pg2])
                ACT(qta[64:80, q4 * 512:(q4 + 1) * 512], pg2[64:80, :], AF.Copy, [b_pg2], [b_qta], scale=-NEGB)

        def attend(h):
            kta, b_kta, qta, b_qta = heads[h]
            steps = [(g, kt) for g in range(NG) for kt in range(4 * g + 4)]
            spd = {}
            posd = {}

            def score(i):
                g, kt = steps[i]
                sp, b_sp = sp_.next()
                diag = kt >= 4 * g
                MM(sp[:], kta[:, kt * 128:(kt + 1) * 128], qta[:, g * 512:(g + 1) * 512], True, not diag,
                   [b_kta, b_qta], [b_sp])
                if diag:
                    MM(sp[:], identb[:], cb[:, kt - 4 * g, :], False, True, [b_c], [b_sp])
                spd[i] = (sp, b_sp)

            def rest(i):
                g, kt = steps[i]
                if kt == 0:
                    posd[g] = po_.next()
                po, b_po = posd[g]
                sp, b_sp = spd.pop(i)
                pt, b_pt = pt_.next()
                ACT(pt[:], sp[:], AF.Exp, [b_sp], [b_pt], scale=0.125)
                MM(po[:, :], vall[:, kt, h * 65:h * 65 + 128], pt[:], kt == 0, kt == 4 * g + 3, [b_pt, b_v], [b_po])
                if kt == 4 * g + 3:
                    fins.append((i + 2, (lambda g=g, po=po, b_po=b_po: finalize(g, po, b_po))))

            def finalize(g, po, b_po):
                ot, b_ot = ot_.next()
                CP(ot[0:65, :], po[0:65, :], [b_po], [b_ot])
                ptr, b_ptr = ptr_.next()
                for qg in range(4):
                    TR(ptr[:, qg * 65:(qg + 1) * 65], ot[0:65, qg * 128:(qg + 1) * 128], identf[0:65, 0:65], [b_ot, b_c], [b_ptr])
                rc, b_rc = rc_.next()
                pv = ptr[:, 0:260].rearrange("p (a d) -> p a d", a=4)
                k.op("dve", lambda e, rc=rc, pv=pv: e.reciprocal(out=rc[:, 0:4], in_=pv[:, :, 64]), [b_ptr], [b_rc])
                for qg in range(4):
                    qt = 4 * g + qg
                    TS(oall[:, qt, h * 64:(h + 1) * 64], ptr[:, qg * 65:qg * 65 + 64], rc[:, qg:qg + 1], None, ALU.mult, ALU.bypass,
                       [b_ptr, b_rc], [b_oall])

            fins = []
            score(0)
            score(1)
            for i in range(len(steps)):
                if i + 2 < len(steps):
                    score(i + 2)
                rest(i)
                while fins and fins[0][0] <= i:
                    fins.pop(0)[1]()
            while fins:
                fins.pop(0)[1]()

        load_and_gate(0)
        load_v()
        for h in range(6):
            if h + 1 < 6:
                load_and_gate(h + 1)
            attend(h)
        for q4 in range(4):
            k.dma("sp", dr[f"OATT{l}"][q4 * 1024:(q4 + 1) * 1024, :].rearrange("(t p) d -> p t d", p=128), oall[:, q4 * 8:(q4 + 1) * 8, :],
                  reads=[b_oall], writes=[db[f"OATT{l}"]])
        k.barrier()
        k.emit()


def phase_lru(P, l, ACT, TS, TT, STT, CP, MM, TR):
    nc, k, dr, db = P.nc, P.k, P.dr, P.db
    with ExitStack() as es:
        sb = lambda name, shape, dt=F32: es.enter_context(nc.sbuf_tensor(P.nm(name), list(shape), dt))
        ps = lambda name, shape, dt=F32: es.enter_context(nc.psum_tensor(P.nm(name), list(shape), dt))
        cw = sb("cw", [128, DEPTH * 12]); vec = sb("lvec", [128, DEPTH * 12]); b_par = Buf()
        wa = sb("wa", [128, DEPTH * 3 * 128]); wx = sb("wx", [128, DEPTH * 3 * 128])
        k.dma("sp", cw[:], dr["lru_cw"], writes=[b_par])
        k.dma("sp", vec[:], dr["lru_vec"], writes=[b_par])
        k.dma("sp", wa[:], dr["lru_wa"], writes=[b_par])
        k.dma("sp", wx[:], dr["lru_wx"], writes=[b_par])
        ones = sb("ones1", [128, 1]); b_ones = Buf()
        k.op("dve", lambda e: e.memset(ones[:], 1.0), [], [b_ones])
        cc = sb("cc", [128, 6]); b_cc = Buf()
        xrp = sb("xrp", [128, S + 4]); b_xrp = Buf()
        xc = sb("xc", [128, S]); b_xc = Buf()
        rr = sb("rr", [128, S]); b_rr = Buf()
        ii = sb("ii", [128, S]); b_ii = Buf()
        a2 = sb("a2", [128, S]); b_a2 = Buf()
        gg = sb("gg", [128, S]); b_gg = Buf()
        ss = sb("ss", [128, NT]); b_ss = Buf()
        pm_ = Ring([ps(f"pm{i}", [128, 512]) for i in range(4)])
        pss = ps("pss", [128, NT]); b_pss = Buf()
        if l == 0:
            P.ada_layer(1, sb, ps)
        k.op("dve", lambda e: e.memset(xrp[:, 0:4], 0.0), [], [b_xrp])
        for c in range(3):
            vb = l * 12 + c * 4
            ACT(cc[:, 2 * c:2 * c + 1], vec[:, vb + 3:vb + 4], AF.Exp, [b_par], [b_cc], scale=-1.0)
            ACT(cc[:, 2 * c:2 * c + 1], cc[:, 2 * c:2 * c + 1], AF.Ln, [b_cc], [b_cc], bias=1.0)
            TS(cc[:, 2 * c + 1:2 * c + 2], cc[:, 2 * c:2 * c + 1], -16.0, None, ALU.mult, ALU.bypass, [b_cc], [b_cc])
            TS(cc[:, 2 * c:2 * c + 1], cc[:, 2 * c:2 * c + 1], -8.0, None, ALU.mult, ALU.bypass, [b_cc], [b_cc])
            k.dma("sp", xrp[:, 4:S + 4], dr[f"XR{l}"][c * 128:(c + 1) * 128, :], reads=[db[f"XR{l}"]], writes=[b_xrp])
            k.dma("sp", gg[:], dr[f"GG{l}"][c * 128:(c + 1) * 128, :], reads=[db[f"GG{l}"]], writes=[b_gg])
            wb = l * 12 + c * 4
            TS(xc[:], xrp[:, 1:S + 1], cw[:, wb:wb + 1], vec[:, vb:vb + 1], ALU.mult, ALU.add, [b_xrp, b_par], [b_xc])
            for j in range(1, 4):
                STT(xc[:], xrp[:, 1 + j:S + 1 + j], cw[:, wb + j:wb + j + 1], xc[:], ALU.mult, ALU.add, [b_xrp, b_par, b_xc], [b_xc])
            wof = (l * 3 + c) * 128
            for tg in range(NG):
                tsl = slice(tg * 512, (tg + 1) * 512)
                pa, b_pa = pm_.next()
                MM(pa[:], wa[:, wof:wof + 128], xc[:, tsl], True, True, [b_par, b_xc], [b_pa])
                ACT(rr[:, tsl], pa[:], AF.Sigmoid, [b_pa, b_par], [b_rr], bias=vec[:, vb + 1:vb + 2])
                pb, b_pb = pm_.next()
                MM(pb[:], wx[:, wof:wof + 128], xc[:, tsl], True, True, [b_par, b_xc], [b_pb])
                ACT(ii[:, tsl], pb[:], AF.Sigmoid, [b_pb, b_par], [b_ii], bias=vec[:, vb + 2:vb + 3])
            ACT(a2[:], rr[:], AF.Exp, [b_rr, b_cc], [b_a2], scale=cc[:, 2 * c + 1:2 * c + 2])
            ACT(rr[:], rr[:], AF.Exp, [b_rr, b_cc], [b_rr], scale=cc[:, 2 * c:2 * c + 1])
            TS(a2[:], a2[:], 1.0, 0.0, ALU.subtract, ALU.min, [b_a2], [b_a2])
            ACT(a2[:], a2[:], AF.Sqrt, [b_a2], [b_a2], scale=-1.0)
            TT(ii[:], ii[:], xc[:], ALU.mult, [b_ii, b_xc], [b_ii])
            TT(ii[:], ii[:], a2[:], ALU.mult, [b_ii, b_a2], [b_ii])
            k.op("dve", lambda e: e.tensor_tensor_scan(out=xc[:], data0=rr[:], data1=ii[:], initial=0.0, op0=ALU.mult, op1=ALU.add),
                 [b_rr, b_ii], [b_xc])
            TT(gg[:], gg[:], xc[:], ALU.mult, [b_gg, b_xc], [b_gg])
            k.dma("sp", dr[f"OLRU{l}"][c * 128:(c + 1) * 128, :], gg[:], reads=[b_gg], writes=[db[f"OLRU{l}"]])
            ACT(a2[:], gg[:], AF.Square, [b_gg], [b_a2])
            for tt in range(NT):
                MM(pss[:, tt:tt + 1], a2[:, tt * 128:(tt + 1) * 128], ones[:, 0:1], True, True, [b_a2, b_ones], [b_pss])
            if c == 0:
                CP(ss[:], pss[:], [b_pss], [b_ss])
            else:
                TT(ss[:], ss[:], pss[:], ALU.add, [b_ss, b_pss], [b_ss])
        k.dma("sp", dr[f"SSL{l}"], ss[:], reads=[b_ss], writes=[db[f"SSL{l}"]])
        k.barrier()
        k.emit()


def phase_s5(P, l, ACT, TS, TT, STT, CP, MM, TR):
    nc, k, dr, db = P.nc, P.k, P.dr, P.db
    TWO_PI = 2.0 * math.pi
    with ExitStack() as es:
        sb = lambda name, shape, dt=F32: es.enter_context(nc.sbuf_tensor(P.nm(name), list(shape), dt))
        ps = lambda name, shape, dt=F32: es.enter_context(nc.psum_tensor(P.nm(name), list(shape), dt))
        b_par = Buf()
        s5p = sb("s5p", [128, 3, 16]); k.dma("sp", s5p[:].rearrange("p a g -> p (a g)"), dr["s5p"][:, l * 48:(l + 1) * 48], writes=[b_par])
        cc = sb("s5c", [128, 2, 16, 16]); k.dma("sp", cc[:].rearrange("p a g h -> p (a g h)"), dr["s5_c"][:, l * 512:(l + 1) * 512], writes=[b_par])
        dg = sb("s5dg", [128, 4]); k.dma("sp", dg[:], dr["s5_dg"][:, l * 4:(l + 1) * 4], writes=[b_par])
        tbm = sb("tbm", [128, 4]); k.dma("sp", tbm[:], dr["tbm"], writes=[b_par])
        tv = sb("tv", [128, S]); k.dma("sp", tv[:], dr["tvals"], writes=[b_par])
        B1 = sb("B1", [128, 16, 128], BF16); B2 = sb("B2", [128, 16, 128], BF16); b_B = Buf()
        k.dma("pool", B1[:].rearrange("p g m -> p (g m)"), dr["s5_b1"][:, l * 2048:(l + 1) * 2048], writes=[b_B])
        k.dma("pool", B2[:].rearrange("p g m -> p (g m)"), dr["s5_b2"][:, l * 2048:(l + 1) * 2048], writes=[b_B])
        TS(B2[:, :, 64:128], B2[:, :, 64:128], -1.0, None, ALU.mult, ALU.bypass, [b_B], [b_B])
        gw = sb("gw", [128, 2, 256], BF16); b_gw = Buf()
        k.dma("pool", gw[:], dr["s5_gw"][l].rearrange("(c p) n -> p c n", p=128), writes=[b_gw])
        ub = sb("ub", [128, 2, S], BF16); b_u = Buf()
        u32_ = Ring([sb(f"u32{i}", [128, 2, 512]) for i in range(2)])
        for ch in range(2):
            k.dma("pool", ub[:, ch, :], dr[f"U{l}"][ch * 128:(ch + 1) * 128, :], reads=[db[f"U{l}"]], writes=[b_u], max_dma_last_dim=2048)
        sm = sb("sm", [128, 16, 16]); b_sm = Buf()
        R = lambda i: sm[:, i, :]
        lr, li, ldt = s5p[:, 0, :], s5p[:, 1, :], s5p[:, 2, :]
        DT, MAG, TH, UT, F1, SN, CS, DEN, NR, CORE, COIM, NCOIM, TMP, TMP2 = (R(i) for i in range(14))
        bs = [b_sm, b_par]
        ACT(DT, ldt, AF.Exp, bs, [b_sm])
        TT(TMP, lr, DT, ALU.mult, bs, [b_sm])
        ACT(MAG, TMP, AF.Exp, bs, [b_sm])
        TT(TH, li, DT, ALU.mult, bs, [b_sm])
        TS(UT, TH, 1.0 / TWO_PI, None, ALU.mult, ALU.bypass, bs, [b_sm])
        for dst, off in ((SN, 0.0), (CS, 0.25)):
            TS(F1, UT, off, None, ALU.add, ALU.bypass, bs, [b_sm])
            TS(TMP, F1, MAGIC, None, ALU.add, ALU.bypass, bs, [b_sm])
            TS(TMP, TMP, MAGIC, None, ALU.subtract, ALU.bypass, bs, [b_sm])
            TT(F1, F1, TMP, ALU.subtract, bs, [b_sm])
            ACT(dst, F1, AF.Sin, bs, [b_sm], scale=TWO_PI)
        TT(SN, SN, MAG, ALU.mult, bs, [b_sm])
        TT(CS, CS, MAG, ALU.mult, bs, [b_sm])
        TS(NR, CS, -1.0, None, ALU.add, ALU.bypass, bs, [b_sm])
        TT(DEN, lr, lr, ALU.mult, bs, [b_sm])
        TT(TMP, li, li, ALU.mult, bs, [b_sm])
        TT(DEN, DEN, TMP, ALU.add, bs, [b_sm])
        k.op("dve", lambda e: e.reciprocal(out=DEN, in_=DEN), bs, [b_sm])
        TT(TMP, NR, lr, ALU.mult, bs, [b_sm])
        TT(TMP2, SN, li, ALU.mult, bs, [b_sm])
        TT(TMP, TMP, TMP2, ALU.add, bs, [b_sm])
        TT(CORE, TMP, DEN, ALU.mult, bs, [b_sm])
        TT(TMP, SN, lr, ALU.mult, bs, [b_sm])
        TT(TMP2, NR, li, ALU.mult, bs, [b_sm])
        TT(TMP, TMP, TMP2, ALU.subtract, bs, [b_sm])
        TT(COIM, TMP, DEN, ALU.mult, bs, [b_sm])
        TS(NCOIM, COIM, -1.0, None, ALU.mult, ALU.bypass, bs, [b_sm])
        Lp = sb("Lp", [128, 16, 2, 128], BF16); b_Lp = Buf()
        k.op("pool", lambda e: e.memset(Lp[:].rearrange("p g a m -> p (g a m)"), 0.0), [], [b_Lp])
        cw_ = sb("cw_", [128, 16, 4, 16]); b_cwg = [Buf() for _ in range(16)]
        b_Lpg = [Buf() for _ in range(16)]
        for g in range(16):
            b_Lpg[g].w = b_Lp.w
        gsl = lambda g: slice(16 * (g % 8), 16 * (g % 8) + 16)
        G16 = range(16)
        for g in G16:
            TS(cw_[:, g, 0, :], cc[:, 0, g, :], CORE[:, g:g + 1], None, ALU.mult, ALU.bypass, bs, [b_cwg[g]])
        for g in G16:
            TS(cw_[:, g, 1, :], cc[:, 0, g, :], COIM[:, g:g + 1], None, ALU.mult, ALU.bypass, bs, [b_cwg[g]])
        for g in G16:
            STT(cw_[:, g, 2, :], cc[:, 1, g, :], NCOIM[:, g:g + 1], cw_[:, g, 0, :], ALU.mult, ALU.add, bs + [b_cwg[g]], [b_cwg[g]])
        for g in G16:
            STT(cw_[:, g, 3, :], cc[:, 1, g, :], CORE[:, g:g + 1], cw_[:, g, 1, :], ALU.mult, ALU.add, bs + [b_cwg[g]], [b_cwg[g]])
        for g in G16:
            TS(cw_[:, g, 0, :], cw_[:, g, 2, :], tbm[:, 0:1], None, ALU.mult, ALU.bypass, bs + [b_cwg[g]], [b_cwg[g]])
        for g in G16:
            TS(cw_[:, g, 1, :], cw_[:, g, 3, :], tbm[:, 1:2], None, ALU.mult, ALU.bypass, bs + [b_cwg[g]], [b_cwg[g]])
        for g in G16:
            STT(Lp[:, g, 0, gsl(g)], cw_[:, g, 3, :], tbm[:, 3:4], cw_[:, g, 0, :], ALU.mult, ALU.add, bs + [b_cwg[g]], [b_Lpg[g]])
        for g in G16:
            STT(Lp[:, g, 1, gsl(g)], cw_[:, g, 2, :], tbm[:, 3:4], cw_[:, g, 1, :], ALU.mult, ALU.add, bs + [b_cwg[g]], [b_Lpg[g]])
        ones = sb("ones5", [128, 512]); b_ones = Buf()
        k.op("pool", lambda e: e.memset(ones[:], 1.0), [], [b_ones])
        wl = sb("wl", [128, 16]); b_wl = [Buf() for _ in range(16)]
        RD = 4
        y1_ = Ring([sb(f"y1{i}", [128, 512]) for i in range(RD)])
        y2_ = Ring([sb(f"y2{i}", [128, 512]) for i in range(RD)])
        gsr_ = Ring([sb(f"gsr{i}", [128, 512]) for i in range(RD)])
        ab_ = Ring([sb(f"ab{i}", [128, 512]) for i in range(RD)])
        sn_ = Ring([sb(f"sn{i}", [128, 512]) for i in range(RD)])
        cs_ = Ring([sb(f"cs{i}", [128, 512]) for i in range(RD)])
        mt_ = Ring([sb(f"mt{i}", [128, 512]) for i in range(RD)])
        ta_ = Ring([sb(f"ta{i}", [128, 512]) for i in range(RD)])
        tb_ = Ring([sb(f"tb{i}", [128, 512]) for i in range(RD)])
        w_ = Ring([sb(f"w{i}", [128, 512]) for i in range(RD)])
        z1_ = Ring([sb(f"z1{i}", [128, 512], BF16) for i in range(RD)])
        z2_ = Ring([sb(f"z2{i}", [128, 512], BF16) for i in range(RD)])
        yv_ = Ring([sb(f"yv{i}", [128, 2, 512]) for i in range(2)])
        ygb_ = Ring([sb(f"ygb{i}", [128, 2, 512], BF16) for i in range(2)])
        og_ = Ring([sb(f"og{i}", [128, 512]) for i in range(2)])
        ra_ = Ring([sb(f"gra{i}", [128, 512]) for i in range(2)])
        rb_ = Ring([sb(f"grb{i}", [128, 512]) for i in range(2)])
        ones1 = sb("ones51", [128, 1]); b_o1 = Buf()
        k.op("pool", lambda e: e.memset(ones1[:], 1.0), [], [b_o1])
        ss = sb("ss5", [128, NT]); b_ss = Buf()
        ps1_ = Ring([ps(f"ps1{i}", [128, 512]) for i in range(2)])
        ps2_ = Ring([ps(f"ps2{i}", [128, 512]) for i in range(2)])
        py = [ps(f"py{i}", [128, 512]) for i in range(2)]; b_py = [Buf(), Buf()]
        pz_ = Ring([ps(f"pz{i}", [128, 512]) for i in range(1)])
        pss = ps("pss5", [128, 512]); b_pss = Buf()
        pend = {}
        tabs = {}

        def bu(cc_, g_):
            p1_, b_p1_ = ps1_.next()
            p2_, b_p2_ = ps2_.next()
            sl_ = slice(cc_ * 512, (cc_ + 1) * 512)
            MM(p1_[:], B1[:, g_, :], ub[:, g_ // 8, sl_], True, True, [b_B, b_u], [b_p1_])
            MM(p2_[:], B2[:, g_, :], ub[:, g_ // 8, sl_], True, True, [b_B, b_u], [b_p2_])
            pend[(cc_, g_)] = (p1_, b_p1_, p2_, b_p2_)

        def tables(cc_, g_):
            sl_ = slice(cc_ * 512, (cc_ + 1) * 512)
            ug = UT[:, g_:g_ + 1]
            y1, b_y1 = y1_.next()
            y2, b_y2 = y2_.next()
            gsr, b_gsr = gsr_.next()
            TS(y1[:], tv[:, sl_], ug, MAGIC, ALU.mult, ALU.add, [b_par, b_sm], [b_y1], eng="pool")
            TS(y2[:], tv[:, sl_], ug, 0.0, ALU.mult, ALU.add, [b_par, b_sm], [b_y2], eng="pool")
            TS(y1[:], y1[:], -MAGIC, 1.0, ALU.add, ALU.mult, [b_y1], [b_y1], eng="pool")
            TT(gsr[:], y2[:], y1[:], ALU.subtract, [b_y1, b_y2], [b_gsr], eng="pool")
            sn, b_sn = sn_.next()
            cs, b_cs = cs_.next()
            ab, b_ab = ab_.next()
            mt, b_mt = mt_.next()
            ACT(ab[:], gsr[:], AF.Abs, [b_gsr], [b_ab])
            ACT(sn[:], gsr[:], AF.Sin, [b_gsr], [b_sn], scale=TWO_PI)
            ACT(mt[:], ones[:], AF.Identity, [b_ones, b_sm], [b_mt], scale=0.0, bias=MAG[:, g_:g_ + 1])
            ACT(cs[:], ab[:], AF.Sin, [b_ab, b_par], [b_cs], scale=-TWO_PI, bias=tbm[:, 2:3])
            tabs[(cc_, g_)] = (sn, b_sn, cs, b_cs, mt, b_mt)

        order = [(c_, g_) for c_ in range(NG) for g_ in range(16)]
        LA = 2
        for i_ in range(LA):
            tables(*order[i_])
        bu(*order[0])
        bu(*order[1])
        pend_tail = []
        for c in range(NG):
            tsl = slice(c * 512, (c + 1) * 512)
            yv, b_yv = yv_.next()
            ygb, b_ygb = ygb_.next()
            u32, b_u32 = u32_.next()
            k.dma("sp", u32[:], dr[f"U{l}"][:, tsl].rearrange("(c p) n -> p c n", p=128), reads=[db[f"U{l}"]], writes=[b_u32])
            for gp in range(0, 16, 2):
                ch = gp // 8
                st_ = []
                for g in (gp, gp + 1):
                    idx = c * 16 + g
                    if idx + LA < len(order):
                        tables(*order[idx + LA])
                    p1, b_p1, p2, b_p2 = pend.pop((c, g))
                    sn, b_sn, cs, b_cs, mt, b_mt = tabs.pop((c, g))
                    ta, b_ta = ta_.next()
                    tb, b_tb = tb_.next()
                    w, b_w = w_.next()
                    z1, b_z1 = z1_.next()
                    z2, b_z2 = z2_.next()
                    st_.append(dict(g=g, p1=p1, b_p1=b_p1, p2=p2, b_p2=b_p2, sn=sn, b_sn=b_sn, cs=cs, b_cs=b_cs, mt=mt, b_mt=b_mt,
                                    ta=ta, b_ta=b_ta, tb=tb, b_tb=b_tb, w=w, b_w=b_w, z1=z1, b_z1=b_z1, z2=z2, b_z2=b_z2))
                for s_ in st_:
                    TT(s_["ta"][:], s_["p1"][:], s_["cs"][:], ALU.mult, [s_["b_p1"], s_["b_cs"]], [s_["b_ta"]])
                for s_ in st_:
                    TT(s_["tb"][:], s_["p2"][:], s_["sn"][:], ALU.mult, [s_["b_p2"], s_["b_sn"]], [s_["b_tb"]])
                for s_ in st_:
                    TT(s_["ta"][:], s_["ta"][:], s_["tb"][:], ALU.add, [s_["b_ta"], s_["b_tb"]], [s_["b_ta"]])
                for dg_ in (2, 3):
                    if c * 16 + gp + dg_ < len(order):
                        bu(*order[c * 16 + gp + dg_])
                for s_ in st_:
                    g = s_["g"]
                    init = 0.0 if c == 0 else wl[:, g:g + 1]
                    k.op("dve", lambda e, s_=s_, init=init: e.tensor_tensor_scan(
                        out=s_["w"][:], data0=s_["mt"][:], data1=s_["ta"][:], initial=init, op0=ALU.mult, op1=ALU.add),
                        [s_["b_mt"], s_["b_ta"], b_wl[g]], [s_["b_w"]])
                for s_ in st_:
                    g = s_["g"]
                    ACT(wl[:, g:g + 1], s_["w"][:, 511:512], AF.Copy, [s_["b_w"]], [b_wl[g]])
                    TT(s_["z1"][:], s_["w"][:], s_["cs"][:], ALU.mult, [s_["b_w"], s_["b_cs"]], [s_["b_z1"]])
                for s_ in st_:
                    TT(s_["z2"][:], s_["w"][:], s_["sn"][:], ALU.mult, [s_["b_w"], s_["b_sn"]], [s_["b_z2"]])
                for s_ in st_:
                    g = s_["g"]
                    MM(py[ch][:], Lp[:, g, 0, :], s_["z1"][:], g % 8 == 0, False, [b_Lpg[g], s_["b_z1"]], [b_py[ch]])
                    MM(py[ch][:], Lp[:, g, 1, :], s_["z2"][:], False, g % 8 == 7, [b_Lpg[g], s_["b_z2"]], [b_py[ch]])
                    if g % 8 == 7:
                        STT(yv[:, ch, :], u32[:, ch, :], dg[:, ch:ch + 1], py[ch][:], ALU.mult, ALU.add, [b_u32, b_par, b_py[ch]], [b_yv])
                if gp == 4 and pend_tail:
                    pend_tail.pop(0)()
            def tail(c, yv, b_yv, ygb, b_ygb, tsl):
                for ch in range(2):
                    class _V:
                        def __init__(s_, ap): s_.ap = ap
                        def __getitem__(s_, key): return s_.ap
                    yview = _V(yv[:, ch, :])
                    gelu_tanh(P, yview, b_yv, yview, b_yv, ra_, rb_, ACT, TS, TT)
                    CP(ygb[:, ch, :], yv[:, ch, :], [b_yv], [b_ygb], eng="pool")
                for oc in range(2):
                    pz, b_pz = pz_.next()
                    for kc in range(2):
                        MM(pz[:], gw[:, kc, oc * 128:(oc + 1) * 128], ygb[:, kc, :], kc == 0, kc == 1, [b_gw, b_ygb], [b_pz])
                    og, b_og = og_.next()
                    ACT(og[:], pz[:], AF.Sigmoid, [b_pz, b_par], [b_og], bias=dg[:, 2 + oc:3 + oc])
                    TT(og[:], og[:], yv[:, oc, :], ALU.mult, [b_og, b_yv], [b_og])
                    k.dma("sp", dr[f"OS5{l}"][oc * 128:(oc + 1) * 128, tsl], og[:], reads=[b_og], writes=[db[f"OS5{l}"]])
                    sq, b_sq = ra_.next()
                    ACT(sq[:], og[:], AF.Square, [b_og], [b_sq])
                    for i in range(4):
                        MM(pss[:, oc * 4 + i:oc * 4 + i + 1], sq[:, i * 128:(i + 1) * 128], ones1[:, 0:1], True, True, [b_sq, b_o1], [b_pss])
                CP(ss[:, c * 4:(c + 1) * 4], pss[:, 0:4], [b_pss], [b_ss])
                TT(ss[:, c * 4:(c + 1) * 4], ss[:, c * 4:(c + 1) * 4], pss[:, 4:8], ALU.add, [b_pss, b_ss], [b_ss])
            pend_tail.append(lambda c=c, yv=yv, b_yv=b_yv, ygb=ygb, b_ygb=b_ygb, tsl=tsl, tail=tail: tail(c, yv, b_yv, ygb, b_ygb, tsl))
        while pend_tail:
            pend_tail.pop(0)()
        k.dma("sp", dr[f"SS5{l}"], ss[:], reads=[b_ss], writes=[db[f"SS5{l}"]])
        k.barrier()
        k.emit()


def phase_out(P, l, G, ACT, TS, TT, STT, CP, MM, TR):
    nc, k, dr, db = P.nc, P.k, P.dr, P.db
    ident, b_ident = G["ident"]
    modT, b_modT = G["modT"]
    gs2T, b_gs2T = G["gs2T"]
    xin = "x" if l == 0 else f"XB{l - 1}"
    moe = (l % 2 == 1)
    with ExitStack() as es:
        sb = lambda name, shape, dt=F32: es.enter_context(nc.sbuf_tensor(P.nm(name), list(shape), dt))
        ps = lambda name, shape, dt=F32: es.enter_context(nc.psum_tensor(P.nm(name), list(shape), dt))
        gbc = sb("gbc", [128, D]); b_gbc = Buf()
        k.dma("sp", gbc[:], dr["GBC"][:, (l * 2) * D:(l * 2 + 1) * D], reads=[db["GBC"]], writes=[b_gbc])
        Wo = sb("Wo", [128, 8, D], BF16); b_W = Buf()
        for kk in range(8):
            k.dma("pool", Wo[:, kk, :], dr["w_out"][l, kk * 128:(kk + 1) * 128, :], writes=[b_W], max_dma_last_dim=2048)
        b_par = Buf()
        gT = sb("gT", [128, 8]); k.dma("sp", gT[:], dr["gainT"][:, l * 8:(l + 1) * 8], writes=[b_par])
        ssl = sb("ssl", [128, NT]); k.dma("sp", ssl[:], dr[f"SSL{l}"], reads=[db[f"SSL{l}"]], writes=[b_par])
        ss5 = sb("ss5o", [128, NT]); k.dma("sp", ss5[:], dr[f"SS5{l}"], reads=[db[f"SS5{l}"]], writes=[b_par])
        rw32 = sb("rw32", [128, 8, 8]); k.dma("sp", rw32[:].rearrange("p a b -> p (a b)"), dr["router_w"], writes=[b_par])
        rwo = sb("rwo", [128, NT, 18]); b_rwo = Buf()
        oa_ = Ring([sb(f"oa{i}", [128, ATT_W]) for i in range(4)])
        of_ = Ring([sb(f"of{i}", [128, 5, 128]) for i in range(4)])
        oT_ = Ring([sb(f"oT{i}", [128, 8, 128], BF16) for i in range(4)])
        xt_ = Ring([sb(f"xto{i}", [128, D]) for i in range(4)])
        xn_ = Ring([sb(f"xno{i}", [128, D]) for i in range(3)])
        tm_ = Ring([sb(f"tmo{i}", [128, 512]) for i in range(4)])
        xw_ = Ring([sb(f"xwo{i}", [128, D]) for i in range(4)])
        sq = sb("sqo", [128, D]); b_sq = Buf()
        st_ = Ring([sb(f"sto{i}", [128, 8]) for i in range(6)])
        h2_ = Ring([sb(f"h2o{i}", [128, 8, 128], BF16) for i in range(3)])
        h32_ = Ring([sb(f"h32o{i}", [128, 8, 128]) for i in range(3)])
        lg_ = Ring([sb(f"lgo{i}", [128, 8]) for i in range(2)])
        t8_ = Ring([sb(f"t8o{i}", [128, 8]) for i in range(2)])
        wv_ = Ring([sb(f"wvo{i}", [128, 4]) for i in range(2)])
        m1_ = Ring([sb(f"m1o{i}", [128, 8]) for i in range(2)])
        py_ = Ring([ps(f"pyo{i}", [128, 512]) for i in range(4)])
        pta_ = Ring([ps(f"ptao{i}", [128, 512]) for i in range(1)])
        ptn_ = Ring([ps(f"ptno{i}", [128, 512]) for i in range(2)])
        plg_ = Ring([ps(f"plgo{i}", [128, 512]) for i in range(1)])
        sh2 = modT[:, l * 48 + 24:l * 48 + 32]
        def stageA(tt):
            tks = slice(tt * 128, (tt + 1) * 128)
            oa, b_oa = oa_.next()
            k.dma("sp", oa[:], dr[f"OATT{l}"][tks, :], reads=[db[f"OATT{l}"]], writes=[b_oa])
            of, b_of = of_.next()
            k.dma("sp", of[:, 0:3, :], dr[f"OLRU{l}"][:, tks].rearrange("(c p) n -> p c n", p=128), reads=[db[f"OLRU{l}"]], writes=[b_of])
            k.dma("sp", of[:, 3:5, :], dr[f"OS5{l}"][:, tks].rearrange("(c p) n -> p c n", p=128), reads=[db[f"OS5{l}"]], writes=[b_of])
            xt, b_x = xt_.next()
            k.dma("sp", xt[:], dr[xin][tks, :], reads=[db[xin]], writes=[b_x])
            st, b_st = st_.next()
            ACT(sq[:, 0:ATT_W], oa[:], AF.Square, [b_oa], [b_sq, b_st], accum_out=st[:, 0:1])
            CP(st[:, 1:2], ssl[:, tt:tt + 1], [b_par, b_st], [b_st])
            TS(st[:, 0:2], st[:, 0:2], 1.0 / ATT_W, EPS, ALU.mult, ALU.add, [b_st], [b_st])
            TS(st[:, 2:3], ss5[:, tt:tt + 1], 1.0 / S5_W, EPS, ALU.mult, ALU.add, [b_par, b_st], [b_st])
            ACT(st[:, 0:3], st[:, 0:3], AF.Sqrt, [b_st], [b_st])
            k.op("dve", lambda e, st=st: e.reciprocal(out=st[:, 0:3], in_=st[:, 0:3]), [b_st], [b_st])
            oT, b_oT = oT_.next()
            pta, b_pta = pta_.next()
            for c in range(3):
                TR(pta[:, c * 128:(c + 1) * 128], oa[:, c * 128:(c + 1) * 128], ident[:], [b_oa, b_ident], [b_pta])
            for c in range(3):
                ACT(oT[:, c, :], pta[:, c * 128:(c + 1) * 128], AF.Copy, [b_pta, b_par], [b_oT], scale=gT[:, c:c + 1])
            for c in range(5):
                TS(oT[:, 3 + c, :], of[:, c, :], gT[:, 3 + c:4 + c], None, ALU.mult, ALU.bypass, [b_of, b_par], [b_oT], eng="pool" if False else "dve")
            xw, b_xw = xw_.next()
            for half in range(2):
                hs = slice(half * 512, (half + 1) * 512)
                pys = []
                for (c0, c1) in ((0, 3), (3, 6), (6, 8)):
                    py, b_py = py_.next()
                    for c in range(c0, c1):
                        MM(py[:], oT[:, c, :], Wo[:, c, hs], c == c0, c == c1 - 1, [b_oT, b_W], [b_py])
                    pys.append((py, b_py))
                tm, b_tm = tm_.next()
                TS(tm[:], pys[0][0][:], st[:, 0:1], None, ALU.mult, ALU.bypass, [pys[0][1], b_st], [b_tm])
                STT(tm[:], pys[1][0][:], st[:, 1:2], tm[:], ALU.mult, ALU.add, [pys[1][1], b_st, b_tm], [b_tm])
                STT(tm[:], pys[2][0][:], st[:, 2:3], tm[:], ALU.mult, ALU.add, [pys[2][1], b_st, b_tm], [b_tm])
                TT(tm[:], tm[:], gbc[:, hs], ALU.mult, [b_tm, b_gbc], [b_tm])
                TT(xw[:, hs], tm[:], xt[:, hs], ALU.add, [b_tm, b_x], [b_xw])
            k.dma("pool", dr[f"XA{l}"][tks, :], xw[:], reads=[b_xw], writes=[db[f"XA{l}"]])
            carry[tt] = (xw, b_xw, st, b_st)

        def stageB(tt):
            tks = slice(tt * 128, (tt + 1) * 128)
            xw, b_xw, st, b_st = carry.pop(tt)
            ACT(sq[:], xw[:], AF.Square, [b_xw], [b_sq, b_st], accum_out=st[:, 4:5])
            TS(st[:, 5:6], st[:, 4:5], 1.0 / D, EPS, ALU.mult, ALU.add, [b_st], [b_st])
            ACT(st[:, 5:6], st[:, 5:6], AF.Sqrt, [b_st], [b_st])
            k.op("dve", lambda e, st=st: e.reciprocal(out=st[:, 5:6], in_=st[:, 5:6]), [b_st], [b_st])
            xn, b_xn = xn_.next()
            TS(xn[:], xw[:], st[:, 5:6], None, ALU.mult, ALU.bypass, [b_xw, b_st], [b_xn])
            h2, b_h2 = h2_.next()
            h32, b_h32 = h32_.next()
            for q in range(2):
                ptn, b_ptn = ptn_.next()
                for kq in range(4):
                    kk = q * 4 + kq
                    TR(ptn[:, kq * 128:(kq + 1) * 128], xn[:, kk * 128:(kk + 1) * 128], ident[:], [b_xn, b_ident], [b_ptn])
                for kq in range(4):
                    kk = q * 4 + kq
                    ACT(h2[:, kk, :], ptn[:, kq * 128:(kq + 1) * 128], AF.Identity, [b_ptn, b_gs2T, b_modT], [b_h2],
                        scale=gs2T[:, l * 8 + kk:l * 8 + kk + 1], bias=sh2[:, kk:kk + 1])
                    if moe:
                        ACT(h32[:, kk, :], ptn[:, kq * 128:(kq + 1) * 128], AF.Identity, [b_ptn, b_gs2T, b_modT], [b_h32],
                            scale=gs2T[:, l * 8 + kk:l * 8 + kk + 1], bias=sh2[:, kk:kk + 1])
            k.dma("pool", dr[f"H2T{l}"][:, tks].rearrange("(k p) n -> p k n", p=128), h2[:], reads=[b_h2], writes=[db[f"H2T{l}"]])
            if moe:
                plg, b_plg = plg_.next()
                for kk in range(8):
                    MM(plg[:, 0:8], h32[:, kk, :], rw32[:, kk, :], kk == 0, kk == 7, [b_h32, b_par], [b_plg])
                lg, b_lg = lg_.next()
                CP(lg[:], plg[:, 0:8], [b_plg], [b_lg])
                t8, b_t8 = t8_.next()
                k.op("dve", lambda e, t8=t8, lg=lg: e.max(out=t8[:], in_=lg[:]), [b_lg], [b_t8])
                wv, b_wv = wv_.next()
                TT(wv[:, 0:1], t8[:, 0:1], t8[:, 1:2], ALU.subtract, [b_t8], [b_wv])
                ACT(wv[:, 1:2], wv[:, 0:1], AF.Sigmoid, [b_wv], [b_wv])
                ACT(wv[:, 2:3], wv[:, 0:1], AF.Sigmoid, [b_wv], [b_wv], scale=-1.0)
                TS(rwo[:, tt, 0:8], lg[:], t8[:, 0:1], None, ALU.is_equal, ALU.bypass, [b_lg, b_t8], [b_rwo])
                TS(rwo[:, tt, 8:16], lg[:], t8[:, 1:2], None, ALU.is_equal, ALU.bypass, [b_lg, b_t8], [b_rwo])
                CP(rwo[:, tt, 16:18], wv[:, 1:3], [b_wv], [b_rwo])
        carry = {}
        stageA(0)
        stageA(1)
        for tt in range(NT):
            if tt + 2 < NT:
                stageA(tt + 2)
            stageB(tt)
        if moe:
            k.dma("sp", dr[f"RW{l}"], rwo[:].rearrange("p t e -> p (t e)"), reads=[b_rwo], writes=[db[f"RW{l}"]])
        k.barrier()
        k.emit()


def phase_ffn(P, l, G, ACT, TS, TT, STT, CP, MM, TR):
    nc, k, dr, db = P.nc, P.k, P.dr, P.db
    moe = (l % 2 == 1)
    last = (l == DEPTH - 1)
    if moe:
        passes = [(e, f0, 4) for e in range(N_EXP) for f0 in range(0, 28, 4)]
        wgn, wun, wdn = "moe_wg", "moe_wu", "moe_wd"
    else:
        passes = [(0, 0, 4), (0, 4, 4), (0, 8, 4), (0, 12, 4), (0, 16, 3), (0, 19, 3)]
        wgn, wun, wdn = "ffn_wg", "ffn_wu", "ffn_wd"
    MAXC = 4
    HT = S // 2
    with ExitStack() as es:
        sb = lambda name, shape, dt=F32: es.enter_context(nc.sbuf_tensor(P.nm(name), list(shape), dt))
        ps = lambda name, shape, dt=F32: es.enter_context(nc.psum_tensor(P.nm(name), list(shape), dt))
        h2T = sb("h2T", [128, 8, HT], BF16); b_h2T = Buf()
        acc = sb("acc", [128, 16, D]); b_acc = [Buf() for _ in range(16)]
        rw = sb("rwf", [128, NT, 8]); b_rw = Buf()
        if moe:
            k.dma("sp", rw[:].rearrange("p t e -> p (t e)"), dr[f"RW{l}"], reads=[db[f"RW{l}"]], writes=[b_rw])
        gbc = sb("gbcf", [128, D]); b_gbc = Buf()
        k.dma("sp", gbc[:], dr["GBC"][:, (l * 2 + 1) * D:(l * 2 + 2) * D], reads=[db["GBC"]], writes=[b_gbc])
        b_fg = Buf()
        if last:
            fg = sb("fg", [128, D])
            k.dma("sp", fg[:], dr["final_g"].partition_broadcast(128), writes=[b_fg])
        wg_ = Ring([sb(f"wg{i}", [128, 8, MAXC * 128], BF16) for i in range(2)])
        wu_ = Ring([sb(f"wu{i}", [128, 8, MAXC * 128], BF16) for i in range(2)])
        wd_ = Ring([sb(f"wd{i}", [128, MAXC, D], BF16) for i in range(2)])
        aT_ = Ring([sb(f"aT{i}", [128, MAXC, 512], BF16) for i in range(2)])
        sg_ = Ring([sb(f"sg{i}", [128, 512]) for i in range(2)])
        xt_ = Ring([sb(f"xtf{i}", [128, D]) for i in range(2)])
        sq = sb("sqf", [128, D], BF16); b_sq = Buf()
        st_ = Ring([sb(f"stf{i}", [128, 2]) for i in range(2)])
        pg_ = Ring([ps(f"pgf{i}", [128, 512]) for i in range(2)])
        pu_ = Ring([ps(f"puf{i}", [128, 512]) for i in range(2)])
        pd_ = Ring([ps(f"pdf{i}", [128, 512]) for i in range(4)])
        pending_down = None
        for th in range(2):
            for kk in range(8):
                k.dma("sp", h2T[:, kk, :], dr[f"H2T{l}"][kk * 128:(kk + 1) * 128, th * HT:(th + 1) * HT], reads=[db[f"H2T{l}"]], writes=[b_h2T])
            for pi, (e, f0, nch) in enumerate(passes):
                wg, b_wg = wg_.next()
                wu, b_wu = wu_.next()
                wd, b_wd = wd_.next()
                fs = slice(f0 * 128, (f0 + nch) * 128)
                for kk in range(8):
                    k.dma("pool", wg[:, kk, 0:nch * 128], dr[wgn][e, kk * 128:(kk + 1) * 128, fs], writes=[b_wg])
                    k.dma("pool", wu[:, kk, 0:nch * 128], dr[wun][e, kk * 128:(kk + 1) * 128, fs], writes=[b_wu])
                for fc in range(nch):
                    k.dma("pool", wd[:, fc, :], dr[wdn][e, (f0 + fc) * 128:(f0 + fc + 1) * 128, :], writes=[b_wd])
                def down(tgi, aT, b_aT, wd=wd, b_wd=b_wd, nch=nch, e=e, pi=pi):
                    for ti in range(4):
                        tl = tgi * 4 + ti
                        for half in range(2):
                            hs = slice(half * 512, (half + 1) * 512)
                            pd, b_pd = pd_.next()
                            for fc in range(nch):
                                MM(pd[:], aT[:, fc, ti * 128:(ti + 1) * 128], wd[:, fc, hs], fc == 0, fc == nch - 1, [b_aT, b_wd], [b_pd])
                            if moe:
                                sc = rw[:, th * 16 + tl, e:e + 1]
                                if pi == 0:
                                    TS(acc[:, tl, hs], pd[:], sc, None, ALU.mult, ALU.bypass, [b_pd, b_rw], [b_acc[tl]])
                                else:
                                    STT(acc[:, tl, hs], pd[:], sc, acc[:, tl, hs], ALU.mult, ALU.add, [b_pd, b_rw, b_acc[tl]], [b_acc[tl]])
                            else:
                                if pi == 0:
                                    CP(acc[:, tl, hs], pd[:], [b_pd], [b_acc[tl]])
                                else:
                                    TT(acc[:, tl, hs], acc[:, tl, hs], pd[:], ALU.add, [b_pd, b_acc[tl]], [b_acc[tl]])
                for tgi in range(4):
                    tsl = slice(tgi * 512, (tgi + 1) * 512)
                    aT, b_aT = aT_.next()
                    for fc in range(nch):
                        pg, b_pg = pg_.next()
                        pu, b_pu = pu_.next()
                        for kk in range(8):
                            MM(pg[:], wg[:, kk, fc * 128:(fc + 1) * 128], h2T[:, kk, tsl], kk == 0, kk == 7, [b_wg, b_h2T], [b_pg])
                        for kk in range(8):
                            MM(pu[:], wu[:, kk, fc * 128:(fc + 1) * 128], h2T[:, kk, tsl], kk == 0, kk == 7, [b_wu, b_h2T], [b_pu])
                        sg, b_sg = sg_.next()
                        ACT(sg[:], pg[:], AF.Silu, [b_pg], [b_sg])
                        TT(aT[:, fc, :], sg[:], pu[:], ALU.mult, [b_sg, b_pu], [b_aT])
                    if pending_down is not None:
                        pending_down()
                    pending_down = (lambda tgi=tgi, aT=aT, b_aT=b_aT, down=down: down(tgi, aT, b_aT))
            if pending_down is not None:
                pending_down()
                pending_down = None
            for tl in range(16):
                tt = th * 16 + tl
                tks = slice(tt * 128, (tt + 1) * 128)
                xt, b_x = xt_.next()
                k.dma("sp", xt[:], dr[f"XA{l}"][tks, :], reads=[db[f"XA{l}"]], writes=[b_x])
                TT(acc[:, tl, :], acc[:, tl, :], gbc[:], ALU.mult, [b_acc[tl], b_gbc], [b_acc[tl]])
                TT(xt[:], xt[:], acc[:, tl, :], ALU.add, [b_x, b_acc[tl]], [b_x])
                if not last:
                    k.dma("pool", dr[f"XB{l}"][tks, :], xt[:], reads=[b_x], writes=[db[f"XB{l}"]])
                else:
                    if f"XB{l}" in P.dbg:
                        k.dma("pool", dr[f"XB{l}"][tks, :], xt[:], reads=[b_x], writes=[db[f"XB{l}"]])
                    st, b_st = st_.next()
                    ACT(sq[:], xt[:], AF.Square, [b_x], [b_sq, b_st], accum_out=st[:, 0:1])
                    TS(st[:, 1:2], st[:, 0:1], 1.0 / D, EPS, ALU.mult, ALU.add, [b_st], [b_st])
                    ACT(st[:, 1:2], st[:, 1:2], AF.Sqrt, [b_st], [b_st])
                    k.op("dve", lambda e, st=st: e.reciprocal(out=st[:, 1:2], in_=st[:, 1:2]), [b_st], [b_st])
                    STT(xt[:], xt[:], st[:, 1:2], fg[:], ALU.mult, ALU.mult, [b_x, b_st, b_fg], [b_x])
                    k.dma("pool", dr["y"][tks, :], xt[:], reads=[b_x], writes=[db["y"]])
        k.barrier()
        k.emit()


def phase_moe(P, l, G, ACT, TS, TT, STT, CP, MM, TR):
    nc, k, dr, db = P.nc, P.k, P.dr, P.db
    U32 = mybir.dt.uint32
    TG = 1024
    NGRP = 16
    last = (l == DEPTH - 1)
    IOA = bass.IndirectOffsetOnAxis
    with ExitStack() as es:
        sb = lambda name, shape, dt=F32: es.enter_context(nc.sbuf_tensor(P.nm(name), list(shape), dt))
        s1u = sb("s1u", [128, NT], U32); s2u = sb("s2u", [128, NT], U32); b_su = Buf()
        idxu = sb("idxu", [128, NGRP, 60], U32); b_idx = Buf()
        rt = sb("rt", [128, NT, 18]); b_rt = Buf()
        identb = sb("identb", [128, 128], BF16); b_c = Buf()
        k.dma("sp", identb[:], dr["identb"], writes=[b_c])
        k.dma("sp", rt[:].rearrange("p t e -> p (t e)"), dr[f"RW{l}"], reads=[db[f"RW{l}"]], writes=[b_rt])
        with ExitStack() as es2:
            sb2 = lambda name, shape, dt=F32: es2.enter_context(nc.sbuf_tensor(P.nm(name), list(shape), dt))
            ps2 = lambda name, shape, dt=F32: es2.enter_context(nc.psum_tensor(P.nm(name), list(shape), dt))
            tri = sb2("tri", [128, 128]); on = sb2("on128", [128, 128]); sTv = sb2("sTv", [128, 16])
            base60 = sb2("base60", [128, 60]); mult60 = sb2("mult60", [128, 60])
            k.dma("sp", tri[:], dr["tri"], writes=[b_c]); k.dma("sp", sTv[:], dr["sTv"], writes=[b_c])
            k.dma("sp", base60[:], dr["base60"], writes=[b_c]); k.dma("sp", mult60[:], dr["mult60"], writes=[b_c])
            k.op("dve", lambda e: e.memset(on[:], 1.0), [], [b_c])
            ind = sb2("ind", [128, NT, 8]); b_ind = Buf()
            TT(ind[:], rt[:, :, 0:8], rt[:, :, 8:16], ALU.add, [b_rt], [b_ind])
            pc1 = ps2("pc1", [128, 256]); pc2 = ps2("pc2", [128, 256]); b_pc = Buf()
            indf = ind[:].rearrange("p t e -> p (t e)")
            MM(pc1[:], tri[:], indf, True, True, [b_c, b_ind], [b_pc])
            MM(pc2[:], on[:], indf, True, True, [b_c, b_ind], [b_pc])
            tot = sb2("tot", [128, NT, 8]); b_tot = Buf()
            CP(tot[:].rearrange("p t e -> p (t e)"), pc2[:], [b_pc], [b_tot])
            cum = sb2("cum", [128, 8, NT]); b_cum = Buf()
            for e_ in range(8):
                k.op("dve", lambda e, e_=e_: e.tensor_tensor_scan(out=cum[:, e_, :], data0=on[:, 0:NT], data1=tot[:, :, e_],
                                                                initial=0.0, op0=ALU.mult, op1=ALU.add), [b_tot, b_c], [b_cum])
            cnt = sb2("cnt", [128, 8]); b_cnt = Buf()
            CP(cnt[:], cum[:, :, NT - 1], [b_cum], [b_cnt])
            excl = sb2("excl", [128, 8, NT]); b_ex = Buf()
            TT(excl[:], cum[:], tot[:].rearrange("p t e -> p e t"), ALU.subtract, [b_cum, b_tot], [b_ex])
            pcn = sb2("pcn", [128, 8]); b_pcn = Buf()
            TS(pcn[:], cnt[:], float(TG - 1), 1.0 / TG, ALU.add, ALU.mult, [b_cnt], [b_pcn])
            TS(pcn[:], pcn[:], -0.4995, MAGIC, ALU.add, ALU.add, [b_pcn], [b_pcn])
            TS(pcn[:], pcn[:], MAGIC, float(TG), ALU.subtract, ALU.mult, [b_pcn], [b_pcn])
            incl = sb2("incl", [128, 8]); b_incl = Buf()
            k.op("dve", lambda e: e.tensor_tensor_scan(out=incl[:], data0=on[:, 0:8], data1=pcn[:], initial=0.0, op0=ALU.mult, op1=ALU.add),
                 [b_pcn, b_c], [b_incl])
            bse = sb2("bse", [128, 8]); b_bse = Buf()
            TT(bse[:], incl[:], pcn[:], ALU.subtract, [b_incl, b_pcn], [b_bse])
            slot = sb2("slot", [128, 8, NT]); b_slot = Buf()
            TT(slot[:], pc1[:].rearrange("p (t e) -> p e t", e=8), excl[:], ALU.add, [b_pc, b_ex], [b_slot])
            for e_ in range(8):
                TS(slot[:, e_, :], slot[:, e_, :], bse[:, e_:e_ + 1], None, ALU.add, ALU.bypass, [b_slot, b_bse], [b_slot])
            tmp = sb2("tmpsl", [128, 8, NT]); b_tmp = Buf()
            sf = sb2("sf", [128, 2, NT]); b_sf = Buf()
            for j in range(2):
                TT(tmp[:], slot[:], rt[:, :, j * 8:(j + 1) * 8].rearrange("p t e -> p e t"), ALU.mult, [b_slot, b_rt, b_tmp], [b_tmp])
                k.op("dve", lambda e, j=j: e.tensor_reduce(out=sf[:, j, :], in_=tmp[:].rearrange("p e t -> p t e"), axis=AX.X, op=ALU.add),
                     [b_tmp], [b_sf])
            CP(s1u[:], sf[:, 0, :], [b_sf], [b_su])
            CP(s2u[:], sf[:, 1, :], [b_sf], [b_su])
            cmp_ = sb2("cmp", [128, 16, 8]); b_cmp = Buf()
            for e_ in range(8):
                TS(cmp_[:, :, e_], sTv[:], incl[:, e_:e_ + 1], None, ALU.is_ge, ALU.bypass, [b_c, b_incl], [b_cmp])
            eid = sb2("eid", [128, 16]); b_eid = Buf()
            k.op("dve", lambda e: e.tensor_reduce(out=eid[:], in_=cmp_[:], axis=AX.X, op=ALU.add), [b_cmp], [b_eid])
            TS(eid[:], eid[:], 7.0, None, ALU.min, ALU.bypass, [b_eid], [b_eid])
            idxf = sb2("idxf", [128, NGRP, 60]); b_if = Buf()
            for s_ in range(NGRP):
                STT(idxf[:, s_, :], mult60[:], eid[:, s_:s_ + 1], base60[:], ALU.mult, ALU.add, [b_c, b_eid], [b_if])
            CP(idxu[:].rearrange("p s c -> p (s c)"), idxf[:].rearrange("p s c -> p (s c)"), [b_if], [b_idx])
            if "SLOTS" in P.dbg:
                k.dma("sp", dr["SLOTS"][:, 0:NT], s1u[:], reads=[b_su], writes=[db["SLOTS"]])
                k.dma("sp", dr["SLOTS"][:, NT:2 * NT], s2u[:], reads=[b_su], writes=[db["SLOTS"]])
            h2t_ = Ring([sb2(f"h2t{i}", [128, 8, 128], BF16) for i in range(6)])
            hrow_ = Ring([sb2(f"hrow{i}", [128, D], BF16) for i in range(6)])
            pT_ = Ring([ps2(f"pTs{i}", [128, D], BF16) for i in range(2)])
            for tt in range(NT):
                tks = slice(tt * 128, (tt + 1) * 128)
                h2t, b_h2t = h2t_.next()
                k.dma("sp", h2t[:], dr[f"H2T{l}"][:, tks].rearrange("(k p) n -> p k n", p=128), reads=[db[f"H2T{l}"]], writes=[b_h2t])
                pT, b_pT = pT_.next()
                for kk in range(8):
                    TR(pT[:, kk * 128:(kk + 1) * 128], h2t[:, kk, :], identb[:], [b_h2t, b_c], [b_pT])
                hrow, b_hrow = hrow_.next()
                if tt % 2 == 0:
                    CP(hrow[:], pT[:], [b_pT], [b_hrow])
                else:
                    ACT(hrow[:], pT[:], AF.Copy, [b_pT], [b_hrow])
                for su in (s1u, s2u):
                    k.op("pool", lambda e, su=su, tt=tt, hrow=hrow: e.indirect_dma_start(
                        out=dr[f"HS{l}"], out_offset=IOA(ap=su[:, tt:tt + 1], axis=0), in_=hrow[:], in_offset=None),
                        [b_hrow, b_su], [db[f"HS{l}"]], dma=True)
            k.barrier()
        NCH = 7
        gbc = sb("gbcm", [128, D]); b_gbc = Buf()
        k.dma("sp", gbc[:], dr["GBC"][:, (l * 2 + 1) * D:(l * 2 + 2) * D], reads=[db["GBC"]], writes=[b_gbc])
        b_fg = Buf()
        if last:
            fg = sb("fgm", [128, D])
            k.dma("sp", fg[:], dr["final_g"].partition_broadcast(128), writes=[b_fg])
        with ExitStack() as es3:
            sb3 = lambda name, shape, dt=F32: es3.enter_context(nc.sbuf_tensor(P.nm(name), list(shape), dt))
            ps3 = lambda name, shape, dt=F32: es3.enter_context(nc.psum_tensor(P.nm(name), list(shape), dt))
            hs_ = Ring([sb3(f"hs{i}", [128, 8, D], BF16) for i in range(1)])
            hT_ = Ring([sb3(f"hTm{i}", [128, 8, TG], BF16) for i in range(2)])
            acc = sb3("accm", [128, 8, D]); b_acc = [Buf() for _ in range(8)]
            wg_ = Ring([sb3(f"wgm{i}", [128, 8, NCH * 128], BF16) for i in range(2)])
            wu_ = Ring([sb3(f"wum{i}", [128, 8, NCH * 128], BF16) for i in range(2)])
            wd_ = Ring([sb3(f"wdm{i}", [128, NCH, D], BF16) for i in range(2)])
            aT_ = Ring([sb3(f"aTm{i}", [128, NCH, 512], BF16) for i in range(2)])
            sg_ = Ring([sb3(f"sgm{i}", [128, 512]) for i in range(2)])
            pg_ = Ring([ps3(f"pgm{i}", [128, 512]) for i in range(2)])
            pu_ = Ring([ps3(f"pum{i}", [128, 512]) for i in range(2)])
            pd_ = Ring([ps3(f"pdm{i}", [128, 512]) for i in range(3)])
            pT_ = Ring([ps3(f"pTm{i}", [128, D], BF16) for i in range(1)])
            pending = [None]

            def prep(s_):
                hs, b_hs = hs_.next()
                k.dma("sp", hs[:], dr[f"HS{l}"][s_ * TG:(s_ + 1) * TG, :].rearrange("(t p) d -> p t d", p=128), reads=[db[f"HS{l}"]], writes=[b_hs])
                hT, b_hT = hT_.next()
                for kk in range(8):
                    pT, b_pT = pT_.next()
                    for j in range(8):
                        TR(pT[:, j * 128:(j + 1) * 128], hs[:, j, kk * 128:(kk + 1) * 128], identb[:], [b_hs, b_c], [b_pT])
                    if kk % 2 == 0:
                        ACT(hT[:, kk, :], pT[:], AF.Copy, [b_pT], [b_hT])
                    else:
                        CP(hT[:, kk, :], pT[:], [b_pT], [b_hT])
                return hT, b_hT

            nxt = prep(0)
            for s_ in range(NGRP):
                hT, b_hT = nxt
                for pi in range(4):
                    wg, b_wg = wg_.next()
                    wu, b_wu = wu_.next()
                    wd, b_wd = wd_.next()
                    for kk in range(8):
                        col = kk * 4 + pi
                        k.op("pool", lambda e, wg=wg, kk=kk, s_=s_, col=col: e.indirect_dma_start(
                            out=wg[:, kk, :], out_offset=None, in_=dr["moe_wg"], in_offset=IOA(ap=idxu[:, s_, col:col + 1], axis=0)),
                            [b_idx], [b_wg], dma=True)
                        k.op("pool", lambda e, wu=wu, kk=kk, s_=s_, col=col: e.indirect_dma_start(
                            out=wu[:, kk, :], out_offset=None, in_=dr["moe_wu"], in_offset=IOA(ap=idxu[:, s_, col:col + 1], axis=0)),
                            [b_idx], [b_wu], dma=True)
                    for fc in range(NCH):
                        col = 32 + pi * NCH + fc
                        k.op("pool", lambda e, wd=wd, fc=fc, s_=s_, col=col: e.indirect_dma_start(
                            out=wd[:, fc, :], out_offset=None, in_=dr["moe_wd"], in_offset=IOA(ap=idxu[:, s_, col:col + 1], axis=0)),
                            [b_idx], [b_wd], dma=True)

                    def down(tgi, aT, b_aT, wd=wd, b_wd=b_wd, pi=pi):
                        for ti in range(4):
                            tl = tgi * 4 + ti
                            for half in range(2):
                                hsl = slice(half * 512, (half + 1) * 512)
                                pd, b_pd = pd_.next()
                                for fc in range(NCH):
                                    MM(pd[:], aT[:, fc, ti * 128:(ti + 1) * 128], wd[:, fc, hsl], fc == 0, fc == NCH - 1, [b_aT, b_wd], [b_pd])
                                if pi == 0:
                                    CP(acc[:, tl, hsl], pd[:], [b_pd], [b_acc[tl]])
                                else:
                                    TT(acc[:, tl, hsl], acc[:, tl, hsl], pd[:], ALU.add, [b_pd, b_acc[tl]], [b_acc[tl]])

                    for tgi in range(2):
                        tsl = slice(tgi * 512, (tgi + 1) * 512)
                        aT, b_aT = aT_.next()
                        for fc in range(NCH):
                            pg, b_pg = pg_.next()
                            pu, b_pu = pu_.next()
                            for kk in range(8):
                                MM(pg[:], wg[:, kk, fc * 128:(fc + 1) * 128], hT[:, kk, tsl], kk == 0, kk == 7, [b_wg, b_hT], [b_pg])
                            for kk in range(8):
                                MM(pu[:], wu[:, kk, fc * 128:(fc + 1) * 128], hT[:, kk, tsl], kk == 0, kk == 7, [b_wu, b_hT], [b_pu])
                            sg, b_sg = sg_.next()
                            ACT(sg[:], pg[:], AF.Silu, [b_pg], [b_sg])
                            TT(aT[:, fc, :], sg[:], pu[:], ALU.mult, [b_sg, b_pu], [b_aT])
                        if pending[0] is not None:
                            pending[0]()
                        pending[0] = (lambda tgi=tgi, aT=aT, b_aT=b_aT, down=down: down(tgi, aT, b_aT))
                    if pi == 2 and s_ + 1 < NGRP:
                        nxt = prep(s_ + 1)
                pending[0]()
                pending[0] = None
                k.dma("sp", dr[f"OS{l}"][s_ * TG:(s_ + 1) * TG, :].rearrange("(t p) d -> p t d", p=128), acc[:], reads=b_acc, writes=[db[f"OS{l}"]])
            k.barrier()
        r1_ = Ring([sb(f"r1{i}", [128, D]) for i in range(4)])
        r2_ = Ring([sb(f"r2{i}", [128, D]) for i in range(4)])
        xt_ = Ring([sb(f"xtm{i}", [128, D]) for i in range(4)])
        sq = sb("sqm", [128, D], BF16); b_sq = Buf()
        st_ = Ring([sb(f"stm{i}", [128, 2]) for i in range(4)])
        for tt in range(NT):
            tks = slice(tt * 128, (tt + 1) * 128)
            r1, b_r1 = r1_.next()
            r2, b_r2 = r2_.next()
            k.op("pool", lambda e, r1=r1, tt=tt: e.indirect_dma_start(out=r1[:], out_offset=None, in_=dr[f"OS{l}"],
                                                                     in_offset=IOA(ap=s1u[:, tt:tt + 1], axis=0)),
                 [db[f"OS{l}"], b_su], [b_r1], dma=True)
            k.op("pool", lambda e, r2=r2, tt=tt: e.indirect_dma_start(out=r2[:], out_offset=None, in_=dr[f"OS{l}"],
                                                                     in_offset=IOA(ap=s2u[:, tt:tt + 1], axis=0)),
                 [db[f"OS{l}"], b_su], [b_r2], dma=True)
            xt, b_x = xt_.next()
            k.dma("sp", xt[:], dr[f"XA{l}"][tks, :], reads=[db[f"XA{l}"]], writes=[b_x])
            TS(r1[:], r1[:], rt[:, tt, 16:17], None, ALU.mult, ALU.bypass, [b_r1, b_rt], [b_r1])
            STT(r1[:], r2[:], rt[:, tt, 17:18], r1[:], ALU.mult, ALU.add, [b_r2, b_rt, b_r1], [b_r1])
            TT(r1[:], r1[:], gbc[:], ALU.mult, [b_r1, b_gbc], [b_r1])
            TT(xt[:], xt[:], r1[:], ALU.add, [b_x, b_r1], [b_x])
            if not last:
                k.dma("act", dr[f"XB{l}"][tks, :], xt[:], reads=[b_x], writes=[db[f"XB{l}"]])
            else:
                if f"XB{l}" in P.dbg:
                    k.dma("act", dr[f"XB{l}"][tks, :], xt[:], reads=[b_x], writes=[db[f"XB{l}"]])
                st, b_st = st_.next()
                ACT(sq[:], xt[:], AF.Square, [b_x], [b_sq, b_st], accum_out=st[:, 0:1])
                TS(st[:, 1:2], st[:, 0:1], 1.0 / D, EPS, ALU.mult, ALU.add, [b_st], [b_st])
                ACT(st[:, 1:2], st[:, 1:2], AF.Sqrt, [b_st], [b_st])
                k.op("dve", lambda e, st=st: e.reciprocal(out=st[:, 1:2], in_=st[:, 1:2]), [b_st], [b_st])
                STT(xt[:], xt[:], st[:, 1:2], fg[:], ALU.mult, ALU.mult, [b_x, b_st, b_fg], [b_x])
                k.dma("act", dr["y"][tks, :], xt[:], reads=[b_x], writes=[db["y"]])
        k.barrier()
        k.emit()


def gelu_tanh(P, out, b_out, x, b_x, ra_, rb_, ACT, TS, TT):
    ra, b_ra = ra_.next()
    rb, b_rb = rb_.next()
    ACT(ra[:], x[:], AF.Square, [b_x], [b_ra])
    TS(ra[:], ra[:], 0.044715, 1.0, ALU.mult, ALU.add, [b_ra], [b_ra])
    TT(rb[:], ra[:], x[:], ALU.mult, [b_ra, b_x], [b_rb])
    ACT(rb[:], rb[:], AF.Sigmoid, [b_rb], [b_rb], scale=1.5957691216057308)
    TT(out[:], rb[:], x[:], ALU.mult, [b_rb, b_x], [b_out])


def _consts():
    inv = (10000.0 ** (-np.arange(0, 64, 2, dtype=np.float32) / 64)).astype(np.float32)
    invt = (inv.astype(np.float64) / (2 * np.pi)).astype(np.float32)
    invt128 = np.tile(invt, 4).reshape(128, 1).astype(np.float32)
    bf = ml_dtypes.bfloat16
    ohk = (np.arange(S)[None, :] // 256 == np.arange(16)[:, None]).astype(np.float32).astype(bf)
    kk_ = np.arange(128)[:, None, None] + 128 * np.arange(4)[None, :, None]
    qq_ = np.arange(512)[None, None, :]
    same = (kk_ // 256) == (qq_ // 256)
    cbias = np.where(same & (kk_ > qq_), NEGB, 0.0).astype(np.float32).reshape(128, 2048).astype(bf)
    n_ = np.arange(16)[None, :]
    j_ = np.arange(16)[:, None]
    padm = np.broadcast_to((n_ < j_).astype(np.float32).reshape(1, 256), (128, 256))
    pada = np.broadcast_to(np.where(n_ == j_, 1e30, np.where(n_ > j_, -1e30, 0.0)).astype(np.float32).reshape(1, 256), (128, 256))
    tv = np.ascontiguousarray(np.broadcast_to(np.arange(S, dtype=np.float32)[None], (128, S)))
    top = (np.arange(128) < 64).astype(np.float32)
    tbm = np.stack([top, -top, np.full(128, np.pi / 2, np.float32), -(1 - top)], axis=1).astype(np.float32)
    p_ = np.arange(128)[:, None]
    tri = (np.arange(128)[:, None] < np.arange(128)[None, :]).astype(np.float32)
    sTv = np.ascontiguousarray(np.broadcast_to((np.arange(16, dtype=np.float32) * 1024.0)[None], (128, 16)))
    b_gu = ((np.arange(8)[None, :, None] * 128 + p_[:, :, None]) * 4 + np.arange(4)[None, None, :]).reshape(128, 32)
    b_d = (np.arange(28)[None, :] * 128 + p_)
    base60 = np.concatenate([b_gu, b_d], axis=1).astype(np.float32)
    mult60 = np.ascontiguousarray(np.broadcast_to(np.concatenate([np.full(32, 4096.0), np.full(28, 3584.0)])[None], (128, 60))).astype(np.float32)
    padm32 = np.ascontiguousarray(padm.reshape(128, 16, 16)[:, np.arange(32) // 2, :].reshape(128, 512))
    pada32 = np.ascontiguousarray(pada.reshape(128, 16, 16)[:, np.arange(32) // 2, :].reshape(128, 512))
    return dict(padm32=padm32, pada32=pada32, tri=tri, sTv=sTv, base60=base60, mult60=mult60, tvals=tv, tbm=tbm, invt=invt128, ident=np.eye(128, dtype=np.float32), ohk=ohk, identb=np.eye(128, dtype=np.float32).astype(bf),
                cbias=cbias, padm=np.ascontiguousarray(padm), pada=np.ascontiguousarray(pada))


def _swap_heads(w):
    w4 = w.reshape(w.shape[0], 6, 2, 32)
    return np.ascontiguousarray(w4[:, :, ::-1, :]).reshape(w.shape[0], 384)


def make_inputs(inp, b):
    f = lambda a: np.ascontiguousarray(a, dtype=np.float32)
    if "consts" not in _CACHE:
        _CACHE["consts"] = _consts()
    m = dict(_CACHE["consts"])
    m["x"] = f(inp["x"][b])
    m["cT"] = f(inp["c"][b].reshape(8, 128).T)
    m["pos"] = np.ascontiguousarray(inp["positions"][b].reshape(1, S).astype(np.int32))
    w_in = inp["w_in"]
    ext = []
    for l in range(DEPTH):
        w = w_in[l]
        q, kk_, rest = w[:, 0:384], w[:, 384:768], w[:, 768:]
        ext.append(np.concatenate([q, _swap_heads(q), kk_, _swap_heads(kk_), rest], axis=1))
    m["w_in"] = f(np.stack(ext))
    m["ada_w"] = f(inp["ada_w"])
    m["ada_b"] = f(inp["ada_b"])
    m["ada_bT"] = f(inp["ada_b"].reshape(DEPTH, 48, 128).transpose(2, 0, 1).reshape(128, DEPTH * 48))
    m["g1T"] = f(inp["norm1_g"].reshape(DEPTH, 8, 128).transpose(2, 0, 1).reshape(128, DEPTH * 8))
    m["g2T"] = f(inp["norm2_g"].reshape(DEPTH, 8, 128).transpose(2, 0, 1).reshape(128, DEPTH * 8))
    m["w_out"] = f(inp["w_out"])
    m["gainT"] = f(inp["mix_gain"].reshape(DEPTH, 8, 128).transpose(2, 0, 1).reshape(128, DEPTH * 8))
    m["router_w"] = f(inp["router_w"][0].reshape(8, 128, 8).transpose(1, 0, 2).reshape(128, 64))
    m["ffn_wg"] = f(inp["ffn_w_gate"]); m["ffn_wu"] = f(inp["ffn_w_up"]); m["ffn_wd"] = f(inp["ffn_w_down"])
    m["moe_wg"] = f(inp["moe_w_gate"][0]).reshape(N_EXP * D * 4, 896)
    m["moe_wu"] = f(inp["moe_w_up"][0]).reshape(N_EXP * D * 4, 896)
    m["moe_wd"] = f(inp["moe_w_down"][0]).reshape(N_EXP * D_FFE, D)
    m["final_g"] = f(inp["final_g"].reshape(1, D))
    L = DEPTH
    dup = lambda a: np.concatenate([a, a], axis=0)
    lrT = dup(inp["s5_lambda_re"].transpose(2, 0, 1))
    liT = dup(inp["s5_lambda_im"].transpose(2, 0, 1))
    ldt = np.broadcast_to(inp["s5_log_dt"][None], (128, L, 16))
    m["s5p"] = f(np.stack([lrT, liT, ldt], axis=2).reshape(128, L * 48))
    br, bi = inp["s5_b_re"], inp["s5_b_im"]
    b1 = np.zeros((128, L, 16, 128), np.float32)
    b2 = np.zeros((128, L, 16, 128), np.float32)
    for g in range(16):
        r0 = 16 * (g % 8)
        b1[r0:r0 + 16, :, g, 0:64] = br[:, g].transpose(2, 0, 1)
        b1[r0:r0 + 16, :, g, 64:128] = bi[:, g].transpose(2, 0, 1)
        b2[r0:r0 + 16, :, g, 0:64] = bi[:, g].transpose(2, 0, 1)
        b2[r0:r0 + 16, :, g, 64:128] = br[:, g].transpose(2, 0, 1)
    m["s5_b1"] = f(b1.reshape(128, -1))
    m["s5_b2"] = f(b2.reshape(128, -1))
    cre = dup(inp["s5_c_re"].transpose(3, 0, 1, 2))
    cim = dup(inp["s5_c_im"].transpose(3, 0, 1, 2))
    m["s5_c"] = f(np.stack([cre, cim], axis=2).reshape(128, L * 512))
    dsk = inp["s5_d"].reshape(L, 2, 128).transpose(2, 0, 1)
    glb = inp["s5_glu_b"].reshape(L, 2, 128).transpose(2, 0, 1)
    m["s5_dg"] = f(np.concatenate([dsk, glb], axis=2).reshape(128, L * 4))
    m["s5_gw"] = f(inp["s5_glu_w"])
    cw = inp["lru_conv_w"].reshape(DEPTH, 4, 3, 128).transpose(3, 0, 2, 1).reshape(128, DEPTH * 12)
    m["lru_cw"] = f(cw)
    vecs = np.stack([inp["lru_conv_b"], inp["lru_b_a"], inp["lru_b_x"], inp["lru_lambda"]], axis=0)
    m["lru_vec"] = f(vecs.reshape(4, DEPTH, 3, 128).transpose(3, 1, 2, 0).reshape(128, DEPTH * 12))
    for nm, key in (("lru_wa", "lru_w_a"), ("lru_wx", "lru_w_x")):
        w = inp[key]
        bd = np.zeros((DEPTH, 3, 128, 128), np.float32)
        for c in range(3):
            for bb in range(2):
                bd[:, c, 64 * bb:64 * bb + 64, 64 * bb:64 * bb + 64] = w[:, 2 * c + bb]
        m[nm] = f(bd.transpose(2, 0, 1, 3).reshape(128, DEPTH * 3 * 128))
    return m


def kernel(**inputs):
    if "prog" not in _CACHE:
        _CACHE["prog"] = build()
    P = _CACHE["prog"]
    names = [n for n in P.dr if True]
    in_maps = []
    for b in range(8):
        m = make_inputs(inputs, b)
        in_maps.append(m)
    res = run_bass_kernel_spmd(P.nc, in_maps, core_ids=list(range(8)))
    return np.stack([np.asarray(r["y"], dtype=np.float32) for r in res.results], axis=0)
```

```python
import math
from contextlib import ExitStack
import numpy as np
import ml_dtypes
import concourse.bass as bass
import concourse.mybir as mybir
from concourse.bass_utils import run_bass_kernel_spmd

F32 = mybir.dt.float32
BF16 = mybir.dt.bfloat16
I32 = mybir.dt.int32
AF = mybir.ActivationFunctionType
ALU = mybir.AluOpType
AX = mybir.AxisListType

S = 4096
D = 1024
NT = S // 128
NG = S // 512
DEPTH = 2
ATT_W = 384
LRU_W = 384
S5_W = 256
IN_EXT = 2176 + 768
D_FF = 2816
N_EXP = 8
D_FFE = 3584
EPS = 1e-6
MAGIC = 12582912.0
NEGB = -30000.0
DEBUG = False


_CACHE = {}


class Buf:
    __slots__ = ("name", "w", "r")

    def __init__(self, name=""):
        self.name = name
        self.w = None
        self.r = []


class Eng:
    def __init__(self, name):
        self.name = name
        self.ops = []
        self.count = 0
        self.sem = None
        self.waited = {}
        self.dma_sems = []
        self.dma_uses = []
        self.dma_next = 0


class K:
    def __init__(self, nc, n_dma_sems=20):
        self.nc = nc
        self.engs = {n: Eng(n) for n in ("pe", "act", "dve", "pool", "sp")}
        self.sem_objs = {}
        self.n_dma_sems = n_dma_sems
        self.es = ExitStack()

    def setup_sems(self):
        sid = 0
        for n, e in self.engs.items():
            e.sem = sid
            self.sem_objs[sid] = self.es.enter_context(self.nc.semaphore("s_" + n))
            sid += 1
        for n in ("sp", "pool", "act"):
            e = self.engs[n]
            for i in range(self.n_dma_sems):
                self.sem_objs[sid] = self.es.enter_context(self.nc.semaphore(f"d_{n}{i}"))
                e.dma_sems.append(sid)
                e.dma_uses.append(0)
                sid += 1

    def op(self, engname, fn, reads=(), writes=(), dma=False):
        E = self.engs[engname]
        deps = {}

        def add(t):
            cur = deps.get(t[0])
            if cur is None or cur[0] < t[1]:
                deps[t[0]] = (t[1], t[2], t[3])

        for b in reads:
            if b.w is not None:
                add(b.w)
        for b in writes:
            if b.w is not None:
                add(b.w)
            for t in b.r:
                add(t)
        waits = []
        for sem, (val, src, src_dma) in deps.items():
            if engname == "pe" and src == "pe" and not src_dma:
                continue
            if E.waited.get(sem, 0) >= val:
                continue
            E.waited[sem] = val
            waits.append((sem, val))
        if dma:
            i = E.dma_next
            E.dma_next = (i + 1) % len(E.dma_sems)
            sem = E.dma_sems[i]
            prev = 16 * E.dma_uses[i]
            E.dma_uses[i] += 1
            val = prev + 16
            if prev > 0 and E.waited.get(sem, 0) < prev:
                E.waited[sem] = prev
                waits.append((sem, prev))
            inc = 16
        else:
            sem = E.sem
            E.count += 1
            val = E.count
            inc = 1
        tok = (sem, val, engname, dma)
        for b in reads:
            b.r.append(tok)
        for b in writes:
            b.w = tok
            b.r = []
        E.ops.append((waits, fn, sem, inc))
        return tok

    def dma(self, q, out, in_, reads=(), writes=(), **kw):
        return self.op(q, lambda e: e.dma_start(out=out, in_=in_, **kw), reads, writes, dma=True)

    def barrier(self):
        targets = []
        for n, e in self.engs.items():
            if e.count > 0:
                targets.append((e.sem, e.count))
            for s, u in zip(e.dma_sems, e.dma_uses):
                if u > 0:
                    targets.append((s, 16 * u))
        for n, e in self.engs.items():
            waits = []
            for s, v in targets:
                if e.waited.get(s, 0) < v:
                    e.waited[s] = v
                    waits.append((s, v))
            if waits:
                e.ops.append((waits, None, None, 0))

    def emit(self):
        nc = self.nc
        so = self.sem_objs
        with nc.Block() as block:
            def mk(E):
                ops = E.ops
                E.ops = []

                def body(e):
                    for waits, fn, sem, inc in ops:
                        for s, v in waits:
                            e.wait_ge(so[s], v)
                        if fn is not None:
                            fn(e).then_inc(so[sem], inc)
                return body
            block.tensor(mk(self.engs["pe"]))
            block.scalar(mk(self.engs["act"]))
            block.vector(mk(self.engs["dve"]))
            block.gpsimd(mk(self.engs["pool"]))
            block.sync(mk(self.engs["sp"]))


class Ring:
    def __init__(self, tiles):
        self.tiles = tiles
        self.bufs = [Buf() for _ in tiles]
        self.i = 0

    def next(self):
        t, b = self.tiles[self.i], self.bufs[self.i]
        self.i = (self.i + 1) % len(self.tiles)
        return t, b


class Prog:
    def __init__(self, dbg=()):
        self.nc = nc = bass.Bass("TRN2", target_bir_lowering=False)
        self.k = K(nc)
        self.k.setup_sems()
        self.dbg = set(dbg)
        self.dr = {}
        self.db = {}

    def nm(self, name):
        self.cnt = getattr(self, "cnt", 0) + 1
        return f"{name}_{self.cnt}"

    def din(self, name, shape, dt=F32):
        self.dr[name] = self.nc.dram_tensor(name, list(shape), dt, kind="ExternalInput").ap()
        self.db[name] = Buf(name)
        return self.dr[name]

    def dscratch(self, name, shape, dt=F32):
        kind = "ExternalOutput" if name in self.dbg else "Internal"
        self.dr[name] = self.nc.dram_tensor(name, list(shape), dt, kind=kind).ap()
        self.db[name] = Buf(name)
        return self.dr[name]

    def dout(self, name, shape, dt=F32):
        self.dr[name] = self.nc.dram_tensor(name, list(shape), dt, kind="ExternalOutput").ap()
        self.db[name] = Buf(name)
        return self.dr[name]


def build(dbg=(), upto="all"):
    P = Prog(dbg)
    nc, k = P.nc, P.k
    dr, db = P.dr, P.db

    P.din("x", [S, D])
    P.din("cT", [128, 8])
    P.din("pos", [1, S], I32)
    P.din("invt", [128, 1])
    P.din("ident", [128, 128])
    P.din("w_in", [DEPTH, D, IN_EXT])
    P.din("ada_w", [DEPTH, D, 6 * D])
    P.din("ada_bT", [128, DEPTH * 48])
    P.din("ada_b", [DEPTH, 6 * D])
    P.din("g1T", [128, DEPTH * 8])
    P.din("g2T", [128, DEPTH * 8])
    P.dout("y", [S, D])
    for l in range(DEPTH):
        P.dscratch(f"QT{l}", [ATT_W, S], BF16)
        P.dscratch(f"KT{l}", [ATT_W, S], BF16)
        P.dscratch(f"KM{l}", [128, 3 * 16], F32)
        P.dscratch(f"V{l}", [S, 6 * 65], BF16)
        P.dscratch(f"XR{l}", [LRU_W, S], F32)
        P.dscratch(f"GG{l}", [LRU_W, S], F32)
        P.dscratch(f"U{l}", [S5_W, S], F32)
    P.dscratch("MODT", [128, DEPTH * 48], F32)
    P.dscratch("GBC", [128, DEPTH * 2 * D], F32)
    P.din("ohk", [16, S], BF16)
    P.din("identb", [128, 128], BF16)
    P.din("cbias", [128, 4 * 512], BF16)
    P.din("padm", [128, 256])
    P.din("pada", [128, 256])
    P.din("padm32", [128, 512])
    P.din("pada32", [128, 512])
    for l in range(DEPTH):
        P.dscratch(f"OATT{l}", [S, ATT_W], F32)
    P.din("s5p", [128, DEPTH * 48])
    P.din("s5_b1", [128, DEPTH * 16 * 128])
    P.din("s5_b2", [128, DEPTH * 16 * 128])
    P.din("s5_c", [128, DEPTH * 2 * 256])
    P.din("s5_dg", [128, DEPTH * 4])
    P.din("s5_gw", [DEPTH, 256, 256])
    P.din("tvals", [128, S])
    P.din("tbm", [128, 4])
    for l in range(DEPTH):
        P.dscratch(f"OS5{l}", [S5_W, S], F32)
        P.dscratch(f"SS5{l}", [128, NT], F32)
    P.din("w_out", [DEPTH, D, D])
    P.din("gainT", [128, DEPTH * 8])
    P.din("router_w", [128, 64])
    P.din("ffn_wg", [1, D, D_FF]); P.din("ffn_wu", [1, D, D_FF]); P.din("ffn_wd", [1, D_FF, D])
    P.din("moe_wg", [N_EXP * D * 4, 896]); P.din("moe_wu", [N_EXP * D * 4, 896]); P.din("moe_wd", [N_EXP * D_FFE, D])
    P.din("tri", [128, 128]); P.din("sTv", [128, 16]); P.din("base60", [128, 60]); P.din("mult60", [128, 60])
    P.din("final_g", [1, D])
    for l in range(DEPTH):
        P.dscratch(f"XA{l}", [S, D], F32)
        P.dscratch(f"XB{l}", [S, D], F32)
        P.dscratch(f"H2T{l}", [D, S], BF16)
        P.dscratch(f"RW{l}", [128, NT * 18], F32)
        if l == 1:
            P.dscratch("SLOTS", [128, 2 * NT], mybir.dt.uint32)
        P.dscratch(f"HS{l}", [16 * 1024, D], BF16)
        P.dscratch(f"OS{l}", [16 * 1024, D], F32)
    P.din("lru_cw", [128, DEPTH * 12])
    P.din("lru_vec", [128, DEPTH * 12])
    P.din("lru_wa", [128, DEPTH * 3 * 128])
    P.din("lru_wx", [128, DEPTH * 3 * 128])
    for l in range(DEPTH):
        P.dscratch(f"OLRU{l}", [LRU_W, S], F32)
        P.dscratch(f"SSL{l}", [128, NT], F32)

    top = ExitStack()
    sbt = lambda name, shape, dt=F32: top.enter_context(nc.sbuf_tensor(P.nm(name), list(shape), dt))

    ident = sbt("ident_sb", [128, 128]); b_ident = Buf()
    modT = sbt("modT", [128, DEPTH * 48]); b_modT = Buf()
    gs1T = sbt("gs1T", [128, DEPTH * 8]); b_gs1T = Buf()
    gs2T = sbt("gs2T", [128, DEPTH * 8]); b_gs2T = Buf()

    def ACT(out, in_, func, reads, writes, **kw):
        return k.op("act", lambda e: e.activation(out=out, in_=in_, func=func, **kw), reads, writes)

    def TS(out, in0, s1, s2, op0, op1, reads, writes, eng="dve", **kw):
        return k.op(eng, lambda e: e.tensor_scalar(out=out, in0=in0, scalar1=s1, scalar2=s2, op0=op0, op1=op1, **kw), reads, writes)

    def TT(out, in0, in1, op, reads, writes, eng="dve"):
        return k.op(eng, lambda e: e.tensor_tensor(out=out, in0=in0, in1=in1, op=op), reads, writes)

    def STT(out, in0, scalar, in1, op0, op1, reads, writes):
        return k.op("dve", lambda e: e.scalar_tensor_tensor(out=out, in0=in0, scalar=scalar, in1=in1, op0=op0, op1=op1), reads, writes)

    def CP(out, in_, reads, writes, eng="dve"):
        return k.op(eng, lambda e: e.tensor_copy(out=out, in_=in_), reads, writes)

    def MM(out, lhsT, rhs, start, stop, reads, writes):
        return k.op("pe", lambda e: e.matmul(out, lhsT, rhs, start=start, stop=stop), reads, writes)

    def TR(out, in_, idn, reads, writes):
        return k.op("pe", lambda e: e.transpose(out, in_, idn), reads, writes)

    cond = sbt("cond", [128, 8]); b_cond = Buf()
    condB = sbt("condB", [128, 8, 128]); b_condB = Buf()
    abT = sbt("abT", [128, DEPTH * 48]); b_abT = Buf()
    g1T = sbt("g1T_sb", [128, DEPTH * 8]); g2T = sbt("g2T_sb", [128, DEPTH * 8]); b_gT = Buf()

    def ada_layer(l, sb, ps):
        gate_bc = sb("gate_bc", [128, 2, D]); b_gate_bc = Buf()
        wring = Ring([sb(f"adaw{i}", [128, 8, 512]) for i in range(2)])
        psm = ps("psm", [128, 48]); b_psm = Buf()
        psb = Ring([ps(f"psb{i}", [128, 512]) for i in range(2)])
        abb = Ring([sb(f"abb{i}", [128, 512]) for i in range(2)])
        for cb in range(12):
            wt, wb = wring.next()
            k.dma("sp", wt[:], dr["ada_w"][l, :, cb * 512:(cb + 1) * 512].rearrange("(k p) n -> p k n", p=128), writes=[wb])
            for s in range(4):
                j = cb * 4 + s
                for kk in range(8):
                    MM(psm[:, j:j + 1], wt[:, kk, s * 128:(s + 1) * 128], cond[:, kk:kk + 1], kk == 0, kk == 7,
                       [wb, b_cond], [b_psm])
            if cb in (4, 5, 10, 11):
                pt, pb = psb.next()
                for kk in range(8):
                    MM(pt[:], condB[:, kk, :], wt[:, kk, :], kk == 0, kk == 7, [wb, b_condB], [pb])
                at, ab = abb.next()
                k.dma("sp", at[:], dr["ada_b"][l:l + 1, cb * 512:(cb + 1) * 512].partition_broadcast(128), writes=[ab])
                gi = 0 if cb < 6 else 1
                half = cb % 2
                TT(gate_bc[:, gi, half * 512:(half + 1) * 512], pt[:], at[:], ALU.add, [pb, ab], [b_gate_bc])
        TT(modT[:, l * 48:(l + 1) * 48], psm[:], abT[:, l * 48:(l + 1) * 48], ALU.add, [b_psm, b_abT], [b_modT])
        STT(gs1T[:, l * 8:(l + 1) * 8], modT[:, l * 48 + 8:l * 48 + 16], 1.0, g1T[:, l * 8:(l + 1) * 8], ALU.add, ALU.mult,
            [b_modT, b_gT], [b_gs1T])
        STT(gs2T[:, l * 8:(l + 1) * 8], modT[:, l * 48 + 32:l * 48 + 40], 1.0, g2T[:, l * 8:(l + 1) * 8], ALU.add, ALU.mult,
            [b_modT, b_gT], [b_gs2T])
        k.dma("sp", dr["GBC"][:, l * 2 * D:(l + 1) * 2 * D], gate_bc[:].rearrange("p a d -> p (a d)"), reads=[b_gate_bc], writes=[db["GBC"]])

    with ExitStack() as es:
        sb = lambda name, shape, dt=F32: es.enter_context(nc.sbuf_tensor(P.nm(name), list(shape), dt))
        ps = lambda name, shape, dt=F32: es.enter_context(nc.psum_tensor(P.nm(name), list(shape), dt))
        k.dma("sp", ident[:], dr["ident"], writes=[b_ident])
        cT = sb("cT_sb", [128, 8]); b_cT = Buf()
        k.dma("sp", cT[:], dr["cT"], writes=[b_cT])
        ACT(cond[:], cT[:], AF.Silu, [b_cT], [b_cond])
        ones = sb("ones", [128, 128]); b_ones = Buf()
        k.op("dve", lambda e: e.memset(ones[:], 1.0), [], [b_ones])
        for kk in range(8):
            TS(condB[:, kk, :], ones[:], cond[:, kk:kk + 1], None, ALU.mult, ALU.bypass, [b_ones, b_cond], [b_condB])
        k.dma("sp", abT[:], dr["ada_bT"], writes=[b_abT])
        k.dma("sp", g1T[:], dr["g1T"], writes=[b_gT])
        k.dma("sp", g2T[:], dr["g2T"], writes=[b_gT])
        ada_layer(0, sb, ps)
        if upto == "setup":
            ada_layer(1, sb, ps)
            if "MODT" in P.dbg:
                k.dma("sp", dr["MODT"], modT[:], reads=[b_modT], writes=[db["MODT"]])
        k.barrier()
        k.emit()

    if upto == "setup":
        return P

    P.ada_layer = ada_layer
    for l in range(DEPTH):
        phase_in(P, l, dict(ident=(ident, b_ident), modT=(modT, b_modT), gs1T=(gs1T, b_gs1T)),
                 ACT, TS, TT, STT, CP, MM, TR)
        if upto == f"in{l}":
            return P
        phase_att(P, l, ACT, TS, TT, STT, CP, MM, TR)
        if upto == f"att{l}":
            return P
        phase_lru(P, l, ACT, TS, TT, STT, CP, MM, TR)
        if upto == f"lru{l}":
            return P
        phase_s5(P, l, ACT, TS, TT, STT, CP, MM, TR)
        if upto == f"s5{l}":
            return P
        G2 = dict(ident=(ident, b_ident), modT=(modT, b_modT), gs2T=(gs2T, b_gs2T))
        phase_out(P, l, G2, ACT, TS, TT, STT, CP, MM, TR)
        if upto == f"out{l}":
            return P
        if l % 2 == 1:
            phase_moe(P, l, G2, ACT, TS, TT, STT, CP, MM, TR)
        else:
            phase_ffn(P, l, G2, ACT, TS, TT, STT, CP, MM, TR)
        if upto == f"ffn{l}":
            return P
    return P


def phase_in(P, l, G, ACT, TS, TT, STT, CP, MM, TR):
    nc, k, dr, db = P.nc, P.k, P.dr, P.db
    ident, b_ident = G["ident"]
    modT, b_modT = G["modT"]
    gs1T, b_gs1T = G["gs1T"]
    xin = "x" if l == 0 else f"XB{l - 1}"
    with ExitStack() as es:
        sb = lambda name, shape, dt=F32: es.enter_context(nc.sbuf_tensor(P.nm(name), list(shape), dt))
        ps = lambda name, shape, dt=F32: es.enter_context(nc.psum_tensor(P.nm(name), list(shape), dt))
        COS = sb("COS", [128, S]); SINS = sb("SINS", [128, S]); b_rope = Buf()
        if True:
            sb2 = sb
            posi = sb2("posi", [128, 1024], I32); b_posi = Buf()
            posf = sb2("posf", [128, 1024]); b_posf = Buf()
            invt = sb2("invt_sb", [128, 1]); b_invt = Buf()
            k.dma("sp", invt[:], dr["invt"], writes=[b_invt])
            t1 = sb2("rt1", [128, 1024]); b_t1 = Buf()
            t2 = sb2("rt2", [128, 1024]); b_t2 = Buf()
            sgn = sb2("sgn", [128, 1]); b_sgn = Buf()
            k.op("dve", lambda e: e.memset(sgn[:], 1.0), [], [b_sgn])
            k.op("dve", lambda e: e.memset(sgn[0:32, :], -1.0), [b_sgn], [b_sgn])
            k.op("dve", lambda e: e.memset(sgn[64:96, :], -1.0), [b_sgn], [b_sgn])
            for q4 in range(4):
                qs = slice(q4 * 1024, (q4 + 1) * 1024)
                k.dma("sp", posi[:], dr["pos"][:, qs].partition_broadcast(128), writes=[b_posi])
                CP(posf[:], posi[:], [b_posi], [b_posf])
                for which, dst, off in (("sin", SINS, 0.0), ("cos", COS, 0.25)):
                    TS(t1[:], posf[:], invt[:, 0:1], off, ALU.mult, ALU.add, [b_posf, b_invt], [b_t1])
                    TS(t2[:], t1[:], MAGIC, None, ALU.add, ALU.bypass, [b_t1], [b_t2])
                    TS(t2[:], t2[:], MAGIC, None, ALU.subtract, ALU.bypass, [b_t2], [b_t2])
                    TT(t1[:], t1[:], t2[:], ALU.subtract, [b_t1, b_t2], [b_t1])
                    ACT(dst[:, qs], t1[:], AF.Sin, [b_t1], [b_rope], scale=2.0 * math.pi)
            TS(SINS[:], SINS[:], sgn[:, 0:1], None, ALU.mult, ALU.bypass, [b_rope, b_sgn], [b_rope])
        W = sb("Win", [128, 8, IN_EXT], BF16); b_W = Buf()
        for kk in range(8):
            k.dma("pool", W[:, kk, :], dr["w_in"][l, kk * 128:(kk + 1) * 128, :], writes=[b_W], max_dma_last_dim=2048)
        xr_ = Ring([sb(f"xt{i}", [128, D]) for i in range(4)])
        xn_ = Ring([sb(f"xn{i}", [128, D]) for i in range(3)])
        sq = sb("sq", [128, D]); b_sq = Buf()
        st_ = Ring([sb(f"st{i}", [128, 2]) for i in range(4)])
        hT_ = Ring([sb(f"hT{i}", [128, 8, 512], BF16) for i in range(3)])
        psT_ = Ring([ps(f"psT{i}", [128, D]) for i in range(2)])
        pm_ = Ring([ps(f"pm{i}", [128, 512]) for i in range(4)])
        ra_ = Ring([sb(f"ra{i}", [128, 512]) for i in range(2)])
        rb_ = Ring([sb(f"rb{i}", [128, 512]) for i in range(2)])
        qo_ = Ring([sb(f"qo{i}", [128, 512], BF16) for i in range(3)])
        fo_ = Ring([sb(f"fo{i}", [128, 512]) for i in range(3)])
        vo_ = Ring([sb(f"vo{i}", [128, 6, 65], BF16) for i in range(2)])
        for vt in vo_.tiles:
            k.op("pool", lambda e, vt=vt: e.memset(vt[:], 1.0), [], [vo_.bufs[vo_.tiles.index(vt)]])
        km = sb("km", [128, 48]); b_km = Buf()
        shT = modT[:, l * 48 + 0:l * 48 + 8]
        def normT(tg):
            hT, b_hT = hT_.next()
            hts[tg] = (hT, b_hT)
            for i in range(4):
                tt = tg * 4 + i
                xt, b_x = xr_.next()
                k.dma("sp", xt[:], dr[xin][tt * 128:(tt + 1) * 128, :], reads=[db[xin]], writes=[b_x])
                st, b_st = st_.next()
                ACT(sq[:], xt[:], AF.Square, [b_x], [b_sq, b_st], accum_out=st[:, 0:1])
                TS(st[:, 1:2], st[:, 0:1], 1.0 / D, EPS, ALU.mult, ALU.add, [b_st], [b_st])
                ACT(st[:, 1:2], st[:, 1:2], AF.Sqrt, [b_st], [b_st])
                k.op("dve", lambda e, st=st: e.reciprocal(out=st[:, 1:2], in_=st[:, 1:2]), [b_st], [b_st])
                xn, b_xn = xn_.next()
                TS(xn[:], xt[:], st[:, 1:2], None, ALU.mult, ALU.bypass, [b_x, b_st], [b_xn])
                pT, b_pT = psT_.next()
                for kk in range(8):
                    TR(pT[:, kk * 128:(kk + 1) * 128], xn[:, kk * 128:(kk + 1) * 128], ident[:], [b_xn, b_ident], [b_pT])
                for kk in range(8):
                    ACT(hT[:, kk, i * 128:(i + 1) * 128], pT[:, kk * 128:(kk + 1) * 128], AF.Identity, [b_pT, b_gs1T, b_modT], [b_hT],
                        scale=gs1T[:, l * 8 + kk:l * 8 + kk + 1], bias=shT[:, kk:kk + 1])
        def proj(tg):
            hT, b_hT = hts.pop(tg)
            tsl = slice(tg * 512, (tg + 1) * 512)
            for which in range(2):
                for c in range(3):
                    base = which * 768 + c * 128
                    pa, b_pa = pm_.next()
                    pb, b_pb = pm_.next()
                    for kk in range(8):
                        MM(pa[:], W[:, kk, base:base + 128], hT[:, kk, :], kk == 0, kk == 7, [b_W, b_hT], [b_pa])
                    for kk in range(8):
                        MM(pb[:], W[:, kk, base + 384:base + 512], hT[:, kk, :], kk == 0, kk == 7, [b_W, b_hT], [b_pb])
                    ra, b_ra = ra_.next()
                    rb, b_rb = rb_.next()
                    TT(ra[:], pa[:], COS[:, tsl], ALU.mult, [b_pa, b_rope], [b_ra])
                    TT(rb[:], pb[:], SINS[:, tsl], ALU.mult, [b_pb, b_rope], [b_rb])
                    qo, b_qo = qo_.next()
                    TT(qo[:], ra[:], rb[:], ALU.add, [b_ra, b_rb], [b_qo])
                    name = ("QT", "KT")[which] + str(l)
                    k.dma("pool", dr[name][c * 128:(c + 1) * 128, tsl], qo[:], reads=[b_qo], writes=[db[name]])
                    if which == 1:
                        k.op("dve", lambda e, qo=qo, c=c, tg=tg: e.tensor_reduce(
                            out=km[:, c * 16 + 2 * tg:c * 16 + 2 * tg + 2], in_=qo[:].rearrange("p (b n) -> p b n", b=2),
                            axis=AX.X, op=ALU.add), [b_qo], [b_km])
            for i in range(4):
                tt = tg * 4 + i
                pa, b_pa = pm_.next()
                for kk in range(8):
                    MM(pa[:, 0:384], hT[:, kk, i * 128:(i + 1) * 128], W[:, kk, 1536:1920], kk == 0, kk == 7, [b_W, b_hT], [b_pa])
                vo, b_vo = vo_.next()
                ACT(vo[:, :, 0:64], pa[:, 0:384].rearrange("p (h d) -> p h d", h=6), AF.Copy, [b_pa], [b_vo])
                k.dma("pool", dr[f"V{l}"][tt * 128:(tt + 1) * 128, :], vo[:].rearrange("p h d -> p (h d)"), reads=[b_vo], writes=[db[f"V{l}"]])
            for j, (name, base, nch) in enumerate((("XR", 1920, 3), ("GG", 2304, 3), ("U", 2688, 2))):
                for c in range(nch):
                    pa, b_pa = pm_.next()
                    for kk in range(8):
                        MM(pa[:], W[:, kk, base + c * 128:base + (c + 1) * 128], hT[:, kk, :], kk == 0, kk == 7, [b_W, b_hT], [b_pa])
                    fo, b_fo = fo_.next()
                    if name == "GG":
                        gelu_tanh(P, fo, b_fo, pa, b_pa, ra_, rb_, ACT, TS, TT)
                    else:
                        ACT(fo[:], pa[:], AF.Copy, [b_pa], [b_fo])
                    k.dma("pool", dr[f"{name}{l}"][c * 128:(c + 1) * 128, tsl], fo[:], reads=[b_fo], writes=[db[f"{name}{l}"]])
        hts = {}
        normT(0)
        normT(1)
        for tg in range(NG):
            if tg + 2 < NG:
                normT(tg + 2)
            proj(tg)
        k.dma("sp", dr[f"KM{l}"], km[:], reads=[b_km], writes=[db[f"KM{l}"]])
        k.barrier()
        k.emit()


def phase_att(P, l, ACT, TS, TT, STT, CP, MM, TR):
    nc, k, dr, db = P.nc, P.k, P.dr, P.db
    with ExitStack() as es:
        sb = lambda name, shape, dt=F32: es.enter_context(nc.sbuf_tensor(P.nm(name), list(shape), dt))
        ps = lambda name, shape, dt=F32: es.enter_context(nc.psum_tensor(P.nm(name), list(shape), dt))
        identf = sb("identf", [128, 128]); identb = sb("identb", [128, 128], BF16); b_c = Buf()
        cb = sb("cb", [128, 4, 512], BF16)
        padm = sb("padm", [128, 16, 16]); pada = sb("pada", [128, 16, 16])
        k.dma("sp", identf[:], dr["ident"], writes=[b_c])
        k.dma("sp", identb[:], dr["identb"], writes=[b_c])
        k.dma("sp", cb[:].rearrange("p a b -> p (a b)"), dr["cbias"], writes=[b_c])
        k.dma("sp", padm[:].rearrange("p a b -> p (a b)"), dr["padm"], writes=[b_c])
        k.dma("sp", pada[:].rearrange("p a b -> p (a b)"), dr["pada"], writes=[b_c])
        vall = sb("vall", [128, NT, 454], BF16); b_v = Buf()
        k.op("pool", lambda e: e.memset(vall[:, :, 390:454], 0.0), [], [b_v])

        def load_v():
            for q4 in range(4):
                k.dma("sp", vall[:, q4 * 8:(q4 + 1) * 8, 0:390], dr[f"V{l}"][q4 * 1024:(q4 + 1) * 1024, :].rearrange("(t p) d -> p t d", p=128),
                      reads=[db[f"V{l}"]], writes=[b_v])
        kta_ = Ring([sb(f"kta{i}", [128, S], BF16) for i in range(2)])
        qta_ = Ring([sb(f"qta{i}", [128, S], BF16) for i in range(2)])
        for t_, b_ in zip(kta_.tiles, kta_.bufs):
            k.op("dve", lambda e, t_=t_: e.memset(t_[64:128, :], 0.0), [], [b_])
            k.dma("sp", t_[64:80, :], dr["ohk"], writes=[b_])
        for t_, b_ in zip(qta_.tiles, qta_.bufs):
            k.op("pool", lambda e, t_=t_: e.memset(t_[64:128, :], 0.0), [], [b_])
        kmf_ = Ring([sb(f"kmf{i}", [64, 16]) for i in range(2)])
        kmb_ = Ring([sb(f"kmb{i}", [64, 16], BF16) for i in range(2)])
        padm32 = sb("padm32", [128, NT, 16]); pada32 = sb("pada32", [128, NT, 16])
        k.dma("sp", padm32[:].rearrange("p a b -> p (a b)"), dr["padm32"], writes=[b_c])
        k.dma("sp", pada32[:].rearrange("p a b -> p (a b)"), dr["pada32"], writes=[b_c])
        g80A = sb("g80A", [128, NT, 80]); b_g80 = Buf()
        k.op("pool", lambda e: e.memset(g80A[:].rearrange("p a b -> p (a b)"), 0.0), [], [b_g80])
        gsA = sb("gsA", [128, NT, 16]); b_gs = Buf()
        t8A = sb("t8A", [128, NT, 8]); b_t8 = Buf()
        pt_ = Ring([sb(f"pt{i}", [128, 512], BF16) for i in range(4)])
        rc_ = Ring([sb(f"rc{i}", [128, 4]) for i in range(4)])
        ot_ = Ring([sb(f"ot{i}", [128, 512]) for i in range(2)])
        oall = sb("oall", [128, NT, ATT_W]); b_oall = Buf()
        pg_ = Ring([ps(f"pg{i}", [128, 512]) for i in range(2)])
        sp_ = Ring([ps(f"sp{i}", [128, 512]) for i in range(3)])
        po_ = Ring([ps(f"po{i}", [128, 512]) for i in range(2)])
        ptr_ = Ring([ps(f"ptr{i}", [128, 512]) for i in range(1)])
        heads = {}

        def load_and_gate(h):
            kta, b_kta = kta_.next()
            qta, b_qta = qta_.next()
            heads[h] = (kta, b_kta, qta, b_qta)
            k.dma("sp", kta[0:64, :], dr[f"KT{l}"][h * 64:(h + 1) * 64, :], reads=[db[f"KT{l}"]], writes=[b_kta])
            k.dma("sp", qta[0:64, :], dr[f"QT{l}"][h * 64:(h + 1) * 64, :], reads=[db[f"QT{l}"]], writes=[b_qta])
            kmf, b_kmf = kmf_.next()
            kmb, b_kmb = kmb_.next()
            c, hb = h // 2, (h % 2) * 64
            k.dma("sp", kmf[:], dr[f"KM{l}"][hb:hb + 64, c * 16:(c + 1) * 16], reads=[db[f"KM{l}"]], writes=[b_kmf])
            CP(kmb[:], kmf[:], [b_kmf], [b_kmb])
            pg, b_pg = pg_.next()
            for qt in range(NT):
                MM(pg[:, qt * 16:(qt + 1) * 16], qta[0:64, qt * 128:(qt + 1) * 128], kmb[:, :], True, True, [b_qta, b_kmb], [b_pg])
            gflat = gsA[:].rearrange("p a b -> p (a b)")
            TT(gflat, pg[:], padm32[:].rearrange("p a b -> p (a b)"), ALU.mult, [b_pg, b_c], [b_gs])
            TT(gflat, gflat, pada32[:].rearrange("p a b -> p (a b)"), ALU.add, [b_gs, b_c], [b_gs])
            for qt in range(NT):
                k.op("dve", lambda e, qt=qt: e.max(out=t8A[:, qt, :], in_=gsA[:, qt, :]), [b_gs], [b_t8])
            for qt in range(NT):
                TS(g80A[:, qt, 64:80], gsA[:, qt, :], t8A[:, qt, 4:5], -1.0, ALU.is_gt, ALU.add, [b_gs, b_t8], [b_g80])
            for q4 in range(NG):
                pg2, b_pg2 = pg_.next()
                for i in range(4):
                    TR(pg2[0:80, i * 128:(i + 1) * 128], g80A[:, q4 * 4 + i, :], identf[:], [b_g80, b_c], [b_pg2])
                ACT(qta[64:80, q4 * 512:(q4 + 1) * 512], pg2[64:80, :], AF.Copy, [b_pg2], [b_qta], scale=-NEGB)

        def attend(h):
            kta, b_kta, qta, b_qta = heads[h]
            steps = [(g, kt) for g in range(NG) for kt in range(4 * g + 4)]
            spd = {}
            posd = {}

            def score(i):
                g, kt = steps[i]
                sp, b_sp = sp_.next()
                diag = kt >= 4 * g
                MM(sp[:], kta[:, kt * 128:(kt + 1) * 128], qta[:, g * 512:(g + 1) * 512], True, not diag,
                   [b_kta, b_qta], [b_sp])
                if diag:
                    MM(sp[:], identb[:], cb[:, kt - 4 * g, :], False, True, [b_c], [b_sp])
                spd[i] = (sp, b_sp)

            def rest(i):
                g, kt = steps[i]
                if kt == 0:
                    posd[g] = po_.next()
                po, b_po = posd[g]
                sp, b_sp = spd.pop(i)
                pt, b_pt = pt_.next()
                ACT(pt[:], sp[:], AF.Exp, [b_sp], [b_pt], scale=0.125)
                MM(po[:, :], vall[:, kt, h * 65:h * 65 + 128], pt[:], kt == 0, kt == 4 * g + 3, [b_pt, b_v], [b_po])
                if kt == 4 * g + 3:
                    fins.append((i + 2, (lambda g=g, po=po, b_po=b_po: finalize(g, po, b_po))))

            def finalize(g, po, b_po):
                ot, b_ot = ot_.next()
                CP(ot[0:65, :], po[0:65, :], [b_po], [b_ot])
                ptr, b_ptr = ptr_.next()
                for qg in range(4):
                    TR(ptr[:, qg * 65:(qg + 1) * 65], ot[0:65, qg * 128:(qg + 1) * 128], identf[0:65, 0:65], [b_ot, b_c], [b_ptr])
                rc, b_rc = rc_.next()
                pv = ptr[:, 0:260].rearrange("p (a d) -> p a d", a=4)
                k.op("dve", lambda e, rc=rc, pv=pv: e.reciprocal(out=rc[:, 0:4], in_=pv[:, :, 64]), [b_ptr], [b_rc])
                for qg in range(4):
                    qt = 4 * g + qg
                    TS(oall[:, qt, h * 64:(h + 1) * 64], ptr[:, qg * 65:qg * 65 + 64], rc[:, qg:qg + 1], None, ALU.mult, ALU.bypass,
                       [b_ptr, b_rc], [b_oall])

            fins = []
            score(0)
            score(1)
            for i in range(len(steps)):
                if i + 2 < len(steps):
                    score(i + 2)
                rest(i)
                while fins and fins[0][0] <= i:
                    fins.pop(0)[1]()
            while fins:
                fins.pop(0)[1]()

        load_and_gate(0)
        load_v()
        for h in range(6):
            if h + 1 < 6:
                load_and_gate(h + 1)
            attend(h)
        for q4 in range(4):
            k.dma("sp", dr[f"OATT{l}"][q4 * 1024:(q4 + 1) * 1024, :].rearrange("(t p) d -> p t d", p=128), oall[:, q4 * 8:(q4 + 1) * 8, :],
                  reads=[b_oall], writes=[db[f"OATT{l}"]])
        k.barrier()
        k.emit()


def phase_lru(P, l, ACT, TS, TT, STT, CP, MM, TR):
    nc, k, dr, db = P.nc, P.k, P.dr, P.db
    with ExitStack() as es:
        sb = lambda name, shape, dt=F32: es.enter_context(nc.sbuf_tensor(P.nm(name), list(shape), dt))
        ps = lambda name, shape, dt=F32: es.enter_context(nc.psum_tensor(P.nm(name), list(shape), dt))
        cw = sb("cw", [128, DEPTH * 12]); vec = sb("lvec", [128, DEPTH * 12]); b_par = Buf()
        wa = sb("wa", [128, DEPTH * 3 * 128]); wx = sb("wx", [128, DEPTH * 3 * 128])
        k.dma("sp", cw[:], dr["lru_cw"], writes=[b_par])
        k.dma("sp", vec[:], dr["lru_vec"], writes=[b_par])
        k.dma("sp", wa[:], dr["lru_wa"], writes=[b_par])
        k.dma("sp", wx[:], dr["lru_wx"], writes=[b_par])
        ones = sb("ones1", [128, 1]); b_ones = Buf()
        k.op("dve", lambda e: e.memset(ones[:], 1.0), [], [b_ones])
        cc = sb("cc", [128, 6]); b_cc = Buf()
        xrp = sb("xrp", [128, S + 4]); b_xrp = Buf()
        xc = sb("xc", [128, S]); b_xc = Buf()
        rr = sb("rr", [128, S]); b_rr = Buf()
        ii = sb("ii", [128, S]); b_ii = Buf()
        a2 = sb("a2", [128, S]); b_a2 = Buf()
        gg = sb("gg", [128, S]); b_gg = Buf()
        ss = sb("ss", [128, NT]); b_ss = Buf()
        pm_ = Ring([ps(f"pm{i}", [128, 512]) for i in range(4)])
        pss = ps("pss", [128, NT]); b_pss = Buf()
        if l == 0:
            P.ada_layer(1, sb, ps)
        k.op("dve", lambda e: e.memset(xrp[:, 0:4], 0.0), [], [b_xrp])
        for c in range(3):
            vb = l * 12 + c * 4
            ACT(cc[:, 2 * c:2 * c + 1], vec[:, vb + 3:vb + 4], AF.Exp, [b_par], [b_cc], scale=-1.0)
            ACT(cc[:, 2 * c:2 * c + 1], cc[:, 2 * c:2 * c + 1], AF.Ln, [b_cc], [b_cc], bias=1.0)
            TS(cc[:, 2 * c + 1:2 * c + 2], cc[:, 2 * c:2 * c + 1], -16.0, None, ALU.mult, ALU.bypass, [b_cc], [b_cc])
            TS(cc[:, 2 * c:2 * c + 1], cc[:, 2 * c:2 * c + 1], -8.0, None, ALU.mult, ALU.bypass, [b_cc], [b_cc])
            k.dma("sp", xrp[:, 4:S + 4], dr[f"XR{l}"][c * 128:(c + 1) * 128, :], reads=[db[f"XR{l}"]], writes=[b_xrp])
            k.dma("sp", gg[:], dr[f"GG{l}"][c * 128:(c + 1) * 128, :], reads=[db[f"GG{l}"]], writes=[b_gg])
            wb = l * 12 + c * 4
            TS(xc[:], xrp[:, 1:S + 1], cw[:, wb:wb + 1], vec[:, vb:vb + 1], ALU.mult, ALU.add, [b_xrp, b_par], [b_xc])
            for j in range(1, 4):
                STT(xc[:], xrp[:, 1 + j:S + 1 + j], cw[:, wb + j:wb + j + 1], xc[:], ALU.mult, ALU.add, [b_xrp, b_par, b_xc], [b_xc])
            wof = (l * 3 + c) * 128
            for tg in range(NG):
                tsl = slice(tg * 512, (tg + 1) * 512)
                pa, b_pa = pm_.next()
                MM(pa[:], wa[:, wof:wof + 128], xc[:, tsl], True, True, [b_par, b_xc], [b_pa])
                ACT(rr[:, tsl], pa[:], AF.Sigmoid, [b_pa, b_par], [b_rr], bias=vec[:, vb + 1:vb + 2])
                pb, b_pb = pm_.next()
                MM(pb[:], wx[:, wof:wof + 128], xc[:, tsl], True, True, [b_par, b_xc], [b_pb])
                ACT(ii[:, tsl], pb[:], AF.Sigmoid, [b_pb, b_par], [b_ii], bias=vec[:, vb + 2:vb + 3])
            ACT(a2[:], rr[:], AF.Exp, [b_rr, b_cc], [b_a2], scale=cc[:, 2 * c + 1:2 * c + 2])
            ACT(rr[:], rr[:], AF.Exp, [b_rr, b_cc], [b_rr], scale=cc[:, 2 * c:2 * c + 1])
            TS(a2[:], a2[:], 1.0, 0.0, ALU.subtract, ALU.min, [b_a2], [b_a2])
            ACT(a2[:], a2[:], AF.Sqrt, [b_a2], [b_a2], scale=-1.0)
            TT(ii[:], ii[:], xc[:], ALU.mult, [b_ii, b_xc], [b_ii])
            TT(ii[:], ii[:], a2[:], ALU.mult, [b_ii, b_a2], [b_ii])
            k.op("dve", lambda e: e.tensor_tensor_scan(out=xc[:], data0=rr[:], data1=ii[:], initial=0.0, op0=ALU.mult, op1=ALU.add),
                 [b_rr, b_ii], [b_xc])
            TT(gg[:], gg[:], xc[:], ALU.mult, [b_gg, b_xc], [b_gg])
            k.dma("sp", dr[f"OLRU{l}"][c * 128:(c + 1) * 128, :], gg[:], reads=[b_gg], writes=[db[f"OLRU{l}"]])
            ACT(a2[:], gg[:], AF.Square, [b_gg], [b_a2])
            for tt in range(NT):
                MM(pss[:, tt:tt + 1], a2[:, tt * 128:(tt + 1) * 128], ones[:, 0:1], True, True, [b_a2, b_ones], [b_pss])
            if c == 0:
                CP(ss[:], pss[:], [b_pss], [b_ss])
            else:
                TT(ss[:], ss[:], pss[:], ALU.add, [b_ss, b_pss], [b_ss])
        k.dma("sp", dr[f"SSL{l}"], ss[:], reads=[b_ss], writes=[db[f"SSL{l}"]])
        k.barrier()
        k.emit()


def phase_s5(P, l, ACT, TS, TT, STT, CP, MM, TR):
    nc, k, dr, db = P.nc, P.k, P.dr, P.db
    TWO_PI = 2.0 * math.pi
    with ExitStack() as es:
        sb = lambda name, shape, dt=F32: es.enter_context(nc.sbuf_tensor(P.nm(name), list(shape), dt))
        ps = lambda name, shape, dt=F32: es.enter_context(nc.psum_tensor(P.nm(name), list(shape), dt))
        b_par = Buf()
        s5p = sb("s5p", [128, 3, 16]); k.dma("sp", s5p[:].rearrange("p a g -> p (a g)"), dr["s5p"][:, l * 48:(l + 1) * 48], writes=[b_par])
        cc = sb("s5c", [128, 2, 16, 16]); k.dma("sp", cc[:].rearrange("p a g h -> p (a g h)"), dr["s5_c"][:, l * 512:(l + 1) * 512], writes=[b_par])
        dg = sb("s5dg", [128, 4]); k.dma("sp", dg[:], dr["s5_dg"][:, l * 4:(l + 1) * 4], writes=[b_par])
        tbm = sb("tbm", [128, 4]); k.dma("sp", tbm[:], dr["tbm"], writes=[b_par])
        tv = sb("tv", [128, S]); k.dma("sp", tv[:], dr["tvals"], writes=[b_par])
        B1 = sb("B1", [128, 16, 128], BF16); B2 = sb("B2", [128, 16, 128], BF16); b_B = Buf()
        k.dma("pool", B1[:].rearrange("p g m -> p (g m)"), dr["s5_b1"][:, l * 2048:(l + 1) * 2048], writes=[b_B])
        k.dma("pool", B2[:].rearrange("p g m -> p (g m)"), dr["s5_b2"][:, l * 2048:(l + 1) * 2048], writes=[b_B])
        TS(B2[:, :, 64:128], B2[:, :, 64:128], -1.0, None, ALU.mult, ALU.bypass, [b_B], [b_B])
        gw = sb("gw", [128, 2, 256], BF16); b_gw = Buf()
        k.dma("pool", gw[:], dr["s5_gw"][l].rearrange("(c p) n -> p c n", p=128), writes=[b_gw])
        ub = sb("ub", [128, 2, S], BF16); b_u = Buf()
        u32_ = Ring([sb(f"u32{i}", [128, 2, 512]) for i in range(2)])
        for ch in range(2):
            k.dma("pool", ub[:, ch, :], dr[f"U{l}"][ch * 128:(ch + 1) * 128, :], reads=[db[f"U{l}"]], writes=[b_u], max_dma_last_dim=2048)
        sm = sb("sm", [128, 16, 16]); b_sm = Buf()
        R = lambda i: sm[:, i, :]
        lr, li, ldt = s5p[:, 0, :], s5p[:, 1, :], s5p[:, 2, :]
        DT, MAG, TH, UT, F1, SN, CS, DEN, NR, CORE, COIM, NCOIM, TMP, TMP2 = (R(i) for i in range(14))
        bs = [b_sm, b_par]
        ACT(DT, ldt, AF.Exp, bs, [b_sm])
        TT(TMP, lr, DT, ALU.mult, bs, [b_sm])
        ACT(MAG, TMP, AF.Exp, bs, [b_sm])
        TT(TH, li, DT, ALU.mult, bs, [b_sm])
        TS(UT, TH, 1.0 / TWO_PI, None, ALU.mult, ALU.bypass, bs, [b_sm])
        for dst, off in ((SN, 0.0), (CS, 0.25)):
            TS(F1, UT, off, None, ALU.add, ALU.bypass, bs, [b_sm])
            TS(TMP, F1, MAGIC, None, ALU.add, ALU.bypass, bs, [b_sm])
            TS(TMP, TMP, MAGIC, None, ALU.subtract, ALU.bypass, bs, [b_sm])
            TT(F1, F1, TMP, ALU.subtract, bs, [b_sm])
            ACT(dst, F1, AF.Sin, bs, [b_sm], scale=TWO_PI)
        TT(SN, SN, MAG, ALU.mult, bs, [b_sm])
        TT(CS, CS, MAG, ALU.mult, bs, [b_sm])
        TS(NR, CS, -1.0, None, ALU.add, ALU.bypass, bs, [b_sm])
        TT(DEN, lr, lr, ALU.mult, bs, [b_sm])
        TT(TMP, li, li, ALU.mult, bs, [b_sm])
        TT(DEN, DEN, TMP, ALU.add, bs, [b_sm])
        k.op("dve", lambda e: e.reciprocal(out=DEN, in_=DEN), bs, [b_sm])
        TT(TMP, NR, lr, ALU.mult, bs, [b_sm])
        TT(TMP2, SN, li, ALU.mult, bs, [b_sm])
        TT(TMP, TMP, TMP2, ALU.add, bs, [b_sm])
        TT(CORE, TMP, DEN, ALU.mult, bs, [b_sm])
        TT(TMP, SN, lr, ALU.mult, bs, [b_sm])
        TT(TMP2, NR, li, ALU.mult, bs, [b_sm])
        TT(TMP, TMP, TMP2, ALU.subtract, bs, [b_sm])
        TT(COIM, TMP, DEN, ALU.mult, bs, [b_sm])
        TS(NCOIM, COIM, -1.0, None, ALU.mult, ALU.bypass, bs, [b_sm])
        Lp = sb("Lp", [128, 16, 2, 128], BF16); b_Lp = Buf()
        k.op("pool", lambda e: e.memset(Lp[:].rearrange("p g a m -> p (g a m)"), 0.0), [], [b_Lp])
        cw_ = sb("cw_", [128, 16, 4, 16]); b_cwg = [Buf() for _ in range(16)]
        b_Lpg = [Buf() for _ in range(16)]
        for g in range(16):
            b_Lpg[g].w = b_Lp.w
        gsl = lambda g: slice(16 * (g % 8), 16 * (g % 8) + 16)
        G16 = range(16)
        for g in G16:
            TS(cw_[:, g, 0, :], cc[:, 0, g, :], CORE[:, g:g + 1], None, ALU.mult, ALU.bypass, bs, [b_cwg[g]])
        for g in G16:
            TS(cw_[:, g, 1, :], cc[:, 0, g, :], COIM[:, g:g + 1], None, ALU.mult, ALU.bypass, bs, [b_cwg[g]])
        for g in G16:
            STT(cw_[:, g, 2, :], cc[:, 1, g, :], NCOIM[:, g:g + 1], cw_[:, g, 0, :], ALU.mult, ALU.add, bs + [b_cwg[g]], [b_cwg[g]])
        for g in G16:
            STT(cw_[:, g, 3, :], cc[:, 1, g, :], CORE[:, g:g + 1], cw_[:, g, 1, :], ALU.mult, ALU.add, bs + [b_cwg[g]], [b_cwg[g]])
        for g in G16:
            TS(cw_[:, g, 0, :], cw_[:, g, 2, :], tbm[:, 0:1], None, ALU.mult, ALU.bypass, bs + [b_cwg[g]], [b_cwg[g]])
        for g in G16:
            TS(cw_[:, g, 1, :], cw_[:, g, 3, :], tbm[:, 1:2], None, ALU.mult, ALU.bypass, bs + [b_cwg[g]], [b_cwg[g]])
        for g in G16:
            STT(Lp[:, g, 0, gsl(g)], cw_[:, g, 3, :], tbm[:, 3:4], cw_[:, g, 0, :], ALU.mult, ALU.add, bs + [b_cwg[g]], [b_Lpg[g]])
        for g in G16:
            STT(Lp[:, g, 1, gsl(g)], cw_[:, g, 2, :], tbm[:, 3:4], cw_[:, g, 1, :], ALU.mult, ALU.add, bs + [b_cwg[g]], [b_Lpg[g]])
        ones = sb("ones5", [128, 512]); b_ones = Buf()
        k.op("pool", lambda e: e.memset(ones[:], 1.0), [], [b_ones])
        wl = sb("wl", [128, 16]); b_wl = [Buf() for _ in range(16)]
        RD = 4
        y1_ = Ring([sb(f"y1{i}", [128, 512]) for i in range(RD)])
        y2_ = Ring([sb(f"y2{i}", [128, 512]) for i in range(RD)])
        gsr_ = Ring([sb(f"gsr{i}", [128, 512]) for i in range(RD)])
        ab_ = Ring([sb(f"ab{i}", [128, 512]) for i in range(RD)])
        sn_ = Ring([sb(f"sn{i}", [128, 512]) for i in range(RD)])
        cs_ = Ring([sb(f"cs{i}", [128, 512]) for i in range(RD)])
        mt_ = Ring([sb(f"mt{i}", [128, 512]) for i in range(RD)])
        ta_ = Ring([sb(f"ta{i}", [128, 512]) for i in range(RD)])
        tb_ = Ring([sb(f"tb{i}", [128, 512]) for i in range(RD)])
        w_ = Ring([sb(f"w{i}", [128, 512]) for i in range(RD)])
        z1_ = Ring([sb(f"z1{i}", [128, 512], BF16) for i in range(RD)])
        z2_ = Ring([sb(f"z2{i}", [128, 512], BF16) for i in range(RD)])
        yv_ = Ring([sb(f"yv{i}", [128, 2, 512]) for i in range(2)])
        ygb_ = Ring([sb(f"ygb{i}", [128, 2, 512], BF16) for i in range(2)])
        og_ = Ring([sb(f"og{i}", [128, 512]) for i in range(2)])
        ra_ = Ring([sb(f"gra{i}", [128, 512]) for i in range(2)])
        rb_ = Ring([sb(f"grb{i}", [128, 512]) for i in range(2)])
        ones1 = sb("ones51", [128, 1]); b_o1 = Buf()
        k.op("pool", lambda e: e.memset(ones1[:], 1.0), [], [b_o1])
        ss = sb("ss5", [128, NT]); b_ss = Buf()
        ps1_ = Ring([ps(f"ps1{i}", [128, 512]) for i in range(2)])
        ps2_ = Ring([ps(f"ps2{i}", [128, 512]) for i in range(2)])
        py = [ps(f"py{i}", [128, 512]) for i in range(2)]; b_py = [Buf(), Buf()]
        pz_ = Ring([ps(f"pz{i}", [128, 512]) for i in range(1)])
        pss = ps("pss5", [128, 512]); b_pss = Buf()
        pend = {}
        tabs = {}

        def bu(cc_, g_):
            p1_, b_p1_ = ps1_.next()
            p2_, b_p2_ = ps2_.next()
            sl_ = slice(cc_ * 512, (cc_ + 1) * 512)
            MM(p1_[:], B1[:, g_, :], ub[:, g_ // 8, sl_], True, True, [b_B, b_u], [b_p1_])
            MM(p2_[:], B2[:, g_, :], ub[:, g_ // 8, sl_], True, True, [b_B, b_u], [b_p2_])
            pend[(cc_, g_)] = (p1_, b_p1_, p2_, b_p2_)

        def tables(cc_, g_):
            sl_ = slice(cc_ * 512, (cc_ + 1) * 512)
            ug = UT[:, g_:g_ + 1]
            y1, b_y1 = y1_.next()
            y2, b_y2 = y2_.next()
            gsr, b_gsr = gsr_.next()
            TS(y1[:], tv[:, sl_], ug, MAGIC, ALU.mult, ALU.add, [b_par, b_sm], [b_y1], eng="pool")
            TS(y2[:], tv[:, sl_], ug, 0.0, ALU.mult, ALU.add, [b_par, b_sm], [b_y2], eng="pool")
            TS(y1[:], y1[:], -MAGIC, 1.0, ALU.add, ALU.mult, [b_y1], [b_y1], eng="pool")
            TT(gsr[:], y2[:], y1[:], ALU.subtract, [b_y1, b_y2], [b_gsr], eng="pool")
            sn, b_sn = sn_.next()
            cs, b_cs = cs_.next()
            ab, b_ab = ab_.next()
            mt, b_mt = mt_.next()
            ACT(ab[:], gsr[:], AF.Abs, [b_gsr], [b_ab])
            ACT(sn[:], gsr[:], AF.Sin, [b_gsr], [b_sn], scale=TWO_PI)
            ACT(mt[:], ones[:], AF.Identity, [b_ones, b_sm], [b_mt], scale=0.0, bias=MAG[:, g_:g_ + 1])
            ACT(cs[:], ab[:], AF.Sin, [b_ab, b_par], [b_cs], scale=-TWO_PI, bias=tbm[:, 2:3])
            tabs[(cc_, g_)] = (sn, b_sn, cs, b_cs, mt, b_mt)

        order = [(c_, g_) for c_ in range(NG) for g_ in range(16)]
        LA = 2
        for i_ in range(LA):
            tables(*order[i_])
        bu(*order[0])
        bu(*order[1])
        pend_tail = []
        for c in range(NG):
            tsl = slice(c * 512, (c + 1) * 512)
            yv, b_yv = yv_.next()
            ygb, b_ygb = ygb_.next()
            u32, b_u32 = u32_.next()
            k.dma("sp", u32[:], dr[f"U{l}"][:, tsl].rearrange("(c p) n -> p c n", p=128), reads=[db[f"U{l}"]], writes=[b_u32])
            for gp in range(0, 16, 2):
                ch = gp // 8
                st_ = []
                for g in (gp, gp + 1):
                    idx = c * 16 + g
                    if idx + LA < len(order):
                        tables(*order[idx + LA])
                    p1, b_p1, p2, b_p2 = pend.pop((c, g))
                    sn, b_sn, cs, b_cs, mt, b_mt = tabs.pop((c, g))
                    ta, b_ta = ta_.next()
                    tb, b_tb = tb_.next()
                    w, b_w = w_.next()
                    z1, b_z1 = z1_.next()
                    z2, b_z2 = z2_.next()
                    st_.append(dict(g=g, p1=p1, b_p1=b_p1, p2=p2, b_p2=b_p2, sn=sn, b_sn=b_sn, cs=cs, b_cs=b_cs, mt=mt, b_mt=b_mt,
                                    ta=ta, b_ta=b_ta, tb=tb, b_tb=b_tb, w=w, b_w=b_w, z1=z1, b_z1=b_z1, z2=z2, b_z2=b_z2))
                for s_ in st_:
                    TT(s_["ta"][:], s_["p1"][:], s_["cs"][:], ALU.mult, [s_["b_p1"], s_["b_cs"]], [s_["b_ta"]])
                for s_ in st_:
                    TT(s_["tb"][:], s_["p2"][:], s_["sn"][:], ALU.mult, [s_["b_p2"], s_["b_sn"]], [s_["b_tb"]])
                for s_ in st_:
                    TT(s_["ta"][:], s_["ta"][:], s_["tb"][:], ALU.add, [s_["b_ta"], s_["b_tb"]], [s_["b_ta"]])
                for dg_ in (2, 3):
                    if c * 16 + gp + dg_ < len(order):
                        bu(*order[c * 16 + gp + dg_])
                for s_ in st_:
                    g = s_["g"]
                    init = 0.0 if c == 0 else wl[:, g:g + 1]
                    k.op("dve", lambda e, s_=s_, init=init: e.tensor_tensor_scan(
                        out=s_["w"][:], data0=s_["mt"][:], data1=s_["ta"][:], initial=init, op0=ALU.mult, op1=ALU.add),
                        [s_["b_mt"], s_["b_ta"], b_wl[g]], [s_["b_w"]])
                for s_ in st_:
                    g = s_["g"]
                    ACT(wl[:, g:g + 1], s_["w"][:, 511:512], AF.Copy, [s_["b_w"]], [b_wl[g]])
                    TT(s_["z1"][:], s_["w"][:], s_["cs"][:], ALU.mult, [s_["b_w"], s_["b_cs"]], [s_["b_z1"]])
                for s_ in st_:
                    TT(s_["z2"][:], s_["w"][:], s_["sn"][:], ALU.mult, [s_["b_w"], s_["b_sn"]], [s_["b_z2"]])
                for s_ in st_:
                    g = s_["g"]
                    MM(py[ch][:], Lp[:, g, 0, :], s_["z1"][:], g % 8 == 0, False, [b_Lpg[g], s_["b_z1"]], [b_py[ch]])
                    MM(py[ch][:], Lp[:, g, 1, :], s_["z2"][:], False, g % 8 == 7, [b_Lpg[g], s_["b_z2"]], [b_py[ch]])
                    if g % 8 == 7:
                        STT(yv[:, ch, :], u32[:, ch, :], dg[:, ch:ch + 1], py[ch][:], ALU.mult, ALU.add, [b_u32, b_par, b_py[ch]], [b_yv])
                if gp == 4 and pend_tail:
                    pend_tail.pop(0)()
            def tail(c, yv, b_yv, ygb, b_ygb, tsl):
                for ch in range(2):
                    class _V:
                        def __init__(s_, ap): s_.ap = ap
                        def __getitem__(s_, key): return s_.ap
                    yview = _V(yv[:, ch, :])
                    gelu_tanh(P, yview, b_yv, yview, b_yv, ra_, rb_, ACT, TS, TT)
                    CP(ygb[:, ch, :], yv[:, ch, :], [b_yv], [b_ygb], eng="pool")
                for oc in range(2):
                    pz, b_pz = pz_.next()
                    for kc in range(2):
                        MM(pz[:], gw[:, kc, oc * 128:(oc + 1) * 128], ygb[:, kc, :], kc == 0, kc == 1, [b_gw, b_ygb], [b_pz])
                    og, b_og = og_.next()
                    ACT(og[:], pz[:], AF.Sigmoid, [b_pz, b_par], [b_og], bias=dg[:, 2 + oc:3 + oc])
                    TT(og[:], og[:], yv[:, oc, :], ALU.mult, [b_og, b_yv], [b_og])
                    k.dma("sp", dr[f"OS5{l}"][oc * 128:(oc + 1) * 128, tsl], og[:], reads=[b_og], writes=[db[f"OS5{l}"]])
                    sq, b_sq = ra_.next()
                    ACT(sq[:], og[:], AF.Square, [b_og], [b_sq])
                    for i in range(4):
                        MM(pss[:, oc * 4 + i:oc * 4 + i + 1], sq[:, i * 128:(i + 1) * 128], ones1[:, 0:1], True, True, [b_sq, b_o1], [b_pss])
                CP(ss[:, c * 4:(c + 1) * 4], pss[:, 0:4], [b_pss], [b_ss])
                TT(ss[:, c * 4:(c + 1) * 4], ss[:, c * 4:(c + 1) * 4], pss[:, 4:8], ALU.add, [b_pss, b_ss], [b_ss])
            pend_tail.append(lambda c=c, yv=yv, b_yv=b_yv, ygb=ygb, b_ygb=b_ygb, tsl=tsl, tail=tail: tail(c, yv, b_yv, ygb, b_ygb, tsl))
        while pend_tail:
            pend_tail.pop(0)()
        k.dma("sp", dr[f"SS5{l}"], ss[:], reads=[b_ss], writes=[db[f"SS5{l}"]])
        k.barrier()
        k.emit()


def phase_out(P, l, G, ACT, TS, TT, STT, CP, MM, TR):
    nc, k, dr, db = P.nc, P.k, P.dr, P.db
    ident, b_ident = G["ident"]
    modT, b_modT = G["modT"]
    gs2T, b_gs2T = G["gs2T"]
    xin = "x" if l == 0 else f"XB{l - 1}"
    moe = (l % 2 == 1)
    with ExitStack() as es:
        sb = lambda name, shape, dt=F32: es.enter_context(nc.sbuf_tensor(P.nm(name), list(shape), dt))
        ps = lambda name, shape, dt=F32: es.enter_context(nc.psum_tensor(P.nm(name), list(shape), dt))
        gbc = sb("gbc", [128, D]); b_gbc = Buf()
        k.dma("sp", gbc[:], dr["GBC"][:, (l * 2) * D:(l * 2 + 1) * D], reads=[db["GBC"]], writes=[b_gbc])
        Wo = sb("Wo", [128, 8, D], BF16); b_W = Buf()
        for kk in range(8):
            k.dma("pool", Wo[:, kk, :], dr["w_out"][l, kk * 128:(kk + 1) * 128, :], writes=[b_W], max_dma_last_dim=2048)
        b_par = Buf()
        gT = sb("gT", [128, 8]); k.dma("sp", gT[:], dr["gainT"][:, l * 8:(l + 1) * 8], writes=[b_par])
        ssl = sb("ssl", [128, NT]); k.dma("sp", ssl[:], dr[f"SSL{l}"], reads=[db[f"SSL{l}"]], writes=[b_par])
        ss5 = sb("ss5o", [128, NT]); k.dma("sp", ss5[:], dr[f"SS5{l}"], reads=[db[f"SS5{l}"]], writes=[b_par])
        rw32 = sb("rw32", [128, 8, 8]); k.dma("sp", rw32[:].rearrange("p a b -> p (a b)"), dr["router_w"], writes=[b_par])
        rwo = sb("rwo", [128, NT, 18]); b_rwo = Buf()
        oa_ = Ring([sb(f"oa{i}", [128, ATT_W]) for i in range(4)])
        of_ = Ring([sb(f"of{i}", [128, 5, 128]) for i in range(4)])
        oT_ = Ring([sb(f"oT{i}", [128, 8, 128], BF16) for i in range(4)])
        xt_ = Ring([sb(f"xto{i}", [128, D]) for i in range(4)])
        xn_ = Ring([sb(f"xno{i}", [128, D]) for i in range(3)])
        tm_ = Ring([sb(f"tmo{i}", [128, 512]) for i in range(4)])
        xw_ = Ring([sb(f"xwo{i}", [128, D]) for i in range(4)])
        sq = sb("sqo", [128, D]); b_sq = Buf()
        st_ = Ring([sb(f"sto{i}", [128, 8]) for i in range(6)])
        h2_ = Ring([sb(f"h2o{i}", [128, 8, 128], BF16) for i in range(3)])
        h32_ = Ring([sb(f"h32o{i}", [128, 8, 128]) for i in range(3)])
        lg_ = Ring([sb(f"lgo{i}", [128, 8]) for i in range(2)])
        t8_ = Ring([sb(f"t8o{i}", [128, 8]) for i in range(2)])
        wv_ = Ring([sb(f"wvo{i}", [128, 4]) for i in range(2)])
        m1_ = Ring([sb(f"m1o{i}", [128, 8]) for i in range(2)])
        py_ = Ring([ps(f"pyo{i}", [128, 512]) for i in range(4)])
        pta_ = Ring([ps(f"ptao{i}", [128, 512]) for i in range(1)])
        ptn_ = Ring([ps(f"ptno{i}", [128, 512]) for i in range(2)])
        plg_ = Ring([ps(f"plgo{i}", [128, 512]) for i in range(1)])
        sh2 = modT[:, l * 48 + 24:l * 48 + 32]
        def stageA(tt):
            tks = slice(tt * 128, (tt + 1) * 128)
            oa, b_oa = oa_.next()
            k.dma("sp", oa[:], dr[f"OATT{l}"][tks, :], reads=[db[f"OATT{l}"]], writes=[b_oa])
            of, b_of = of_.next()
            k.dma("sp", of[:, 0:3, :], dr[f"OLRU{l}"][:, tks].rearrange("(c p) n -> p c n", p=128), reads=[db[f"OLRU{l}"]], writes=[b_of])
            k.dma("sp", of[:, 3:5, :], dr[f"OS5{l}"][:, tks].rearrange("(c p) n -> p c n", p=128), reads=[db[f"OS5{l}"]], writes=[b_of])
            xt, b_x = xt_.next()
            k.dma("sp", xt[:], dr[xin][tks, :], reads=[db[xin]], writes=[b_x])
            st, b_st = st_.next()
            ACT(sq[:, 0:ATT_W], oa[:], AF.Square, [b_oa], [b_sq, b_st], accum_out=st[:, 0:1])
            CP(st[:, 1:2], ssl[:, tt:tt + 1], [b_par, b_st], [b_st])
            TS(st[:, 0:2], st[:, 0:2], 1.0 / ATT_W, EPS, ALU.mult, ALU.add, [b_st], [b_st])
            TS(st[:, 2:3], ss5[:, tt:tt + 1], 1.0 / S5_W, EPS, ALU.mult, ALU.add, [b_par, b_st], [b_st])
            ACT(st[:, 0:3], st[:, 0:3], AF.Sqrt, [b_st], [b_st])
            k.op("dve", lambda e, st=st: e.reciprocal(out=st[:, 0:3], in_=st[:, 0:3]), [b_st], [b_st])
            oT, b_oT = oT_.next()
            pta, b_pta = pta_.next()
            for c in range(3):
                TR(pta[:, c * 128:(c + 1) * 128], oa[:, c * 128:(c + 1) * 128], ident[:], [b_oa, b_ident], [b_pta])
            for c in range(3):
                ACT(oT[:, c, :], pta[:, c * 128:(c + 1) * 128], AF.Copy, [b_pta, b_par], [b_oT], scale=gT[:, c:c + 1])
            for c in range(5):
                TS(oT[:, 3 + c, :], of[:, c, :], gT[:, 3 + c:4 + c], None, ALU.mult, ALU.bypass, [b_of, b_par], [b_oT], eng="pool" if False else "dve")
            xw, b_xw = xw_.next()
            for half in range(2):
                hs = slice(half * 512, (half + 1) * 512)
                pys = []
                for (c0, c1) in ((0, 3), (3, 6), (6, 8)):
                    py, b_py = py_.next()
                    for c in range(c0, c1):
                        MM(py[:], oT[:, c, :], Wo[:, c, hs], c == c0, c == c1 - 1, [b_oT, b_W], [b_py])
                    pys.append((py, b_py))
                tm, b_tm = tm_.next()
                TS(tm[:], pys[0][0][:], st[:, 0:1], None, ALU.mult, ALU.bypass, [pys[0][1], b_st], [b_tm])
                STT(tm[:], pys[1][0][:], st[:, 1:2], tm[:], ALU.mult, ALU.add, [pys[1][1], b_st, b_tm], [b_tm])
                STT(tm[:], pys[2][0][:], st[:, 2:3], tm[:], ALU.mult, ALU.add, [pys[2][1], b_st, b_tm], [b_tm])
                TT(tm[:], tm[:], gbc[:, hs], ALU.mult, [b_tm, b_gbc], [b_tm])
                TT(xw[:, hs], tm[:], xt[:, hs], ALU.add, [b_tm, b_x], [b_xw])
            k.dma("pool", dr[f"XA{l}"][tks, :], xw[:], reads=[b_xw], writes=[db[f"XA{l}"]])
            carry[tt] = (xw, b_xw, st, b_st)

        def stageB(tt):
            tks = slice(tt * 128, (tt + 1) * 128)
            xw, b_xw, st, b_st = carry.pop(tt)
            ACT(sq[:], xw[:], AF.Square, [b_xw], [b_sq, b_st], accum_out=st[:, 4:5])
            TS(st[:, 5:6], st[:, 4:5], 1.0 / D, EPS, ALU.mult, ALU.add, [b_st], [b_st])
            ACT(st[:, 5:6], st[:, 5:6], AF.Sqrt, [b_st], [b_st])
            k.op("dve", lambda e, st=st: e.reciprocal(out=st[:, 5:6], in_=st[:, 5:6]), [b_st], [b_st])
            xn, b_xn = xn_.next()
            TS(xn[:], xw[:], st[:, 5:6], None, ALU.mult, ALU.bypass, [b_xw, b_st], [b_xn])
            h2, b_h2 = h2_.next()
            h32, b_h32 = h32_.next()
            for q in range(2):
                ptn, b_ptn = ptn_.next()
                for kq in range(4):
                    kk = q * 4 + kq
                    TR(ptn[:, kq * 128:(kq + 1) * 128], xn[:, kk * 128:(kk + 1) * 128], ident[:], [b_xn, b_ident], [b_ptn])
                for kq in range(4):
                    kk = q * 4 + kq
                    ACT(h2[:, kk, :], ptn[:, kq * 128:(kq + 1) * 128], AF.Identity, [b_ptn, b_gs2T, b_modT], [b_h2],
                        scale=gs2T[:, l * 8 + kk:l * 8 + kk + 1], bias=sh2[:, kk:kk + 1])
                    if moe:
                        ACT(h32[:, kk, :], ptn[:, kq * 128:(kq + 1) * 128], AF.Identity, [b_ptn, b_gs2T, b_modT], [b_h32],
                            scale=gs2T[:, l * 8 + kk:l * 8 + kk + 1], bias=sh2[:, kk:kk + 1])
            k.dma("pool", dr[f"H2T{l}"][:, tks].rearrange("(k p) n -> p k n", p=128), h2[:], reads=[b_h2], writes=[db[f"H2T{l}"]])
            if moe:
                plg, b_plg = plg_.next()
                for kk in range(8):
                    MM(plg[:, 0:8], h32[:, kk, :], rw32[:, kk, :], kk == 0, kk == 7, [b_h32, b_par], [b_plg])
                lg, b_lg = lg_.next()
                CP(lg[:], plg[:, 0:8], [b_plg], [b_lg])
                t8, b_t8 = t8_.next()
                k.op("dve", lambda e, t8=t8, lg=lg: e.max(out=t8[:], in_=lg[:]), [b_lg], [b_t8])
                wv, b_wv = wv_.next()
                TT(wv[:, 0:1], t8[:, 0:1], t8[:, 1:2], ALU.subtract, [b_t8], [b_wv])
                ACT(wv[:, 1:2], wv[:, 0:1], AF.Sigmoid, [b_wv], [b_wv])
                ACT(wv[:, 2:3], wv[:, 0:1], AF.Sigmoid, [b_wv], [b_wv], scale=-1.0)
                TS(rwo[:, tt, 0:8], lg[:], t8[:, 0:1], None, ALU.is_equal, ALU.bypass, [b_lg, b_t8], [b_rwo])
                TS(rwo[:, tt, 8:16], lg[:], t8[:, 1:2], None, ALU.is_equal, ALU.bypass, [b_lg, b_t8], [b_rwo])
                CP(rwo[:, tt, 16:18], wv[:, 1:3], [b_wv], [b_rwo])
        carry = {}
        stageA(0)
        stageA(1)
        for tt in range(NT):
            if tt + 2 < NT:
                stageA(tt + 2)
            stageB(tt)
        if moe:
            k.dma("sp", dr[f"RW{l}"], rwo[:].rearrange("p t e -> p (t e)"), reads=[b_rwo], writes=[db[f"RW{l}"]])
        k.barrier()
        k.emit()


def phase_ffn(P, l, G, ACT, TS, TT, STT, CP, MM, TR):
    nc, k, dr, db = P.nc, P.k, P.dr, P.db
    moe = (l % 2 == 1)
    last = (l == DEPTH - 1)
    if moe:
        passes = [(e, f0, 4) for e in range(N_EXP) for f0 in range(0, 28, 4)]
        wgn, wun, wdn = "moe_wg", "moe_wu", "moe_wd"
    else:
        passes = [(0, 0, 4), (0, 4, 4), (0, 8, 4), (0, 12, 4), (0, 16, 3), (0, 19, 3)]
        wgn, wun, wdn = "ffn_wg", "ffn_wu", "ffn_wd"
    MAXC = 4
    HT = S // 2
    with ExitStack() as es:
        sb = lambda name, shape, dt=F32: es.enter_context(nc.sbuf_tensor(P.nm(name), list(shape), dt))
        ps = lambda name, shape, dt=F32: es.enter_context(nc.psum_tensor(P.nm(name), list(shape), dt))
        h2T = sb("h2T", [128, 8, HT], BF16); b_h2T = Buf()
        acc = sb("acc", [128, 16, D]); b_acc = [Buf() for _ in range(16)]
        rw = sb("rwf", [128, NT, 8]); b_rw = Buf()
        if moe:
            k.dma("sp", rw[:].rearrange("p t e -> p (t e)"), dr[f"RW{l}"], reads=[db[f"RW{l}"]], writes=[b_rw])
        gbc = sb("gbcf", [128, D]); b_gbc = Buf()
        k.dma("sp", gbc[:], dr["GBC"][:, (l * 2 + 1) * D:(l * 2 + 2) * D], reads=[db["GBC"]], writes=[b_gbc])
        b_fg = Buf()
        if last:
            fg = sb("fg", [128, D])
            k.dma("sp", fg[:], dr["final_g"].partition_broadcast(128), writes=[b_fg])
        wg_ = Ring([sb(f"wg{i}", [128, 8, MAXC * 128], BF16) for i in range(2)])
        wu_ = Ring([sb(f"wu{i}", [128, 8, MAXC * 128], BF16) for i in range(2)])
        wd_ = Ring([sb(f"wd{i}", [128, MAXC, D], BF16) for i in range(2)])
        aT_ = Ring([sb(f"aT{i}", [128, MAXC, 512], BF16) for i in range(2)])
        sg_ = Ring([sb(f"sg{i}", [128, 512]) for i in range(2)])
        xt_ = Ring([sb(f"xtf{i}", [128, D]) for i in range(2)])
        sq = sb("sqf", [128, D], BF16); b_sq = Buf()
        st_ = Ring([sb(f"stf{i}", [128, 2]) for i in range(2)])
        pg_ = Ring([ps(f"pgf{i}", [128, 512]) for i in range(2)])
        pu_ = Ring([ps(f"puf{i}", [128, 512]) for i in range(2)])
        pd_ = Ring([ps(f"pdf{i}", [128, 512]) for i in range(4)])
        pending_down = None
        for th in range(2):
            for kk in range(8):
                k.dma("sp", h2T[:, kk, :], dr[f"H2T{l}"][kk * 128:(kk + 1) * 128, th * HT:(th + 1) * HT], reads=[db[f"H2T{l}"]], writes=[b_h2T])
            for pi, (e, f0, nch) in enumerate(passes):
                wg, b_wg = wg_.next()
                wu, b_wu = wu_.next()
                wd, b_wd = wd_.next()
                fs = slice(f0 * 128, (f0 + nch) * 128)
                for kk in range(8):
                    k.dma("pool", wg[:, kk, 0:nch * 128], dr[wgn][e, kk * 128:(kk + 1) * 128, fs], writes=[b_wg])
                    k.dma("pool", wu[:, kk, 0:nch * 128], dr[wun][e, kk * 128:(kk + 1) * 128, fs], writes=[b_wu])
                for fc in range(nch):
                    k.dma("pool", wd[:, fc, :], dr[wdn][e, (f0 + fc) * 128:(f0 + fc + 1) * 128, :], writes=[b_wd])
                def down(tgi, aT, b_aT, wd=wd, b_wd=b_wd, nch=nch, e=e, pi=pi):
                    for ti in range(4):
                        tl = tgi * 4 + ti
                        for half in range(2):
                            hs = slice(half * 512, (half + 1) * 512)
                            pd, b_pd = pd_.next()
                            for fc in range(nch):
                                MM(pd[:], aT[:, fc, ti * 128:(ti + 1) * 128], wd[:, fc, hs], fc == 0, fc == nch - 1, [b_aT, b_wd], [b_pd])
                            if moe:
                                sc = rw[:, th * 16 + tl, e:e + 1]
                                if pi == 0:
                                    TS(acc[:, tl, hs], pd[:], sc, None, ALU.mult, ALU.bypass, [b_pd, b_rw], [b_acc[tl]])
                                else:
                                    STT(acc[:, tl, hs], pd[:], sc, acc[:, tl, hs], ALU.mult, ALU.add, [b_pd, b_rw, b_acc[tl]], [b_acc[tl]])
                            else:
                                if pi == 0:
                                    CP(acc[:, tl, hs], pd[:], [b_pd], [b_acc[tl]])
                                else:
                                    TT(acc[:, tl, hs], acc[:, tl, hs], pd[:], ALU.add, [b_pd, b_acc[tl]], [b_acc[tl]])
                for tgi in range(4):
                    tsl = slice(tgi * 512, (tgi + 1) * 512)
                    aT, b_aT = aT_.next()
                    for fc in range(nch):
                        pg, b_pg = pg_.next()
                        pu, b_pu = pu_.next()
                        for kk in range(8):
                            MM(pg[:], wg[:, kk, fc * 128:(fc + 1) * 128], h2T[:, kk, tsl], kk == 0, kk == 7, [b_wg, b_h2T], [b_pg])
                        for kk in range(8):
                            MM(pu[:], wu[:, kk, fc * 128:(fc + 1) * 128], h2T[:, kk, tsl], kk == 0, kk == 7, [b_wu, b_h2T], [b_pu])
                        sg, b_sg = sg_.next()
                        ACT(sg[:], pg[:], AF.Silu, [b_pg], [b_sg])
                        TT(aT[:, fc, :], sg[:], pu[:], ALU.mult, [b_sg, b_pu], [b_aT])
                    if pending_down is not None:
                        pending_down()
                    pending_down = (lambda tgi=tgi, aT=aT, b_aT=b_aT, down=down: down(tgi, aT, b_aT))
            if pending_down is not None:
                pending_down()
                pending_down = None
            for tl in range(16):
                tt = th * 16 + tl
                tks = slice(tt * 128, (tt + 1) * 128)
                xt, b_x = xt_.next()
                k.dma("sp", xt[:], dr[f"XA{l}"][tks, :], reads=[db[f"XA{l}"]], writes=[b_x])
                TT(acc[:, tl, :], acc[:, tl, :], gbc[:], ALU.mult, [b_acc[tl], b_gbc], [b_acc[tl]])
                TT(xt[:], xt[:], acc[:, tl, :], ALU.add, [b_x, b_acc[tl]], [b_x])
                if not last:
                    k.dma("pool", dr[f"XB{l}"][tks, :], xt[:], reads=[b_x], writes=[db[f"XB{l}"]])
                else:
                    if f"XB{l}" in P.dbg:
                        k.dma("pool", dr[f"XB{l}"][tks, :], xt[:], reads=[b_x], writes=[db[f"XB{l}"]])
                    st, b_st = st_.next()
                    ACT(sq[:], xt[:], AF.Square, [b_x], [b_sq, b_st], accum_out=st[:, 0:1])
                    TS(st[:, 1:2], st[:, 0:1], 1.0 / D, EPS, ALU.mult, ALU.add, [b_st], [b_st])
                    ACT(st[:, 1:2], st[:, 1:2], AF.Sqrt, [b_st], [b_st])
                    k.op("dve", lambda e, st=st: e.reciprocal(out=st[:, 1:2], in_=st[:, 1:2]), [b_st], [b_st])
                    STT(xt[:], xt[:], st[:, 1:2], fg[:], ALU.mult, ALU.mult, [b_x, b_st, b_fg], [b_x])
                    k.dma("pool", dr["y"][tks, :], xt[:], reads=[b_x], writes=[db["y"]])
        k.barrier()
        k.emit()


def phase_moe(P, l, G, ACT, TS, TT, STT, CP, MM, TR):
    nc, k, dr, db = P.nc, P.k, P.dr, P.db
    U32 = mybir.dt.uint32
    TG = 1024
    NGRP = 16
    last = (l == DEPTH - 1)
    IOA = bass.IndirectOffsetOnAxis
    with ExitStack() as es:
        sb = lambda name, shape, dt=F32: es.enter_context(nc.sbuf_tensor(P.nm(name), list(shape), dt))
        s1u = sb("s1u", [128, NT], U32); s2u = sb("s2u", [128, NT], U32); b_su = Buf()
        idxu = sb("idxu", [128, NGRP, 60], U32); b_idx = Buf()
        rt = sb("rt", [128, NT, 18]); b_rt = Buf()
        identb = sb("identb", [128, 128], BF16); b_c = Buf()
        k.dma("sp", identb[:], dr["identb"], writes=[b_c])
        k.dma("sp", rt[:].rearrange("p t e -> p (t e)"), dr[f"RW{l}"], reads=[db[f"RW{l}"]], writes=[b_rt])
        with ExitStack() as es2:
            sb2 = lambda name, shape, dt=F32: es2.enter_context(nc.sbuf_tensor(P.nm(name), list(shape), dt))
            ps2 = lambda name, shape, dt=F32: es2.enter_context(nc.psum_tensor(P.nm(name), list(shape), dt))
            tri = sb2("tri", [128, 128]); on = sb2("on128", [128, 128]); sTv = sb2("sTv", [128, 16])
            base60 = sb2("base60", [128, 60]); mult60 = sb2("mult60", [128, 60])
            k.dma("sp", tri[:], dr["tri"], writes=[b_c]); k.dma("sp", sTv[:], dr["sTv"], writes=[b_c])
            k.dma("sp", base60[:], dr["base60"], writes=[b_c]); k.dma("sp", mult60[:], dr["mult60"], writes=[b_c])
            k.op("dve", lambda e: e.memset(on[:], 1.0), [], [b_c])
            ind = sb2("ind", [128, NT, 8]); b_ind = Buf()
            TT(ind[:], rt[:, :, 0:8], rt[:, :, 8:16], ALU.add, [b_rt], [b_ind])
            pc1 = ps2("pc1", [128, 256]); pc2 = ps2("pc2", [128, 256]); b_pc = Buf()
            indf = ind[:].rearrange("p t e -> p (t e)")
            MM(pc1[:], tri[:], indf, True, True, [b_c, b_ind], [b_pc])
            MM(pc2[:], on[:], indf, True, True, [b_c, b_ind], [b_pc])
            tot = sb2("tot", [128, NT, 8]); b_tot = Buf()
            CP(tot[:].rearrange("p t e -> p (t e)"), pc2[:], [b_pc], [b_tot])
            cum = sb2("cum", [128, 8, NT]); b_cum = Buf()
            for e_ in range(8):
                k.op("dve", lambda e, e_=e_: e.tensor_tensor_scan(out=cum[:, e_, :], data0=on[:, 0:NT], data1=tot[:, :, e_],
                                                                initial=0.0, op0=ALU.mult, op1=ALU.add), [b_tot, b_c], [b_cum])
            cnt = sb2("cnt", [128, 8]); b_cnt = Buf()
            CP(cnt[:], cum[:, :, NT - 1], [b_cum], [b_cnt])
            excl = sb2("excl", [128, 8, NT]); b_ex = Buf()
            TT(excl[:], cum[:], tot[:].rearrange("p t e -> p e t"), ALU.subtract, [b_cum, b_tot], [b_ex])
            pcn = sb2("pcn", [128, 8]); b_pcn = Buf()
            TS(pcn[:], cnt[:], float(TG - 1), 1.0 / TG, ALU.add, ALU.mult, [b_cnt], [b_pcn])
            TS(pcn[:], pcn[:], -0.4995, MAGIC, ALU.add, ALU.add, [b_pcn], [b_pcn])
            TS(pcn[:], pcn[:], MAGIC, float(TG), ALU.subtract, ALU.mult, [b_pcn], [b_pcn])
            incl = sb2("incl", [128, 8]); b_incl = Buf()
            k.op("dve", lambda e: e.tensor_tensor_scan(out=incl[:], data0=on[:, 0:8], data1=pcn[:], initial=0.0, op0=ALU.mult, op1=ALU.add),
                 [b_pcn, b_c], [b_incl])
            bse = sb2("bse", [128, 8]); b_bse = Buf()
            TT(bse[:], incl[:], pcn[:], ALU.subtract, [b_incl, b_pcn], [b_bse])
            slot = sb2("slot", [128, 8, NT]); b_slot = Buf()
            TT(slot[:], pc1[:].rearrange("p (t e) -> p e t", e=8), excl[:], ALU.add, [b_pc, b_ex], [b_slot])
            for e_ in range(8):
                TS(slot[:, e_, :], slot[:, e_, :], bse[:, e_:e_ + 1], None, ALU.add, ALU.bypass, [b_slot, b_bse], [b_slot])
            tmp = sb2("tmpsl", [128, 8, NT]); b_tmp = Buf()
            sf = sb2("sf", [128, 2, NT]); b_sf = Buf()
            for j in range(2):
                TT(tmp[:], slot[:], rt[:, :, j * 8:(j + 1) * 8].rearrange("p t e -> p e t"), ALU.mult, [b_slot, b_rt, b_tmp], [b_tmp])
                k.op("dve", lambda e, j=j: e.tensor_reduce(out=sf[:, j, :], in_=tmp[:].rearrange("p e t -> p t e"), axis=AX.X, op=ALU.add),
                     [b_tmp], [b_sf])
            CP(s1u[:], sf[:, 0, :], [b_sf], [b_su])
            CP(s2u[:], sf[:, 1, :], [b_sf], [b_su])
            cmp_ = sb2("cmp", [128, 16, 8]); b_cmp = Buf()
            for e_ in range(8):
                TS(cmp_[:, :, e_], sTv[:], incl[:, e_:e_ + 1], None, ALU.is_ge, ALU.bypass, [b_c, b_incl], [b_cmp])
            eid = sb2("eid", [128, 16]); b_eid = Buf()
            k.op("dve", lambda e: e.tensor_reduce(out=eid[:], in_=cmp_[:], axis=AX.X, op=ALU.add), [b_cmp], [b_eid])
            TS(eid[:], eid[:], 7.0, None, ALU.min, ALU.bypass, [b_eid], [b_eid])
            idxf = sb2("idxf", [128, NGRP, 60]); b_if = Buf()
            for s_ in range(NGRP):
                STT(idxf[:, s_, :], mult60[:], eid[:, s_:s_ + 1], base60[:], ALU.mult, ALU.add, [b_c, b_eid], [b_if])
            CP(idxu[:].rearrange("p s c -> p (s c)"), idxf[:].rearrange("p s c -> p (s c)"), [b_if], [b_idx])
            if "SLOTS" in P.dbg:
                k.dma("sp", dr["SLOTS"][:, 0:NT], s1u[:], reads=[b_su], writes=[db["SLOTS"]])
                k.dma("sp", dr["SLOTS"][:, NT:2 * NT], s2u[:], reads=[b_su], writes=[db["SLOTS"]])
            h2t_ = Ring([sb2(f"h2t{i}", [128, 8, 128], BF16) for i in range(6)])
            hrow_ = Ring([sb2(f"hrow{i}", [128, D], BF16) for i in range(6)])
            pT_ = Ring([ps2(f"pTs{i}", [128, D], BF16) for i in range(2)])
            for tt in range(NT):
                tks = slice(tt * 128, (tt + 1) * 128)
                h2t, b_h2t = h2t_.next()
                k.dma("sp", h2t[:], dr[f"H2T{l}"][:, tks].rearrange("(k p) n -> p k n", p=128), reads=[db[f"H2T{l}"]], writes=[b_h2t])
                pT, b_pT = pT_.next()
                for kk in range(8):
                    TR(pT[:, kk * 128:(kk + 1) * 128], h2t[:, kk, :], identb[:], [b_h2t, b_c], [b_pT])
                hrow, b_hrow = hrow_.next()
                if tt % 2 == 0:
                    CP(hrow[:], pT[:], [b_pT], [b_hrow])
                else:
                    ACT(hrow[:], pT[:], AF.Copy, [b_pT], [b_hrow])
                for su in (s1u, s2u):
                    k.op("pool", lambda e, su=su, tt=tt, hrow=hrow: e.indirect_dma_start(
                        out=dr[f"HS{l}"], out_offset=IOA(ap=su[:, tt:tt + 1], axis=0), in_=hrow[:], in_offset=None),
                        [b_hrow, b_su], [db[f"HS{l}"]], dma=True)
            k.barrier()
        NCH = 7
        gbc = sb("gbcm", [128, D]); b_gbc = Buf()
        k.dma("sp", gbc[:], dr["GBC"][:, (l * 2 + 1) * D:(l * 2 + 2) * D], reads=[db["GBC"]], writes=[b_gbc])
        b_fg = Buf()
        if last:
            fg = sb("fgm", [128, D])
            k.dma("sp", fg[:], dr["final_g"].partition_broadcast(128), writes=[b_fg])
        with ExitStack() as es3:
            sb3 = lambda name, shape, dt=F32: es3.enter_context(nc.sbuf_tensor(P.nm(name), list(shape), dt))
            ps3 = lambda name, shape, dt=F32: es3.enter_context(nc.psum_tensor(P.nm(name), list(shape), dt))
            hs_ = Ring([sb3(f"hs{i}", [128, 8, D], BF16) for i in range(1)])
            hT_ = Ring([sb3(f"hTm{i}", [128, 8, TG], BF16) for i in range(2)])
            acc = sb3("accm", [128, 8, D]); b_acc = [Buf() for _ in range(8)]
            wg_ = Ring([sb3(f"wgm{i}", [128, 8, NCH * 128], BF16) for i in range(2)])
            wu_ = Ring([sb3(f"wum{i}", [128, 8, NCH * 128], BF16) for i in range(2)])
            wd_ = Ring([sb3(f"wdm{i}", [128, NCH, D], BF16) for i in range(2)])
            aT_ = Ring([sb3(f"aTm{i}", [128, NCH, 512], BF16) for i in range(2)])
            sg_ = Ring([sb3(f"sgm{i}", [128, 512]) for i in range(2)])
            pg_ = Ring([ps3(f"pgm{i}", [128, 512]) for i in range(2)])
            pu_ = Ring([ps3(f"pum{i}", [128, 512]) for i in range(2)])
            pd_ = Ring([ps3(f"pdm{i}", [128, 512]) for i in range(3)])
            pT_ = Ring([ps3(f"pTm{i}", [128, D], BF16) for i in range(1)])
            pending = [None]

            def prep(s_):
                hs, b_hs = hs_.next()
                k.dma("sp", hs[:], dr[f"HS{l}"][s_ * TG:(s_ + 1) * TG, :].rearrange("(t p) d -> p t d", p=128), reads=[db[f"HS{l}"]], writes=[b_hs])
                hT, b_hT = hT_.next()
                for kk in range(8):
                    pT, b_pT = pT_.next()
                    for j in range(8):
                        TR(pT[:, j * 128:(j + 1) * 128], hs[:, j, kk * 128:(kk + 1) * 128], identb[:], [b_hs, b_c], [b_pT])
                    if kk % 2 == 0:
                        ACT(hT[:, kk, :], pT[:], AF.Copy, [b_pT], [b_hT])
                    else:
                        CP(hT[:, kk, :], pT[:], [b_pT], [b_hT])
                return hT, b_hT

            nxt = prep(0)
            for s_ in range(NGRP):
                hT, b_hT = nxt
                for pi in range(4):
                    wg, b_wg = wg_.next()
                    wu, b_wu = wu_.next()
                    wd, b_wd = wd_.next()
                    for kk in range(8):
                        col = kk * 4 + pi
                        k.op("pool", lambda e, wg=wg, kk=kk, s_=s_, col=col: e.indirect_dma_start(
                            out=wg[:, kk, :], out_offset=None, in_=dr["moe_wg"], in_offset=IOA(ap=idxu[:, s_, col:col + 1], axis=0)),
                            [b_idx], [b_wg], dma=True)
                        k.op("pool", lambda e, wu=wu, kk=kk, s_=s_, col=col: e.indirect_dma_start(
                            out=wu[:, kk, :], out_offset=None, in_=dr["moe_wu"], in_offset=IOA(ap=idxu[:, s_, col:col + 1], axis=0)),
                            [b_idx], [b_wu], dma=True)
                    for fc in range(NCH):
                        col = 32 + pi * NCH + fc
                        k.op("pool", lambda e, wd=wd, fc=fc, s_=s_, col=col: e.indirect_dma_start(
                            out=wd[:, fc, :], out_offset=None, in_=dr["moe_wd"], in_offset=IOA(ap=idxu[:, s_, col:col + 1], axis=0)),
                            [b_idx], [b_wd], dma=True)

                    def down(tgi, aT, b_aT, wd=wd, b_wd=b_wd, pi=pi):
                        for ti in range(4):
                            tl = tgi * 4 + ti
                            for half in range(2):
                                hsl = slice(half * 512, (half + 1) * 512)
                                pd, b_pd = pd_.next()
                                for fc in range(NCH):
                                    MM(pd[:], aT[:, fc, ti * 128:(ti + 1) * 128], wd[:, fc, hsl], fc == 0, fc == NCH - 1, [b_aT, b_wd], [b_pd])
                                if pi == 0:
                                    CP(acc[:, tl, hsl], pd[:], [b_pd], [b_acc[tl]])
                                else:
                                    TT(acc[:, tl, hsl], acc[:, tl, hsl], pd[:], ALU.add, [b_pd, b_acc[tl]], [b_acc[tl]])

                    for tgi in range(2):
                        tsl = slice(tgi * 512, (tgi + 1) * 512)
                        aT, b_aT = aT_.next()
                        for fc in range(NCH):
                            pg, b_pg = pg_.next()
                            pu, b_pu = pu_.next()
                            for kk in range(8):
                                MM(pg[:], wg[:, kk, fc * 128:(fc + 1) * 128], hT[:, kk, tsl], kk == 0, kk == 7, [b_wg, b_hT], [b_pg])
                            for kk in range(8):
                                MM(pu[:], wu[:, kk, fc * 128:(fc + 1) * 128], hT[:, kk, tsl], kk == 0, kk == 7, [b_wu, b_hT], [b_pu])
                            sg, b_sg = sg_.next()
                            ACT(sg[:], pg[:], AF.Silu, [b_pg], [b_sg])
                            TT(aT[:, fc, :], sg[:], pu[:], ALU.mult, [b_sg, b_pu], [b_aT])
                        if pending[0] is not None:
                            pending[0]()
                        pending[0] = (lambda tgi=tgi, aT=aT, b_aT=b_aT, down=down: down(tgi, aT, b_aT))
                    if pi == 2 and s_ + 1 < NGRP:
                        nxt = prep(s_ + 1)
                pending[0]()
                pending[0] = None
                k.dma("sp", dr[f"OS{l}"][s_ * TG:(s_ + 1) * TG, :].rearrange("(t p) d -> p t d", p=128), acc[:], reads=b_acc, writes=[db[f"OS{l}"]])
            k.barrier()
        r1_ = Ring([sb(f"r1{i}", [128, D]) for i in range(4)])
        r2_ = Ring([sb(f"r2{i}", [128, D]) for i in range(4)])
        xt_ = Ring([sb(f"xtm{i}", [128, D]) for i in range(4)])
        sq = sb("sqm", [128, D], BF16); b_sq = Buf()
        st_ = Ring([sb(f"stm{i}", [128, 2]) for i in range(4)])
        for tt in range(NT):
            tks = slice(tt * 128, (tt + 1) * 128)
            r1, b_r1 = r1_.next()
            r2, b_r2 = r2_.next()
            k.op("pool", lambda e, r1=r1, tt=tt: e.indirect_dma_start(out=r1[:], out_offset=None, in_=dr[f"OS{l}"],
                                                                     in_offset=IOA(ap=s1u[:, tt:tt + 1], axis=0)),
                 [db[f"OS{l}"], b_su], [b_r1], dma=True)
            k.op("pool", lambda e, r2=r2, tt=tt: e.indirect_dma_start(out=r2[:], out_offset=None, in_=dr[f"OS{l}"],
                                                                     in_offset=IOA(ap=s2u[:, tt:tt + 1], axis=0)),
                 [db[f"OS{l}"], b_su], [b_r2], dma=True)
            xt, b_x = xt_.next()
            k.dma("sp", xt[:], dr[f"XA{l}"][tks, :], reads=[db[f"XA{l}"]], writes=[b_x])
            TS(r1[:], r1[:], rt[:, tt, 16:17], None, ALU.mult, ALU.bypass, [b_r1, b_rt], [b_r1])
            STT(r1[:], r2[:], rt[:, tt, 17:18], r1[:], ALU.mult, ALU.add, [b_r2, b_rt, b_r1], [b_r1])
            TT(r1[:], r1[:], gbc[:], ALU.mult, [b_r1, b_gbc], [b_r1])
            TT(xt[:], xt[:], r1[:], ALU.add, [b_x, b_r1], [b_x])
            if not last:
                k.dma("act", dr[f"XB{l}"][tks, :], xt[:], reads=[b_x], writes=[db[f"XB{l}"]])
            else:
                if f"XB{l}" in P.dbg:
                    k.dma("act", dr[f"XB{l}"][tks, :], xt[:], reads=[b_x], writes=[db[f"XB{l}"]])
                st, b_st = st_.next()
                ACT(sq[:], xt[:], AF.Square, [b_x], [b_sq, b_st], accum_out=st[:, 0:1])
                TS(st[:, 1:2], st[:, 0:1], 1.0 / D, EPS, ALU.mult, ALU.add, [b_st], [b_st])
                ACT(st[:, 1:2], st[:, 1:2], AF.Sqrt, [b_st], [b_st])
                k.op("dve", lambda e, st=st: e.reciprocal(out=st[:, 1:2], in_=st[:, 1:2]), [b_st], [b_st])
                STT(xt[:], xt[:], st[:, 1:2], fg[:], ALU.mult, ALU.mult, [b_x, b_st, b_fg], [b_x])
                k.dma("act", dr["y"][tks, :], xt[:], reads=[b_x], writes=[db["y"]])
        k.barrier()
        k.emit()


def gelu_tanh(P, out, b_out, x, b_x, ra_, rb_, ACT, TS, TT):
    ra, b_ra = ra_.next()
    rb, b_rb = rb_.next()
    ACT(ra[:], x[:], AF.Square, [b_x], [b_ra])
    TS(ra[:], ra[:], 0.044715, 1.0, ALU.mult, ALU.add, [b_ra], [b_ra])
    TT(rb[:], ra[:], x[:], ALU.mult, [b_ra, b_x], [b_rb])
    ACT(rb[:], rb[:], AF.Sigmoid, [b_rb], [b_rb], scale=1.5957691216057308)
    TT(out[:], rb[:], x[:], ALU.mult, [b_rb, b_x], [b_out])


def _consts():
    inv = (10000.0 ** (-np.arange(0, 64, 2, dtype=np.float32) / 64)).astype(np.float32)
    invt = (inv.astype(np.float64) / (2 * np.pi)).astype(np.float32)
    invt128 = np.tile(invt, 4).reshape(128, 1).astype(np.float32)
    bf = ml_dtypes.bfloat16
    ohk = (np.arange(S)[None, :] // 256 == np.arange(16)[:, None]).astype(np.float32).astype(bf)
    kk_ = np.arange(128)[:, None, None] + 128 * np.arange(4)[None, :, None]
    qq_ = np.arange(512)[None, None, :]
    same = (kk_ // 256) == (qq_ // 256)
    cbias = np.where(same & (kk_ > qq_), NEGB, 0.0).astype(np.float32).reshape(128, 2048).astype(bf)
    n_ = np.arange(16)[None, :]
    j_ = np.arange(16)[:, None]
    padm = np.broadcast_to((n_ < j_).astype(np.float32).reshape(1, 256), (128, 256))
    pada = np.broadcast_to(np.where(n_ == j_, 1e30, np.where(n_ > j_, -1e30, 0.0)).astype(np.float32).reshape(1, 256), (128, 256))
    tv = np.ascontiguousarray(np.broadcast_to(np.arange(S, dtype=np.float32)[None], (128, S)))
    top = (np.arange(128) < 64).astype(np.float32)
    tbm = np.stack([top, -top, np.full(128, np.pi / 2, np.float32), -(1 - top)], axis=1).astype(np.float32)
    p_ = np.arange(128)[:, None]
    tri = (np.arange(128)[:, None] < np.arange(128)[None, :]).astype(np.float32)
    sTv = np.ascontiguousarray(np.broadcast_to((np.arange(16, dtype=np.float32) * 1024.0)[None], (128, 16)))
    b_gu = ((np.arange(8)[None, :, None] * 128 + p_[:, :, None]) * 4 + np.arange(4)[None, None, :]).reshape(128, 32)
    b_d = (np.arange(28)[None, :] * 128 + p_)
    base60 = np.concatenate([b_gu, b_d], axis=1).astype(np.float32)
    mult60 = np.ascontiguousarray(np.broadcast_to(np.concatenate([np.full(32, 4096.0), np.full(28, 3584.0)])[None], (128, 60))).astype(np.float32)
    padm32 = np.ascontiguousarray(padm.reshape(128, 16, 16)[:, np.arange(32) // 2, :].reshape(128, 512))
    pada32 = np.ascontiguousarray(pada.reshape(128, 16, 16)[:, np.arange(32) // 2, :].reshape(128, 512))
    return dict(padm32=padm32, pada32=pada32, tri=tri, sTv=sTv, base60=base60, mult60=mult60, tvals=tv, tbm=tbm, invt=invt128, ident=np.eye(128, dtype=np.float32), ohk=ohk, identb=np.eye(128, dtype=np.float32).astype(bf),
                cbias=cbias, padm=np.ascontiguousarray(padm), pada=np.ascontiguousarray(pada))


def _swap_heads(w):
    w4 = w.reshape(w.shape[0], 6, 2, 32)
    return np.ascontiguousarray(w4[:, :, ::-1, :]).reshape(w.shape[0], 384)


def make_inputs(inp, b):
    f = lambda a: np.ascontiguousarray(a, dtype=np.float32)
    if "consts" not in _CACHE:
        _CACHE["consts"] = _consts()
    m = dict(_CACHE["consts"])
    m["x"] = f(inp["x"][b])
    m["cT"] = f(inp["c"][b].reshape(8, 128).T)
    m["pos"] = np.ascontiguousarray(inp["positions"][b].reshape(1, S).astype(np.int32))
    w_in = inp["w_in"]
    ext = []
    for l in range(DEPTH):
        w = w_in[l]
        q, kk_, rest = w[:, 0:384], w[:, 384:768], w[:, 768:]
        ext.append(np.concatenate([q, _swap_heads(q), kk_, _swap_heads(kk_), rest], axis=1))
    m["w_in"] = f(np.stack(ext))
    m["ada_w"] = f(inp["ada_w"])
    m["ada_b"] = f(inp["ada_b"])
    m["ada_bT"] = f(inp["ada_b"].reshape(DEPTH, 48, 128).transpose(2, 0, 1).reshape(128, DEPTH * 48))
    m["g1T"] = f(inp["norm1_g"].reshape(DEPTH, 8, 128).transpose(2, 0, 1).reshape(128, DEPTH * 8))
    m["g2T"] = f(inp["norm2_g"].reshape(DEPTH, 8, 128).transpose(2, 0, 1).reshape(128, DEPTH * 8))
    m["w_out"] = f(inp["w_out"])
    m["gainT"] = f(inp["mix_gain"].reshape(DEPTH, 8, 128).transpose(2, 0, 1).reshape(128, DEPTH * 8))
    m["router_w"] = f(inp["router_w"][0].reshape(8, 128, 8).transpose(1, 0, 2).reshape(128, 64))
    m["ffn_wg"] = f(inp["ffn_w_gate"]); m["ffn_wu"] = f(inp["ffn_w_up"]); m["ffn_wd"] = f(inp["ffn_w_down"])
    m["moe_wg"] = f(inp["moe_w_gate"][0]).reshape(N_EXP * D * 4, 896)
    m["moe_wu"] = f(inp["moe_w_up"][0]).reshape(N_EXP * D * 4, 896)
    m["moe_wd"] = f(inp["moe_w_down"][0]).reshape(N_EXP * D_FFE, D)
    m["final_g"] = f(inp["final_g"].reshape(1, D))
    L = DEPTH
    dup = lambda a: np.concatenate([a, a], axis=0)
    lrT = dup(inp["s5_lambda_re"].transpose(2, 0, 1))
    liT = dup(inp["s5_lambda_im"].transpose(2, 0, 1))
    ldt = np.broadcast_to(inp["s5_log_dt"][None], (128, L, 16))
    m["s5p"] = f(np.stack([lrT, liT, ldt], axis=2).reshape(128, L * 48))
    br, bi = inp["s5_b_re"], inp["s5_b_im"]
    b1 = np.zeros((128, L, 16, 128), np.float32)
    b2 = np.zeros((128, L, 16, 128), np.float32)
    for g in range(16):
        r0 = 16 * (g % 8)
        b1[r0:r0 + 16, :, g, 0:64] = br[:, g].transpose(2, 0, 1)
        b1[r0:r0 + 16, :, g, 64:128] = bi[:, g].transpose(2, 0, 1)
        b2[r0:r0 + 16, :, g, 0:64] = bi[:, g].transpose(2, 0, 1)
        b2[r0:r0 + 16, :, g, 64:128] = br[:, g].transpose(2, 0, 1)
    m["s5_b1"] = f(b1.reshape(128, -1))
    m["s5_b2"] = f(b2.reshape(128, -1))
    cre = dup(inp["s5_c_re"].transpose(3, 0, 1, 2))
    cim = dup(inp["s5_c_im"].transpose(3, 0, 1, 2))
    m["s5_c"] = f(np.stack([cre, cim], axis=2).reshape(128, L * 512))
    dsk = inp["s5_d"].reshape(L, 2, 128).transpose(2, 0, 1)
    glb = inp["s5_glu_b"].reshape(L, 2, 128).transpose(2, 0, 1)
    m["s5_dg"] = f(np.concatenate([dsk, glb], axis=2).reshape(128, L * 4))
    m["s5_gw"] = f(inp["s5_glu_w"])
    cw = inp["lru_conv_w"].reshape(DEPTH, 4, 3, 128).transpose(3, 0, 2, 1).reshape(128, DEPTH * 12)
    m["lru_cw"] = f(cw)
    vecs = np.stack([inp["lru_conv_b"], inp["lru_b_a"], inp["lru_b_x"], inp["lru_lambda"]], axis=0)
    m["lru_vec"] = f(vecs.reshape(4, DEPTH, 3, 128).transpose(3, 1, 2, 0).reshape(128, DEPTH * 12))
    for nm, key in (("lru_wa", "lru_w_a"), ("lru_wx", "lru_w_x")):
        w = inp[key]
        bd = np.zeros((DEPTH, 3, 128, 128), np.float32)
        for c in range(3):
            for bb in range(2):
                bd[:, c, 64 * bb:64 * bb + 64, 64 * bb:64 * bb + 64] = w[:, 2 * c + bb]
        m[nm] = f(bd.transpose(2, 0, 1, 3).reshape(128, DEPTH * 3 * 128))
    return m


def kernel(**inputs):
    if "prog" not in _CACHE:
        _CACHE["prog"] = build()
    P = _CACHE["prog"]
    names = [n for n in P.dr if True]
    in_maps = []
    for b in range(8):
        m = make_inputs(inputs, b)
        in_maps.append(m)
    res = run_bass_kernel_spmd(P.nc, in_maps, core_ids=list(range(8)))
    return np.stack([np.asarray(r["y"], dtype=np.float32) for r in res.results], axis=0)
```
